# Optimizing a Trainium2 kernel written in Bass

```python
import math
import jax, jax.numpy as jnp
from jax import lax
import numpy as np

D_MODEL = 1024
BATCH = 8
SEQ = 2048
DEPTH = 2

CTX_LEN = 256
GRID_W = 64
EPS = 1e-6
F32 = jnp.float32

HY_WIDTH = D_MODEL // 2
HY_ORDER = 2
HY_IN = (HY_ORDER + 1) * HY_WIDTH
HY_SHORT = 3
HY_BANDS = 16
HY_EMB = 2 * HY_BANDS + 1
HY_HIDDEN = 64
HY_DECAY_SLOW = -math.log(1e-2) / 1.5
HY_DECAY_FAST = -math.log(1e-2) / 0.3
HEAD_DIM = 64
N_Q_HEADS = (D_MODEL // 2) // HEAD_DIM
N_KV_HEADS = N_Q_HEADS // 4
Q_PER_KV = N_Q_HEADS // N_KV_HEADS
ATTN_WIDTH = N_Q_HEADS * HEAD_DIM
KV_WIDTH = N_KV_HEADS * HEAD_DIM
IN_EVEN = HY_IN + ATTN_WIDTH + 2 * KV_WIDTH
MIX_EVEN = HY_WIDTH + ATTN_WIDTH
ROPE_THETA = 10000.0
Q_BLOCK = 128
D_FF = 256 * ((8 * D_MODEL // 3 + 255) // 256)

S5_WIDTH = D_MODEL
S5_GROUP = 16
S5_GROUPS = S5_WIDTH // S5_GROUP
S5_STATE = 64
S5_DT_MIN = 1e-3
S5_DT_MAX = 1e-1
N_EXPERTS = 8
TOP_K = 2
D_FF_EXPERT = 7 * D_MODEL // 2

N_EVEN = (DEPTH + 1) // 2
N_ODD = DEPTH // 2

kernel_name = 'hybrid_hyena_gqa_s5_moe_dit'


def rms_norm(x, gain):
    xf = x.astype(F32)
    xf = xf * lax.rsqrt(jnp.mean(xf * xf, axis=-1, keepdims=True) + EPS)
    return (xf * gain.astype(F32)).astype(x.dtype)


def ada_params(cond, w, b):
    m = jax.nn.silu(cond) @ w + b
    m = m.reshape(m.shape[:-1] + (1, 6, D_MODEL))
    return tuple(m[..., k, :] for k in range(6))


def modulate(h, shift, scale):
    return h * (1.0 + scale) + shift


def centred_short_conv(u, w, b):
    L = u.shape[1]
    pad = HY_SHORT // 2
    up = jnp.pad(u, ((0, 0), (pad, pad), (0, 0)))
    y = b
    for k in range(HY_SHORT):
        y = y + up[:, k:k + L] * w[k]
    return y


def hyena_filters(L, w1, b1, w2, b2, wout, freq):
    t = jnp.linspace(0.0, 1.0, L, dtype=F32)[:, None]
    bands = jnp.linspace(1e-4, HY_BANDS - 1, HY_BANDS, dtype=F32)
    phase = (2.0 * math.pi / L) * jnp.arange(L, dtype=F32)[:, None] * bands
    z = jnp.concatenate([t, jnp.cos(phase), -jnp.sin(phase)], axis=-1)
    fr = freq.astype(F32)
    h = jnp.sin(fr * (z @ w1.astype(F32) + b1.astype(F32)))
    h = jnp.sin(fr * (h @ w2.astype(F32) + b2.astype(F32)))
    h = (h @ wout.astype(F32)).reshape(L, 2, HY_ORDER, HY_WIDTH)
    deltas = jnp.linspace(HY_DECAY_SLOW, HY_DECAY_FAST, HY_WIDTH, dtype=F32)
    decay = jnp.exp(-t * deltas)
    return h * decay[:, None, None, :]


def two_sided_fftconv(u, h2, skip):
    L, C = u.shape[1], u.shape[2]
    k = jnp.concatenate([h2[:, 0], jnp.zeros((1, C), F32), h2[:0:-1, 1]], axis=0)
    kf = jnp.fft.rfft(k, axis=0)
    uf = jnp.fft.rfft(u.astype(F32), n=2 * L, axis=1)
    y = jnp.fft.irfft(uf * kf, n=2 * L, axis=1)[:, :L]
    return (y + u.astype(F32) * skip.astype(F32)).astype(u.dtype)


def hyena_mixer(p, filt, conv_w, conv_b, skip):
    z = centred_short_conv(p, conv_w, conv_b)
    parts = jnp.split(z, HY_ORDER + 1, axis=-1)
    v = parts[0]
    for o in range(HY_ORDER):
        v = parts[o + 1] * two_sided_fftconv(v, filt[:, :, o], skip[o])
    return v


def axial_rope(L):
    rows = L // GRID_W
    row = jnp.repeat(jnp.arange(rows, dtype=F32), GRID_W)
    col = jnp.tile(jnp.arange(GRID_W, dtype=F32), rows)
    n_freq = HEAD_DIM // 4
    inv = ROPE_THETA ** (-jnp.arange(n_freq, dtype=F32) / n_freq)
    ang = jnp.concatenate([row[:, None] * inv, col[:, None] * inv], axis=-1)
    return jnp.cos(ang), jnp.sin(ang)


def apply_rope(x, cos, sin):
    xf = x.astype(F32)
    x1, x2 = jnp.split(xf, 2, axis=-1)
    c = cos[None, :, None, :]
    s = sin[None, :, None, :]
    return jnp.concatenate([x1 * c - x2 * s, x1 * s + x2 * c], axis=-1).astype(x.dtype)


def gqa_core(q, k, v):
    s = jnp.einsum('bqhgd,bkhd->bhgqk', q, k).astype(F32) * (HEAD_DIM ** -0.5)
    p = jax.nn.softmax(s, axis=-1).astype(v.dtype)
    return jnp.einsum('bhgqk,bkhd->bqhgd', p, v)


def gqa_context(q, k, v):
    B, L = q.shape[:2]
    o = gqa_core(q.reshape(B, L, N_KV_HEADS, Q_PER_KV, HEAD_DIM), k, v)
    return o.reshape(B, L, ATTN_WIDTH)


def gqa_latent_blocked(q, k, v):
    B, L = q.shape[:2]
    nb = L // Q_BLOCK
    qb = q.reshape(B, nb, Q_BLOCK, N_KV_HEADS, Q_PER_KV, HEAD_DIM).transpose(1, 0, 2, 3, 4, 5)
    o = lax.map(lambda qi: gqa_core(qi, k, v), qb)
    return o.transpose(1, 0, 2, 3, 4, 5).reshape(B, L, ATTN_WIDTH)


def even_mixer(h_l, h_c, w_in, conv_w, conv_b, f_w1, f_b1, f_w2, f_b2, f_wout, f_freq, hy_skip,
               q_norm, k_norm, w_out, need_ctx):
    def project(h):
        B, L = h.shape[:2]
        p = h @ w_in
        hy = p[..., :HY_IN]
        q = p[..., HY_IN:HY_IN + ATTN_WIDTH].reshape(B, L, N_Q_HEADS, HEAD_DIM)
        k = p[..., HY_IN + ATTN_WIDTH:HY_IN + ATTN_WIDTH + KV_WIDTH].reshape(B, L, N_KV_HEADS, HEAD_DIM)
        v = p[..., HY_IN + ATTN_WIDTH + KV_WIDTH:].reshape(B, L, N_KV_HEADS, HEAD_DIM)
        return hy, rms_norm(q, q_norm), rms_norm(k, k_norm), v

    filt_args = (f_w1, f_b1, f_w2, f_b2, f_wout, f_freq)
    hy_c, q_c, k_c, v_c = project(h_c)
    hy_l, q_l, k_l, v_l = project(h_l)
    L = h_l.shape[1]
    cos, sin = axial_rope(L)
    q_l = apply_rope(q_l, cos, sin)
    k_l = apply_rope(k_l, cos, sin)
    y_hy_l = hyena_mixer(hy_l, hyena_filters(L, *filt_args), conv_w, conv_b, hy_skip)
    y_at_l = gqa_latent_blocked(q_l, jnp.concatenate([k_c, k_l], axis=1),
                                jnp.concatenate([v_c, v_l], axis=1))
    out_l = jnp.concatenate([y_hy_l, y_at_l], axis=-1) @ w_out
    out_c = None
    if need_ctx:
        y_hy_c = hyena_mixer(hy_c, hyena_filters(h_c.shape[1], *filt_args), conv_w, conv_b, hy_skip)
        y_at_c = gqa_context(q_c, k_c, v_c)
        out_c = jnp.concatenate([y_hy_c, y_at_c], axis=-1) @ w_out
    return out_l, out_c


def s5_discretise(lam_re, lam_im, log_step, b_re, b_im):
    lr = jnp.minimum(lam_re.astype(F32), -1e-4)
    li = lam_im.astype(F32)
    dt = jnp.exp(log_step.astype(F32))[:, None]
    mag = jnp.exp(lr * dt)
    ab_re = mag * jnp.cos(li * dt)
    ab_im = mag * jnp.sin(li * dt)
    den = lr * lr + li * li
    nr, ni = ab_re - 1.0, ab_im
    co_re = (nr * lr + ni * li) / den
    co_im = (ni * lr - nr * li) / den
    br, bi = b_re.astype(F32), b_im.astype(F32)
    bb_re = co_re[..., None] * br - co_im[..., None] * bi
    bb_im = co_re[..., None] * bi + co_im[..., None] * br
    return ab_re, ab_im, bb_re, bb_im


def s5_scan(bu_re, bu_im, ab_re, ab_im, s0_re, s0_im):
    bu_re = bu_re.at[:, 0].add(ab_re * s0_re - ab_im * s0_im)
    bu_im = bu_im.at[:, 0].add(ab_re * s0_im + ab_im * s0_re)
    L = bu_re.shape[1]
    a_re = jnp.broadcast_to(ab_re, (1, L) + ab_re.shape)
    a_im = jnp.broadcast_to(ab_im, (1, L) + ab_im.shape)

    def combine(e1, e2):
        a1r, a1i, b1r, b1i = e1
        a2r, a2i, b2r, b2i = e2
        return (a2r * a1r - a2i * a1i, a2r * a1i + a2i * a1r,
                a2r * b1r - a2i * b1i + b2r, a2r * b1i + a2i * b1r + b2i)

    _, _, s_re, s_im = lax.associative_scan(combine, (a_re, a_im, bu_re, bu_im), axis=1)
    return s_re, s_im


def s5_direction(u_c, u_l, lam_re, lam_im, log_step, b_re, b_im, c_re, c_im, need_ctx):
    ab_re, ab_im, bb_re, bb_im = s5_discretise(lam_re, lam_im, log_step, b_re, b_im)
    cr, ci = c_re.astype(F32), c_im.astype(F32)

    def drive(u):
        return (jnp.einsum('blgk,gnk->blgn', u, bb_re), jnp.einsum('blgk,gnk->blgn', u, bb_im))

    def readout(s_re, s_im):
        return jnp.einsum('blgn,gkn->blgk', s_re, cr) - jnp.einsum('blgn,gkn->blgk', s_im, ci)

    zero = jnp.zeros(u_c.shape[:1] + ab_re.shape, F32)
    sc_re, sc_im = s5_scan(*drive(u_c), ab_re, ab_im, zero, zero)
    sl_re, sl_im = s5_scan(*drive(u_l), ab_re, ab_im, sc_re[:, -1], sc_im[:, -1])
    y_l = readout(sl_re, sl_im)
    y_c = readout(sc_re, sc_im) if need_ctx else None
    return y_l, y_c


def s5_glu(y, w_a, w_b, dtype):
    z = jax.nn.gelu(y).astype(dtype)
    return (z @ w_a) * jax.nn.sigmoid(z @ w_b)


def odd_mixer(h_l, h_c, w_in, lam_re, lam_im, log_step, b_re, b_im, c_re, c_im, d_skip, w_a, w_b, need_ctx):
    B, L = h_l.shape[:2]
    Lc = h_c.shape[1]
    u_l = (h_l @ w_in).astype(F32)
    u_c = (h_c @ w_in).astype(F32)
    gl = u_l.reshape(B, L, S5_GROUPS, S5_GROUP)
    gc = u_c.reshape(B, Lc, S5_GROUPS, S5_GROUP)
    yf_l, yf_c = s5_direction(gc, gl, lam_re[0], lam_im[0], log_step[0], b_re[0], b_im[0],
                              c_re[0], c_im[0], need_ctx)
    yb_l, yb_c = s5_direction(gc[:, ::-1], gl[:, ::-1], lam_re[1], lam_im[1], log_step[1], b_re[1], b_im[1],
                              c_re[1], c_im[1], need_ctx)
    d = d_skip.astype(F32)
    y_l = (yf_l + yb_l[:, ::-1]).reshape(B, L, S5_WIDTH) + d * u_l
    out_l = s5_glu(y_l, w_a, w_b, h_l.dtype)
    out_c = None
    if need_ctx:
        y_c = (yf_c + yb_c[:, ::-1]).reshape(B, Lc, S5_WIDTH) + d * u_c
        out_c = s5_glu(y_c, w_a, w_b, h_c.dtype)
    return out_l, out_c


def dense_swiglu(h, w_gate, w_up, w_down):
    return (jax.nn.silu(h @ w_gate) * (h @ w_up)) @ w_down


def moe_swiglu(h, router, w_gate, w_up, w_down):
    shp = h.shape
    t = h.reshape(-1, D_MODEL)
    logits = (t @ router).astype(F32)
    top_val, top_idx = lax.top_k(logits, TOP_K)
    top_w = jax.nn.softmax(top_val, axis=-1)
    gates = jnp.sum(jax.nn.one_hot(top_idx, N_EXPERTS, dtype=F32) * top_w[..., None], axis=1)
    out = jnp.zeros_like(t)
    for e in range(N_EXPERTS):
        he = jax.nn.silu(t @ w_gate[e]) * (t @ w_up[e])
        out = out + gates[:, e:e + 1].astype(t.dtype) * (he @ w_down[e])
    return out.reshape(shp)


def setup_inputs(seed: int = 0) -> dict:
    key = jax.random.key(seed)
    ks = iter(jax.random.split(key, 64))

    def nrm(shape, scale):
        return jax.random.normal(next(ks), shape, F32) * scale

    def gain(shape):
        return 1.0 + nrm(shape, 0.01)

    g, n = S5_GROUPS, S5_STATE
    n_idx = jnp.arange(S5_STATE, dtype=F32)
    return {
        'x': nrm((BATCH, SEQ, D_MODEL), 1.0),
        'c': nrm((BATCH, D_MODEL), 1.0),
        'ctx': nrm((BATCH, CTX_LEN, D_MODEL), 1.0),
        'c_ctx': nrm((D_MODEL,), 1.0),
        'ada_w': nrm((DEPTH, D_MODEL, 6 * D_MODEL), 0.5 * D_MODEL ** -0.5),
        'ada_b': nrm((DEPTH, 6 * D_MODEL), 0.01),
        'norm_mix_pre': gain((DEPTH, D_MODEL)),
        'norm_mix_post': gain((DEPTH, D_MODEL)),
        'norm_ffn_pre': gain((DEPTH, D_MODEL)),
        'norm_ffn_post': gain((DEPTH, D_MODEL)),
        'ev_w_in': nrm((N_EVEN, D_MODEL, IN_EVEN), D_MODEL ** -0.5),
        'ev_hy_conv_w': nrm((N_EVEN, HY_SHORT, HY_IN), HY_SHORT ** -0.5),
        'ev_hy_conv_b': nrm((N_EVEN, HY_IN), 0.01),
        'ev_hy_f_w1': nrm((N_EVEN, HY_EMB, HY_HIDDEN), HY_EMB ** -0.5),
        'ev_hy_f_b1': nrm((N_EVEN, HY_HIDDEN), 0.01),
        'ev_hy_f_w2': nrm((N_EVEN, HY_HIDDEN, HY_HIDDEN), HY_HIDDEN ** -0.5),
        'ev_hy_f_b2': nrm((N_EVEN, HY_HIDDEN), 0.01),
        'ev_hy_f_wout': nrm((N_EVEN, HY_HIDDEN, 2 * HY_ORDER * HY_WIDTH), 0.02),
        'ev_hy_freq': gain((N_EVEN, HY_HIDDEN)),
        'ev_hy_skip': nrm((N_EVEN, HY_ORDER, HY_WIDTH), 0.5),
        'ev_q_norm': gain((N_EVEN, HEAD_DIM)),
        'ev_k_norm': gain((N_EVEN, HEAD_DIM)),
        'ev_w_out': nrm((N_EVEN, MIX_EVEN, D_MODEL), MIX_EVEN ** -0.5),
        'ev_ffn_w_gate': nrm((N_EVEN, D_MODEL, D_FF), D_MODEL ** -0.5),
        'ev_ffn_w_up': nrm((N_EVEN, D_MODEL, D_FF), D_MODEL ** -0.5),
        'ev_ffn_w_down': nrm((N_EVEN, D_FF, D_MODEL), D_FF ** -0.5),
        'od_w_in': nrm((N_ODD, D_MODEL, S5_WIDTH), D_MODEL ** -0.5),
        'od_s5_lambda_re': -0.5 + nrm((N_ODD, 2, g, n), 0.01),
        'od_s5_lambda_im': math.pi * n_idx + nrm((N_ODD, 2, g, n), 0.01),
        'od_s5_log_step': jax.random.uniform(next(ks), (N_ODD, 2, g), F32,
                                             math.log(S5_DT_MIN), math.log(S5_DT_MAX)),
        'od_s5_b_re': nrm((N_ODD, 2, g, n, S5_GROUP), (2 * S5_GROUP) ** -0.5),
        'od_s5_b_im': nrm((N_ODD, 2, g, n, S5_GROUP), (2 * S5_GROUP) ** -0.5),
        'od_s5_c_re': nrm((N_ODD, 2, g, S5_GROUP, n), S5_STATE ** -0.5),
        'od_s5_c_im': nrm((N_ODD, 2, g, S5_GROUP, n), S5_STATE ** -0.5),
        'od_s5_d': nrm((N_ODD, S5_WIDTH), 0.5),
        'od_glu_w_a': nrm((N_ODD, S5_WIDTH, D_MODEL), S5_WIDTH ** -0.5),
        'od_glu_w_b': nrm((N_ODD, S5_WIDTH, D_MODEL), S5_WIDTH ** -0.5),
        'od_router': nrm((N_ODD, D_MODEL, N_EXPERTS), D_MODEL ** -0.5),
        'od_moe_w_gate': nrm((N_ODD, N_EXPERTS, D_MODEL, D_FF_EXPERT), D_MODEL ** -0.5),
        'od_moe_w_up': nrm((N_ODD, N_EXPERTS, D_MODEL, D_FF_EXPERT), D_MODEL ** -0.5),
        'od_moe_w_down': nrm((N_ODD, N_EXPERTS, D_FF_EXPERT, D_MODEL), D_FF_EXPERT ** -0.5),
    }


def reference(x, c, ctx, c_ctx, ada_w, ada_b, norm_mix_pre, norm_mix_post, norm_ffn_pre, norm_ffn_post,
              ev_w_in, ev_hy_conv_w, ev_hy_conv_b, ev_hy_f_w1, ev_hy_f_b1, ev_hy_f_w2, ev_hy_f_b2,
              ev_hy_f_wout, ev_hy_freq, ev_hy_skip, ev_q_norm, ev_k_norm, ev_w_out,
              ev_ffn_w_gate, ev_ffn_w_up, ev_ffn_w_down,
              od_w_in, od_s5_lambda_re, od_s5_lambda_im, od_s5_log_step, od_s5_b_re, od_s5_b_im,
              od_s5_c_re, od_s5_c_im, od_s5_d, od_glu_w_a, od_glu_w_b,
              od_router, od_moe_w_gate, od_moe_w_up, od_moe_w_down):
    for i in range(DEPTH):
        last = i == DEPTH - 1
        j = i // 2
        sh_m, sc_m, g_m, sh_f, sc_f, g_f = ada_params(c, ada_w[i], ada_b[i])
        cm = ada_params(c_ctx, ada_w[i], ada_b[i])
        h_l = modulate(rms_norm(x, norm_mix_pre[i]), sh_m, sc_m)
        h_c = modulate(rms_norm(ctx, norm_mix_pre[i]), cm[0], cm[1])
        if i % 2 == 0:
            out_l, out_c = even_mixer(h_l, h_c, ev_w_in[j], ev_hy_conv_w[j], ev_hy_conv_b[j],
                                      ev_hy_f_w1[j], ev_hy_f_b1[j], ev_hy_f_w2[j], ev_hy_f_b2[j],
                                      ev_hy_f_wout[j], ev_hy_freq[j], ev_hy_skip[j],
                                      ev_q_norm[j], ev_k_norm[j], ev_w_out[j], not last)
            ffn = lambda h: dense_swiglu(h, ev_ffn_w_gate[j], ev_ffn_w_up[j], ev_ffn_w_down[j])
        else:
            out_l, out_c = odd_mixer(h_l, h_c, od_w_in[j], od_s5_lambda_re[j], od_s5_lambda_im[j],
                                     od_s5_log_step[j], od_s5_b_re[j], od_s5_b_im[j],
                                     od_s5_c_re[j], od_s5_c_im[j], od_s5_d[j],
                                     od_glu_w_a[j], od_glu_w_b[j], not last)
            ffn = lambda h: moe_swiglu(h, od_router[j], od_moe_w_gate[j], od_moe_w_up[j], od_moe_w_down[j])
        x = x + g_m * rms_norm(out_l, norm_mix_post[i])
        hf = modulate(rms_norm(x, norm_ffn_pre[i]), sh_f, sc_f)
        x = x + g_f * rms_norm(ffn(hf), norm_ffn_post[i])
        if not last:
            ctx = ctx + cm[2] * rms_norm(out_c, norm_mix_post[i])
            hf_c = modulate(rms_norm(ctx, norm_ffn_pre[i]), cm[3], cm[4])
            ctx = ctx + cm[5] * rms_norm(ffn(hf_c), norm_ffn_post[i])
    return x
```

```python
import math
from contextlib import ExitStack
import numpy as np
import ml_dtypes
import concourse.bass as bass
import concourse.mybir as mybir
from concourse.bass_utils import run_bass_kernel_spmd

F32 = mybir.dt.float32
BF16 = mybir.dt.bfloat16
I32 = mybir.dt.int32
AF = mybir.ActivationFunctionType
ALU = mybir.AluOpType
AX = mybir.AxisListType
EPS = 1e-6
PI = math.pi
NT = 18
TOK = 2304


def _prod(xs):
    r = 1
    for x in xs:
        r *= int(x)
    return r


class Sched:
    ENG = ['pe', 'act', 'dve', 'pool', 'sp']

    def __init__(self, nc):
        self.nc = nc
        self.ops = {e: [] for e in self.ENG}
        self.seq = {e: 0 for e in self.ENG}
        self.sems = {e: nc.alloc_semaphore("sem_" + e) for e in self.ENG}
        self.dma_sems = {}
        self.dma_cnt = {}
        self.dma_slot = {}
        self.free_slots = []
        self.slot_cls = {}
        self.nslots = 0
        self.waited = {e: {} for e in self.ENG}
        self.recs = {}

    def _region(self, ap):
        t = ap.tensor
        name = ap.name
        pairs = [(int(s), int(c)) for s, c in ap.ap]
        off = int(ap.offset)
        if 'DRAM' in str(ap.space).upper():
            lo = hi = off
            for s, c in pairs:
                if s >= 0:
                    hi += s * (c - 1)
                else:
                    lo += s * (c - 1)
            return name, 0, 1, lo, hi
        rowsize = _prod(list(t.shape)[1:])
        p0 = off // rowsize
        f0 = off % rowsize
        ps, pc = pairs[0]
        if ps == 0:
            pc = 1
        lo = hi = f0
        for s, c in pairs[1:]:
            if s >= 0:
                hi += s * (c - 1)
            else:
                lo += s * (c - 1)
        if 'PSUM' in str(ap.space).upper():
            epb = 2048 // (2 if ap.dtype == BF16 else 4)
            lo = (lo // epb) * epb
            hi = (hi // epb + 1) * epb - 1
            q0 = (p0 // 32) * 32
            q1 = ((p0 + pc + 31) // 32) * 32
            return name, q0, q1, lo, hi
        return name, p0, p0 + pc, lo, hi

    def _deps_and_update(self, eng, tok, reads, writes):
        deps = []
        for ap in reads:
            name, p0, p1, f0, f1 = self._region(ap)
            lst = self.recs.setdefault(name, [])
            is_psum = 'PSUM' in str(ap.space).upper()
            for r in lst:
                if r[0] < p1 and p0 < r[1] and r[2] <= f1 and f0 <= r[3]:
                    if r[4] == 'w':
                        deps.append(r[5])
                    elif is_psum and r[6] != eng:
                        deps.append(r[5])
            found = False
            for i, r in enumerate(lst):
                if r[4] == 'r' and r[6] == eng and r[0] == p0 and r[1] == p1 and r[2] == f0 and r[3] == f1:
                    lst[i] = (p0, p1, f0, f1, 'r', tok, eng)
                    found = True
                    break
            if not found:
                lst.append((p0, p1, f0, f1, 'r', tok, eng))
        for ap in writes:
            name, p0, p1, f0, f1 = self._region(ap)
            lst = self.recs.setdefault(name, [])
            keep = []
            for r in lst:
                ov = r[0] < p1 and p0 < r[1] and r[2] <= f1 and f0 <= r[3]
                if ov:
                    if r[5] == tok:
                        keep.append(r)
                        continue
                    deps.append(r[5])
                    contained = r[0] >= p0 and r[1] <= p1 and r[2] >= f0 and r[3] <= f1
                    if not contained:
                        keep.append(r)
                else:
                    keep.append(r)
            keep.append((p0, p1, f0, f1, 'w', tok, eng))
            self.recs[name] = keep
        return deps

    def _resolve_waits(self, eng, deps):
        waits = []
        for d in deps:
            if d[0] == 'dma':
                key = d[1]
                val = 16 * self.dma_cnt[key]
                sem = self.dma_sems[key]
                wk = ('dma', key)
            else:
                e2, val = d
                if e2 == 'pe' and eng == 'pe':
                    continue
                sem = self.sems[e2]
                wk = e2
            if self.waited[eng].get(wk, 0) >= val:
                continue
            self.waited[eng][wk] = val
            waits.append((sem, val))
        return waits

    def op(self, eng, fn, reads=(), writes=()):
        self.seq[eng] += 1
        tok = (eng, self.seq[eng])
        deps = self._deps_and_update(eng, tok, list(reads), list(writes))
        waits = self._resolve_waits(eng, deps)
        self.ops[eng].append((waits, fn, self.sems[eng], 1))

    def dma(self, eng, out, in_, key=None, **kw):
        if key is None:
            key = out.name if 'DRAM' not in str(out.space).upper() else 'st_' + in_.name
        cls = 'sw' if eng == 'pool' else 'hw'
        key = (key, cls)
        if key not in self.dma_slot:
            fl = [x for x in self.free_slots if self.slot_cls[x] == cls]
            if fl:
                slot = fl[0]
                self.free_slots.remove(slot)
            else:
                slot = self.nslots
                self.nslots += 1
                self.dma_sems[slot] = self.nc.alloc_semaphore("dsem_%d" % slot)
                self.dma_cnt[slot] = 0
                self.slot_cls[slot] = cls
            self.dma_slot[key] = slot
        key = self.dma_slot[key]
        tok = ('dma', key)
        deps = self._deps_and_update(eng, tok, [in_], [out])
        if any(d == tok for d in deps):
            deps = [d for d in deps if d != tok] + [tok]
        waits = self._resolve_waits(eng, deps)
        self.dma_cnt[key] += 1
        fn = (lambda e, out=out, in_=in_, kw=kw: e.dma_start(out=out, in_=in_, **kw))
        self.ops[eng].append((waits, fn, self.dma_sems[key], 16))

    def barrier(self):
        for eng in self.ENG:
            deps = [(e2, self.seq[e2]) for e2 in self.ENG if self.seq[e2] > 0 and e2 != eng]
            deps += [('dma', k) for k in self.dma_sems if self.dma_cnt[k] > 0]
            waits = self._resolve_waits(eng, deps)
            if waits:
                self.ops[eng].append((waits, None, None, 0))
        self.recs = {}
        self.free_slots = sorted(set(self.free_slots) | set(self.dma_slot.values()), reverse=True)
        self.dma_slot = {}

    def emit(self):
        nc = self.nc
        ops = self.ops

        def run(engine, lst):
            for waits, fn, sem, inc in lst:
                for s, v in waits:
                    engine.wait_ge(s, v)
                if fn is not None:
                    ins = fn(engine)
                    ins.then_inc(sem, inc)

        with nc.Block() as block:
            @block.tensor
            def _(e):
                run(e, ops['pe'])

            @block.scalar
            def _(e):
                run(e, ops['act'])

            @block.vector
            def _(e):
                run(e, ops['dve'])

            @block.gpsimd
            def _(e):
                run(e, ops['pool'])

            @block.sync
            def _(e):
                run(e, ops['sp'])


_CONSTS = None


def _bf(a):
    return np.ascontiguousarray(a.astype(np.float32)).astype(ml_dtypes.bfloat16)


def _consts():
    global _CONSTS
    if _CONSTS is not None:
        return _CONSTS
    c = {}
    c['ident'] = np.eye(128, dtype=np.float32)
    c['ones'] = np.ones((128, 128), np.float32)
    par = np.zeros((128, 2), np.float32)
    for p in range(128):
        par[p, (p // 16) % 2] = 1.0
    c['par'] = par
    rm = np.zeros((128, 4), np.float32)
    cm = np.zeros((128, 4, 128), np.float32)
    for m in range(4):
        rm[32 * m:32 * m + 32, m] = 1.0
        cm[:, m, 32 * m:32 * m + 32] = 1.0
    c['rowmask'] = rm
    c['colmask'] = cm
    for nm, L in (('l', 2048), ('c', 256)):
        N = 2 * L
        nt = L // 128
        t = np.arange(L, dtype=np.float64)
        f = np.arange(L, dtype=np.float64) + 0.5
        ang = 2.0 * np.pi * np.outer(t, f) / N
        C = np.cos(ang)
        Sn = np.sin(ang)
        c['dfc_' + nm] = _bf(C.reshape(nt, 128, nt, 128).transpose(2, 1, 0, 3))
        c['dfs_' + nm] = _bf(Sn.reshape(nt, 128, nt, 128).transpose(2, 1, 0, 3))
        sc = 2.0 / N
        c['dic_' + nm] = _bf((sc * C).reshape(nt, 128, nt, 128).transpose(0, 3, 2, 1))
        c['dis_' + nm] = _bf((sc * Sn).reshape(nt, 128, nt, 128).transpose(0, 3, 2, 1))
        tl = np.linspace(0.0, 1.0, L, dtype=np.float32)[:, None]
        bands = np.linspace(1e-4, 15, 16, dtype=np.float32)
        phase = (np.float32(2.0 * math.pi / L) * np.arange(L, dtype=np.float32)[:, None]) * bands
        z = np.concatenate([tl, np.cos(phase), -np.sin(phase)], axis=-1).astype(np.float32)
        c['zT_' + nm] = np.ascontiguousarray(z.T)
        slow = -math.log(1e-2) / 1.5
        fast = -math.log(1e-2) / 0.3
        deltas = np.linspace(slow, fast, 512, dtype=np.float32)
        dec = np.exp(-tl * deltas).astype(np.float32)
        c['dec_' + nm] = np.ascontiguousarray(dec.reshape(nt, 128, 512).transpose(1, 0, 2))
    rows = 2048 // 64
    row = np.repeat(np.arange(rows, dtype=np.float32), 64)
    col = np.tile(np.arange(64, dtype=np.float32), rows)
    inv = (10000.0 ** (-np.arange(16, dtype=np.float32) / 16)).astype(np.float32)
    ang = np.concatenate([row[:, None] * inv, col[:, None] * inv], axis=-1)
    c['ropec'] = np.ascontiguousarray(np.cos(ang).astype(np.float32).reshape(16, 128, 32).transpose(1, 0, 2))
    c['ropes'] = np.ascontiguousarray(np.sin(ang).astype(np.float32).reshape(16, 128, 32).transpose(1, 0, 2))
    posf = np.arange(TOK, dtype=np.float32)
    posb = np.concatenate([255.0 - np.arange(256), 256.0 + 2047.0 - np.arange(2048)]).astype(np.float32)
    c['pos'] = np.ascontiguousarray(np.stack([np.tile(posf, (128, 1)), np.tile(posb, (128, 1))], axis=1))
    _CONSTS = c
    return c


_CONST_DT = {'dfc_l': BF16, 'dfs_l': BF16, 'dic_l': BF16, 'dis_l': BF16,
             'dfc_c': BF16, 'dfs_c': BF16, 'dic_c': BF16, 'dis_c': BF16}

_IN_SHAPES = {
    'x': [2048, 1024], 'c': [1024], 'ctx': [256, 1024], 'c_ctx': [1024],
    'ada_w': [2, 1024, 6144], 'ada_b': [2, 6144],
    'norm_mix_pre': [2, 1024], 'norm_mix_post': [2, 1024], 'norm_ffn_pre': [2, 1024], 'norm_ffn_post': [2, 1024],
    'ev_w_in': [1024, 2304], 'ev_hy_conv_w': [3, 1536], 'ev_hy_conv_b': [1536],
    'ev_hy_f_w1': [33, 64], 'ev_hy_f_b1': [64], 'ev_hy_f_w2': [64, 64], 'ev_hy_f_b2': [64],
    'ev_hy_f_wout': [64, 2048], 'ev_hy_freq': [64], 'ev_hy_skip': [1024],
    'ev_q_norm': [64], 'ev_k_norm': [64], 'ev_w_out': [1024, 1024],
    'ev_ffn_w_gate': [1, 1024, 2816], 'ev_ffn_w_up': [1, 1024, 2816], 'ev_ffn_w_down': [1, 2816, 1024],
    'od_w_in': [1024, 1024], 'od_s5_lambda_re': [2, 32, 128], 'od_s5_lambda_im': [2, 32, 128],
    'od_s5_log_step': [2, 32, 2], 'od_s5_b_re': [2, 64, 64, 16], 'od_s5_b_im': [2, 64, 64, 16],
    'od_s5_c_re': [2, 1024, 64], 'od_s5_c_im': [2, 1024, 64], 'od_s5_d': [1024],
    'od_glu_w_a': [1024, 1024], 'od_glu_w_b': [1024, 1024], 'od_router': [1024, 8],
    'od_moe_w_gate': [8, 1024, 3584], 'od_moe_w_up': [8, 1024, 3584], 'od_moe_w_down': [8, 3584, 1024],
}


def build(upto=99):
    nc = bass.Bass("TRN2", target_bir_lowering=False)
    S = Sched(nc)
    D = {}
    for k, shp in _IN_SHAPES.items():
        D[k] = nc.dram_tensor(k, list(shp), F32, kind="ExternalInput").ap()
    for k, v in _consts().items():
        D[k] = nc.dram_tensor(k, list(v.shape), _CONST_DT.get(k, F32), kind="ExternalInput").ap()
    xs_out = nc.dram_tensor("xs", [TOK, 1024], F32, kind="ExternalOutput").ap()
    xs = nc.dram_tensor("xs_scr", [TOK, 1024], F32, kind="Internal").ap()
    kscr = {nm: nc.dram_tensor("kscr_" + nm, [2, 2, L, 512], BF16, kind="Internal").ap()
            for nm, L in (('l', 2048), ('c', 256))}

    uid = [0]

    class Pool:
        def __init__(self):
            self.st = ExitStack()

        def sb(self, name, shape, dt=F32):
            uid[0] += 1
            t = self.st.enter_context(nc.sbuf_tensor("%s_%d" % (name, uid[0]), list(shape), dt))
            return t.ap()

        def ps(self, name, shape, dt=F32):
            uid[0] += 1
            epb = 2048 // (2 if dt == BF16 else 4)
            shape = [shape[0], ((shape[1] + epb - 1) // epb) * epb]
            t = self.st.enter_context(nc.psum_tensor("%s_%d" % (name, uid[0]), list(shape), dt))
            return t.ap()

        def close(self):
            S.barrier()
            self.st.close()

    def mm(out, lhsT, rhs, start=True, stop=True):
        S.op('pe', lambda e: e.matmul(out, lhsT=lhsT, rhs=rhs, start=start, stop=stop), reads=[lhsT, rhs], writes=[out])

    def tr(out, in_, ident):
        S.op('pe', lambda e: e.transpose(out, in_, ident), reads=[in_, ident], writes=[out])

    def _isap(x):
        return not isinstance(x, (int, float)) and x is not None

    def act(out, in_, func, bias=None, scale=None, accum=None):
        kw = {}
        rd = [in_]
        if bias is not None:
            kw['bias'] = bias
            if _isap(bias):
                rd.append(bias)
        if scale is not None:
            kw['scale'] = scale
            if _isap(scale):
                rd.append(scale)
        wr = [out]
        if accum is not None:
            kw['accum_out'] = accum
            wr.append(accum)
        S.op('act', lambda e: e.activation(out=out, in_=in_, func=func, **kw), reads=rd, writes=wr)

    def ts(out, in0, s1, s2=None, op0=ALU.mult, op1=None, eng='dve'):
        rd = [in0] + [s for s in (s1, s2) if _isap(s)]
        if op1 is None:
            S.op(eng, lambda e: e.tensor_scalar(out=out, in0=in0, scalar1=s1, scalar2=None, op0=op0), reads=rd, writes=[out])
        else:
            S.op(eng, lambda e: e.tensor_scalar(out=out, in0=in0, scalar1=s1, scalar2=s2, op0=op0, op1=op1), reads=rd, writes=[out])

    def tt(out, in0, in1, op, eng='dve'):
        S.op(eng, lambda e: e.tensor_tensor(out=out, in0=in0, in1=in1, op=op), reads=[in0, in1], writes=[out])

    def stt(out, in0, scalar, in1, op0, op1):
        rd = [in0, in1] + ([scalar] if _isap(scalar) else [])
        S.op('dve', lambda e: e.scalar_tensor_tensor(out=out, in0=in0, scalar=scalar, in1=in1, op0=op0, op1=op1), reads=rd, writes=[out])

    def cp(out, in_, eng='dve'):
        if eng == 'act':
            S.op('act', lambda e: e.copy(out=out, in_=in_), reads=[in_], writes=[out])
        else:
            S.op(eng, lambda e: e.tensor_copy(out=out, in_=in_), reads=[in_], writes=[out])

    def recip(out, in_):
        S.op('dve', lambda e: e.reciprocal(out=out, in_=in_), reads=[in_], writes=[out])

    def memset(ap, val, eng='pool'):
        S.op(eng, lambda e: e.memset(ap, val), writes=[ap])

    def dma(out, in_, eng='sp', **kw):
        S.dma(eng, out, in_, **kw)

    def v3(ap, b):
        return ap.rearrange("p (a b) -> p a b", b=b)

    def bc_last(ap, n):
        return ap.unsqueeze(2).to_broadcast([ap.shape[0], ap.shape[1], n])

    def bc_mid(ap, n):
        return ap.unsqueeze(1).to_broadcast([ap.shape[0], n, ap.shape[1]])

    G = Pool()
    ident = G.sb("ident", [128, 128])
    identb = G.sb("identb", [128, 128], BF16)
    ones = G.sb("ones", [128, 128])
    par = G.sb("par", [128, 2])
    colsA = G.sb("colsA", [128, 112])
    colsB = G.sb("colsB", [128, 120])
    coef = [G.sb("coef%d" % i, [128, 6, 2, 8]) for i in range(2)]
    dma(ident, D['ident'])
    dma(ones, D['ones'])
    dma(par, D['par'])
    cp(identb, ident)
    dma(xs[0:256, :], D['ctx'])
    dma(xs[256:TOK, :], D['x'])

    def phase_filters():
        P = Pool()
        w1 = P.sb("fw1", [33, 64]); w2 = P.sb("fw2", [64, 64]); wout = P.sb("fwout", [64, 2048])
        cols = P.sb("fcols", [64, 3]); frb = P.sb("ffrb", [64, 2])
        dma(w1, D['ev_hy_f_w1']); dma(w2, D['ev_hy_f_w2']); dma(wout, D['ev_hy_f_wout'])
        for i, k in enumerate(('ev_hy_f_b1', 'ev_hy_f_b2', 'ev_hy_freq')):
            dma(cols[:, i:i + 1], D[k].rearrange("(p o) -> p o", o=1))
        tt(frb[:, 0:1], cols[:, 0:1], cols[:, 2:3], ALU.mult)
        tt(frb[:, 1:2], cols[:, 1:2], cols[:, 2:3], ALU.mult)
        psm = [P.ps("fps%d" % i, [128, 512]) for i in range(4)]
        for nm, L in (('l', 2048), ('c', 256)):
            nt = L // 128
            zT = P.sb("fzT", [33, L]); h1 = P.sb("fh1", [64, L]); h2 = P.sb("fh2", [64, L])
            tmp = [P.sb("ftmp%d" % i, [64, 512]) for i in range(2)]
            tki = P.sb("ftki", [64, 512], I32); tkf = P.sb("ftkf", [64, 512])
            dec = P.sb("fdec", [128, nt, 512])
            dma(zT, D['zT_' + nm]); dma(dec, D['dec_' + nm])
            for (wm, src, dst, bcol) in ((w1, zT, h1, 0), (w2, h1, h2, 1)):
                for bi, b0 in enumerate(range(0, L, 512)):
                    n = min(512, L - b0)
                    ps = psm[bi % 2]
                    mm(ps[0:64, 0:n], wm, src[:, b0:b0 + n])
                    t_ = tmp[bi % 2]
                    ts(t_[:, 0:n], ps[0:64, 0:n], cols[:, 2:3], frb[:, bcol:bcol + 1], ALU.mult, ALU.add)
                    ts(tki[:, 0:n], t_[:, 0:n], 1.0 / (2 * PI), None, ALU.mult)
                    cp(tkf[:, 0:n], tki[:, 0:n])
                    stt(t_[:, 0:n], tkf[:, 0:n], -2 * PI, t_[:, 0:n], ALU.mult, ALU.add)
                    ts(t_[:, 0:n], t_[:, 0:n], PI, -PI, ALU.min, ALU.max)
                    act(dst[:, b0:b0 + n], t_[:, 0:n], AF.Sin)
            tf = [P.sb("ftf%d" % i, [128, 512]) for i in range(2)]
            tb = [P.sb("ftb%d" % i, [128, 512]) for i in range(2)]
            dft = [[P.sb("fdft%d%d" % (i, j), [128, nt, 128], BF16) for j in range(2)] for i in range(2)]
            kst = [[P.sb("fkst%d%d" % (i, j), [128, 512], BF16) for j in range(2)] for i in range(2)]
            for o in range(2):
                hs = P.sb("fhs", [128, nt, 512], BF16); hd = P.sb("fhd", [128, nt, 512], BF16)
                for t_i in range(nt):
                    pa = psm[0 + (t_i % 2) * 2]; pb = psm[1 + (t_i % 2) * 2]
                    mm(pa, h2[:, t_i * 128:(t_i + 1) * 128], wout[:, o * 512:(o + 1) * 512])
                    mm(pb, h2[:, t_i * 128:(t_i + 1) * 128], wout[:, 1024 + o * 512:1024 + (o + 1) * 512])
                    a = tf[t_i % 2]; b = tb[t_i % 2]
                    tt(a, pa, dec[:, t_i, :], ALU.mult)
                    tt(b, pb, dec[:, t_i, :], ALU.mult)
                    if t_i == 0:
                        memset(b[0:1, :], 0.0, eng='dve')
                    tt(hs[:, t_i, :], a, b, ALU.add, eng='pool')
                    tt(hd[:, t_i, :], a, b, ALU.subtract, eng='pool')
                for fc in range(nt):
                    cf = dft[fc % 2][0]; sf = dft[fc % 2][1]
                    dma(cf, D['dfc_' + nm][fc]); dma(sf, D['dfs_' + nm][fc])
                    pr = psm[(fc % 2) * 2]; pi_ = psm[(fc % 2) * 2 + 1]
                    for tc in range(nt):
                        mm(pr, cf[:, tc, :], hs[:, tc, :], start=(tc == 0), stop=(tc == nt - 1))
                    for tc in range(nt):
                        mm(pi_, sf[:, tc, :], hd[:, tc, :], start=(tc == 0), stop=(tc == nt - 1))
                    kr = kst[fc % 2][0]; ki = kst[fc % 2][1]
                    cp(kr, pr, eng='dve'); cp(ki, pi_, eng='act')
                    dma(kscr[nm][o, 0, fc * 128:(fc + 1) * 128, :], kr, key='kst')
                    dma(kscr[nm][o, 1, fc * 128:(fc + 1) * 128, :], ki, key='kst')
                S.barrier()
        P.close()

    def phase_ada():
        P = Pool()
        vecA = P.sb("vecA", [112, 128]); vecB = P.sb("vecB", [120, 128])
        dma(vecA[0:8, :], D['c'].rearrange("(r p) -> r p", p=128))
        dma(vecA[8:16, :], D['c_ctx'].rearrange("(r p) -> r p", p=128))
        for i in range(2):
            dma(vecA[16 + 48 * i:64 + 48 * i, :], D['ada_b'][i].rearrange("(r p) -> r p", p=128))
        r0 = 0
        for k in ('norm_mix_pre', 'norm_mix_post', 'norm_ffn_pre', 'norm_ffn_post'):
            dma(vecB[r0:r0 + 16, :], D[k].rearrange("i (r p) -> (i r) p", p=128))
            r0 += 16
        dma(vecB[64:100, :], D['ev_hy_conv_w'].rearrange("k (r p) -> (k r) p", p=128))
        dma(vecB[100:112, :], D['ev_hy_conv_b'].rearrange("(r p) -> r p", p=128))
        dma(vecB[112:120, :], D['od_s5_d'].rearrange("(r p) -> r p", p=128))
        pt = P.ps("apt", [128, 512])
        tr(pt[:, 0:112], vecA, ident[0:112, 0:112])
        cp(colsA, pt[:, 0:112])
        pt2 = P.ps("apt2", [128, 512])
        tr(pt2[:, 0:120], vecB, ident[0:120, 0:120])
        cp(colsB, pt2[:, 0:120])
        sc2 = P.sb("sc2", [128, 8, 2])
        act(sc2[:, :, 0], colsA[:, 0:8], AF.Silu)
        act(sc2[:, :, 1], colsA[:, 8:16], AF.Silu)
        aw = [P.sb("aw%d" % i, [128, 8, 512]) for i in range(2)]
        pm = [P.ps("apm%d" % i, [128, 96]) for i in range(2)]
        mod = P.sb("mod", [128, 48, 2])
        for i in range(2):
            for nb in range(12):
                a = aw[nb % 2]
                dma(a, D['ada_w'][i][:, nb * 512:(nb + 1) * 512].rearrange("(kc p) n -> p kc n", p=128))
                for q in range(4):
                    m = nb * 4 + q
                    for kc in range(8):
                        mm(pm[i][:, m * 2:m * 2 + 2], a[:, kc, q * 128:(q + 1) * 128], sc2[:, kc, :],
                           start=(kc == 0), stop=(kc == 7))
            tt(mod, v3(pm[i][:, 0:96], 2), bc_last(colsA[:, 16 + 48 * i:64 + 48 * i], 2), ALU.add)
            nmp = colsB[:, 0 + 8 * i:8 + 8 * i]; nmpost = colsB[:, 16 + 8 * i:24 + 8 * i]
            nfp = colsB[:, 32 + 8 * i:40 + 8 * i]; nfpost = colsB[:, 48 + 8 * i:56 + 8 * i]
            for s in range(2):
                stt(coef[i][:, 0, s, :], mod[:, 8:16, s], 1.0, nmp, ALU.add, ALU.mult)
                cp(coef[i][:, 1, s, :], mod[:, 0:8, s])
                tt(coef[i][:, 2, s, :], mod[:, 16:24, s], nmpost, ALU.mult)
                stt(coef[i][:, 3, s, :], mod[:, 32:40, s], 1.0, nfp, ALU.add, ALU.mult)
                cp(coef[i][:, 4, s, :], mod[:, 24:32, s])
                tt(coef[i][:, 5, s, :], mod[:, 40:48, s], nfpost, ALU.mult)
        P.close()

    def make_bc(P, dst, col, pp):
        dg = [P.sb("dg%d" % i, [128, 128]) for i in range(2)]
        for j in range(8):
            d = dg[j % 2]
            ts(d, ident, col[:, j:j + 1], None, ALU.mult)
            mm(pp[:, j * 128:(j + 1) * 128], ones, d)
        cp(dst, pp)

    def norm_to_hT(P, i, kind, hT, tiles, pp, extra=None):
        xin = [P.sb("nx%d" % k, [128, 1024]) for k in range(2)]
        xn = [P.sb("nxn%d" % k, [128, 1024]) for k in range(2)]
        junk = P.sb("njunk", [128, 1024])
        st = P.sb("nst", [128, 3 * NT])
        ka = 0 if kind == 'mix' else 3
        DBG = 9
        for idx, n in enumerate(tiles):
            s = 1 if n < 2 else 0
            xt = xin[idx % 2]; xo = xn[idx % 2]; p2 = pp[idx % 2]
            dma(xt, xs[n * 128:(n + 1) * 128, :])
            if DBG < 1:
                continue
            act(junk, xt, AF.Square, accum=st[:, n:n + 1])
            act(st[:, NT + n:NT + n + 1], st[:, n:n + 1], AF.Sqrt, bias=EPS, scale=1.0 / 1024)
            recip(st[:, 2 * NT + n:2 * NT + n + 1], st[:, NT + n:NT + n + 1])
            act(xo, xt, AF.Identity, scale=st[:, 2 * NT + n:2 * NT + n + 1])
            if DBG < 2:
                continue
            for j in range(8):
                tr(p2[:, j * 128:(j + 1) * 128], xo[:, j * 128:(j + 1) * 128], ident)
            if DBG < 4:
                continue
            for j in range(8):
                A = coef[i][:, ka, s, j:j + 1]; B = coef[i][:, ka + 1, s, j:j + 1]
                o = hT[:, j, idx * 128:(idx + 1) * 128]
                if j < 4:
                    ts(o, p2[:, j * 128:(j + 1) * 128], A, B, ALU.mult, ALU.add)
                else:
                    act(o, p2[:, j * 128:(j + 1) * 128], AF.Identity, bias=B, scale=A)
            if extra is not None:
                extra(idx, n, p2, coef[i][:, ka, s, :], coef[i][:, ka + 1, s, :])

    def residual_update(P, bufs, idx, n, src, Gbc):
        xt, tmp, st, junk = bufs
        xt = xt[idx % 2]; tmp = tmp[idx % 2]
        c0 = (idx % 2) * 3
        dma(xt, xs[n * 128:(n + 1) * 128, :])
        act(junk, src, AF.Square, accum=st[:, c0:c0 + 1])
        if upto < 2.82:
            return
        act(st[:, c0 + 1:c0 + 2], st[:, c0:c0 + 1], AF.Sqrt, bias=EPS, scale=1.0 / 1024)
        recip(st[:, c0 + 2:c0 + 3], st[:, c0 + 1:c0 + 2])
        if upto < 2.83:
            return
        stt(tmp, src, st[:, c0 + 2:c0 + 3], Gbc, ALU.mult, ALU.mult)
        if upto < 2.84:
            return
        tt(tmp, tmp, xt, ALU.add, eng='pool')
        if upto < 2.85:
            return
        dma(xs[n * 128:(n + 1) * 128, :], tmp)

    def res_bufs(P):
        return ([P.sb("rx%d" % k, [128, 1024]) for k in range(2)], [P.sb("rt%d" % k, [128, 1024]) for k in range(2)],
                P.sb("rst", [128, 6]), P.sb("rjunk", [128, 1024]))

    def phase_even_mixer():
        P = Pool()
        z_tok = P.sb("z_tok", [128, NT, 1536], BF16)
        y_at = P.sb("y_at", [128, NT, 512], BF16)
        PH = Pool()
        hT = PH.sb("hT", [128, 8, TOK], BF16)
        P3 = Pool()
        pp = [P3.ps("pp%d" % k, [128, 1024]) for k in range(2)]
        norm_to_hT(P3, 0, 'mix', hT, list(range(NT)), pp)
        P3.close()
        if upto < 2.2:
            return
        PQ = Pool()
        QT = PQ.sb("QT", [64, 8, TOK], BF16)
        KT = PQ.sb("KT", [64, 2, TOK], BF16)
        Va = PQ.sb("Va", [128, NT, 2, 65], BF16)
        memset(Va, 1.0)
        P3 = Pool()
        W = P3.sb("Wqkv", [128, 8, 768], BF16)
        for kc in range(8):
            dma(W[:, kc, :], D['ev_w_in'][kc * 128:(kc + 1) * 128, 1536:2304], eng='pool')
        pp = [P3.ps("pp%d" % k, [128, 1024]) for k in range(2)]
        ptb = [P3.ps("ptb%d" % k, [128, 1024], BF16) for k in range(2)]
        gq = P3.sb("gq", [128, 64]); gk = P3.sb("gk", [128, 64])
        dma(gq, D['ev_q_norm'].partition_broadcast(128)); dma(gk, D['ev_k_norm'].partition_broadcast(128))
        qkg = P3.sb("qkg", [128, 10, 64])
        cp(qkg[:, 0:8, :], bc_mid(gq, 8)); cp(qkg[:, 8:10, :], bc_mid(gk, 2))
        ropec = P3.sb("ropec", [128, 16, 32]); ropes = P3.sb("ropes", [128, 16, 32])
        dma(ropec, D['ropec']); dma(ropes, D['ropes'])
        sq = P3.sb("sq", [128, 640]); sst = P3.sb("sst", [128, 30])
        qn = P3.sb("qn", [128, 10, 64]); qr = [P3.sb("qr%d" % k, [128, 10, 64], BF16) for k in range(2)]
        rt = [P3.sb("rt%d" % k, [128, 10, 32]) for k in range(4)]
        for n in range(NT):
            pq = pp[n % 2]
            for kc in range(8):
                mm(pq[:, 0:512], hT[:, kc, n * 128:(n + 1) * 128], W[:, kc, 0:512], start=(kc == 0), stop=(kc == 7))
            for kc in range(8):
                mm(pq[:, 512:768], hT[:, kc, n * 128:(n + 1) * 128], W[:, kc, 512:768], start=(kc == 0), stop=(kc == 7))
            act(sq, pq[:, 0:640], AF.Square)
            S.op('dve', lambda e, o=sst[:, 0:10], i_=v3(sq, 64): e.tensor_reduce(out=o, in_=i_, axis=AX.X, op=ALU.add),
                 reads=[sq], writes=[sst[:, 0:10]])
            act(sst[:, 10:20], sst[:, 0:10], AF.Sqrt, bias=EPS, scale=1.0 / 64)
            recip(sst[:, 20:30], sst[:, 10:20])
            tt(qn, v3(pq[:, 0:640], 64), bc_last(sst[:, 20:30], 64), ALU.mult)
            q_ = qr[n % 2]
            if n >= 2:
                tt(qn, qn, qkg, ALU.mult)
                cc = bc_mid(ropec[:, n - 2, :], 10); ss_ = bc_mid(ropes[:, n - 2, :], 10)
                x1 = qn[:, :, 0:32]; x2 = qn[:, :, 32:64]
                tt(rt[0], x1, cc, ALU.mult); tt(rt[1], x2, ss_, ALU.mult)
                tt(q_[:, :, 0:32], rt[0], rt[1], ALU.subtract, eng='pool')
                tt(rt[2], x1, ss_, ALU.mult); tt(rt[3], x2, cc, ALU.mult)
                tt(q_[:, :, 32:64], rt[2], rt[3], ALU.add, eng='pool')
            else:
                tt(q_, qn, qkg, ALU.mult)
            cp(Va[:, n, :, 0:64], v3(pq[:, 640:768], 64), eng='act')
            pt0 = ptb[0]; pt1 = ptb[1]
            for h in range(8):
                tr(pt0[0:64, h * 128:(h + 1) * 128], q_[:, h, :], identb)
            for h in range(2):
                tr(pt1[0:64, h * 128:(h + 1) * 128], q_[:, 8 + h, :], identb)
            cp(QT[:, :, n * 128:(n + 1) * 128], v3(pt0[0:64, :], 128), eng='act')
            cp(KT[:, :, n * 128:(n + 1) * 128], v3(pt1[0:64, 0:256], 128), eng='dve')
        P3.close()
        if upto < 2.3:
            return
        P3 = Pool()
        psc = [P3.ps("psc%d" % k, [128, 512]) for k in range(4)]
        po = [P3.ps("po%d" % k, [128, 512]) for k in range(2)]
        PT = [P3.sb("PT%d" % k, [128, NT, 512], BF16) for k in range(2)]
        rc = P3.sb("rc", [128, 8])
        it = 0
        for h in range(8):
            g = h // 4
            jobs = [(0, 256, [0, 1], 0)] + [(256 + qb * 512, 512, list(range(NT)), 2 + qb * 4) for qb in range(4)]
            for (q0, nq, kcs, tile0) in jobs:
                pt_ = PT[it % 2]
                for kc in kcs:
                    ps = psc[kc % 4]
                    mm(ps[:, 0:nq], KT[:, g, kc * 128:(kc + 1) * 128], QT[:, h, q0:q0 + nq])
                    act(pt_[:, kc, 0:nq], ps[:, 0:nq], AF.Exp, scale=0.125)
                pov = po[it % 2]
                nqt = nq // 128
                for qt in range(nqt):
                    for ki, kc in enumerate(kcs):
                        mm(pov[:, qt * 65:(qt + 1) * 65], pt_[:, kc, qt * 128:(qt + 1) * 128], Va[:, kc, g, :],
                           start=(ki == 0), stop=(ki == len(kcs) - 1))
                pv = v3(pov[:, 0:nqt * 65], 65)
                r_ = rc[:, (it % 2) * 4:(it % 2) * 4 + nqt]
                recip(r_, pv[:, :, 64])
                tt(y_at[:, tile0:tile0 + nqt, h * 64:(h + 1) * 64], pv[:, :, 0:64], bc_last(r_, 64), ALU.mult)
                it += 1
        P3.close()
        PQ.close()
        if upto < 2.4:
            return
        P3 = Pool()
        W = P3.sb("Why", [128, 8, 1536], BF16)
        for kc in range(8):
            dma(W[:, kc, :], D['ev_w_in'][kc * 128:(kc + 1) * 128, 0:1536], eng='pool')
        pa = [P3.ps("pa%d" % k, [128, 512]) for k in range(2)]
        ptb = [P3.ps("ptb%d" % k, [128, 1024], BF16) for k in range(2)]
        pc_c = [P3.sb("pc_c%d" % k, [128, 258]) for k in range(2)]
        pc_l = [P3.sb("pc_l%d" % k, [128, 2050]) for k in range(2)]
        for k in range(2):
            memset(pc_c[k], 0.0); memset(pc_l[k], 0.0)
        zf = P3.sb("zf", [128, TOK])
        zc = [P3.sb("zc%d" % k, [128, TOK], BF16) for k in range(2)]
        blocks = [(0, 256), (256, 512), (768, 512), (1280, 512), (1792, 512)]
        for c in range(12):
            pcc = pc_c[c % 2]; pcl = pc_l[c % 2]
            for bi, (t0, n) in enumerate(blocks):
                ps = pa[bi % 2]
                for kc in range(8):
                    mm(ps[:, 0:n], W[:, kc, c * 128:(c + 1) * 128], hT[:, kc, t0:t0 + n], start=(kc == 0), stop=(kc == 7))
                dst = pcc[:, 1:257] if t0 == 0 else pcl[:, 1 + t0 - 256:1 + t0 - 256 + n]
                cp(dst, ps[:, 0:n], eng='act')
            w0 = colsB[:, 64 + c:65 + c]; w1c = colsB[:, 76 + c:77 + c]; w2c = colsB[:, 88 + c:89 + c]
            bcl = colsB[:, 100 + c:101 + c]
            zcc = zc[c % 2]
            for (pc, L, off) in ((pcc, 256, 0), (pcl, 2048, 256)):
                ts(zf[:, off:off + L], pc[:, 1:L + 1], w1c, bcl, ALU.mult, ALU.add)
                stt(zf[:, off:off + L], pc[:, 0:L], w0, zf[:, off:off + L], ALU.mult, ALU.add)
                stt(zcc[:, off:off + L], pc[:, 2:L + 2], w2c, zf[:, off:off + L], ALU.mult, ALU.add)
            for gi, n0 in enumerate((0, 8, 16)):
                cnt = min(8, NT - n0)
                pt = ptb[gi % 2]
                for k in range(cnt):
                    tr(pt[:, k * 128:(k + 1) * 128], zcc[:, (n0 + k) * 128:(n0 + k + 1) * 128], identb)
                cp(z_tok[:, n0:n0 + cnt, c * 128:(c + 1) * 128], v3(pt[:, 0:cnt * 128], 128), eng=('act' if gi % 2 else 'dve'))
        P3.close()
        PH.close()
        if upto < 2.5:
            return
        P3 = Pool()
        skip = P3.sb("skip", [128, 1024])
        dma(skip, D['ev_hy_skip'].partition_broadcast(128))
        psm = [P3.ps("hps%d" % k, [128, 512]) for k in range(6)]
        for nm, L, tile0 in (('l', 2048, 2), ('c', 256, 0)):
            nt = L // 128
            Yr = P3.sb("Yr_" + nm, [128, nt, 512], BF16); Yi = P3.sb("Yi_" + nm, [128, nt, 512], BF16)
            v1 = P3.sb("v1_" + nm, [128, nt, 512], BF16)
            dft = [[P3.sb("hdft%s%d%d" % (nm, i, j), [128, nt, 128], BF16) for j in range(2)] for i in range(2)]
            tm = [P3.sb("htm%s%d" % (nm, i), [128, 512]) for i in range(6)]
            for o in range(2):
                vsrc = (lambda t_: z_tok[:, tile0 + t_, 0:512]) if o == 0 else (lambda t_: v1[:, t_, :])
                vdst = (lambda t_: v1[:, t_, :]) if o == 0 else (lambda t_: z_tok[:, tile0 + t_, 0:512])
                dma(Yr, kscr[nm][o, 0].rearrange("(f p) c -> p f c", p=128))
                dma(Yi, kscr[nm][o, 1].rearrange("(f p) c -> p f c", p=128))
                for fc in range(nt):
                    cf = dft[fc % 2][0]; sf = dft[fc % 2][1]
                    dma(cf, D['dfc_' + nm][fc]); dma(sf, D['dfs_' + nm][fc])
                    pr = psm[(fc % 2) * 2]; pi_ = psm[(fc % 2) * 2 + 1]
                    for tc in range(nt):
                        mm(pr, cf[:, tc, :], vsrc(tc), start=(tc == 0), stop=(tc == nt - 1))
                    for tc in range(nt):
                        mm(pi_, sf[:, tc, :], vsrc(tc), start=(tc == 0), stop=(tc == nt - 1))
                    tt(tm[0], pr, Yr[:, fc, :], ALU.mult); tt(tm[1], pi_, Yi[:, fc, :], ALU.mult)
                    tt(tm[2], pr, Yi[:, fc, :], ALU.mult); tt(tm[3], pi_, Yr[:, fc, :], ALU.mult)
                    tt(Yr[:, fc, :], tm[0], tm[1], ALU.subtract, eng='pool')
                    tt(Yi[:, fc, :], tm[2], tm[3], ALU.add, eng='pool')
                for t_i in range(nt):
                    ci = dft[t_i % 2][0]; si = dft[t_i % 2][1]
                    dma(ci, D['dic_' + nm][t_i]); dma(si, D['dis_' + nm][t_i])
                    py = psm[4 + t_i % 2]
                    for fc in range(nt):
                        mm(py, ci[:, fc, :], Yr[:, fc, :], start=(fc == 0), stop=False)
                    for fc in range(nt):
                        mm(py, si[:, fc, :], Yi[:, fc, :], start=False, stop=(fc == nt - 1))
                    a = tm[4 + t_i % 2]
                    tt(a, vsrc(t_i), skip[:, o * 512:(o + 1) * 512], ALU.mult)
                    tt(a, a, py, ALU.add)
                    tt(vdst(t_i), a, z_tok[:, tile0 + t_i, (o + 1) * 512:(o + 2) * 512], ALU.mult)
        P3.close()
        if upto < 2.6:
            return
        P3 = Pool()
        Wo = P3.sb("Wo", [128, 8, 1024], BF16)
        for kc in range(8):
            dma(Wo[:, kc, :], D['ev_w_out'][kc * 128:(kc + 1) * 128, :], eng='pool')
        pp = [P3.ps("opp%d" % k, [128, 1024]) for k in range(2)]
        ptb = [P3.ps("optb%d" % k, [128, 1024], BF16) for k in range(2)]
        Gbc = [P3.sb("Gbc%d" % s, [128, 1024]) for s in range(2)]
        for s in range(2):
            make_bc(P3, Gbc[s], coef[0][:, 2, s, :], pp[s])
        mixT = [P3.sb("mixT%d" % k, [128, 8, 128], BF16) for k in range(2)]
        rb = res_bufs(P3)
        if upto < 2.7:
            return
        for n in range(NT):
            pt = ptb[n % 2]; mt = mixT[n % 2]
            for j in range(4):
                tr(pt[:, j * 128:(j + 1) * 128], z_tok[:, n, j * 128:(j + 1) * 128], identb)
            for j in range(4):
                tr(pt[:, (4 + j) * 128:(5 + j) * 128], y_at[:, n, j * 128:(j + 1) * 128], identb)
            cp(mt, v3(pt, 128), eng='act')
            po_ = pp[n % 2]
            for hf in range(2):
                for j in range(8):
                    mm(po_[:, hf * 512:(hf + 1) * 512], mt[:, j, :], Wo[:, j, hf * 512:(hf + 1) * 512], start=(j == 0), stop=(j == 7))
            if upto >= 2.8:
                residual_update(P3, rb, n, n, po_, Gbc[1 if n < 2 else 0])
        P3.close()
        P.close()

    def phase_ffn(i, tiles, experts, dff, G_, router=None):
        P = Pool()
        ntl = len(tiles)
        ntok = ntl * 128
        hT = P.sb("fhT", [128, 8, ntok], BF16)
        acc = P.sb("facc", [128, ntl, 1024])
        gates = None
        P2 = Pool()
        pp = [P2.ps("fpp%d" % k, [128, 1024]) for k in range(2)]
        extra = None
        if router is not None:
            gates = P.sb("gates", [128, ntl, 8])
            rw = P2.sb("rw", [128, 8, 8])
            dma(rw, router.rearrange("(kc p) e -> p kc e", p=128))
            h32 = [P2.sb("h32_%d" % k, [128, 8, 128]) for k in range(2)]
            pl = [P2.ps("fpl%d" % k, [128, 8]) for k in range(2)]
            gs = P2.sb("gs", [128, 48])

            def extra(idx, n, p2, Acol, Bcol):
                h = h32[idx % 2]
                for j in range(8):
                    ts(h[:, j, :], p2[:, j * 128:(j + 1) * 128], Acol[:, j:j + 1], Bcol[:, j:j + 1], ALU.mult, ALU.add)
                plg = pl[idx % 2][:, 0:8]
                for j in range(8):
                    mm(plg, h[:, j, :], rw[:, j, :], start=(j == 0), stop=(j == 7))
                lg = gs[:, 0:8]; m8 = gs[:, 8:16]; ex = gs[:, 16:24]; mk = gs[:, 24:32]
                nm1 = gs[:, 32:33]; e2 = gs[:, 33:34]; rd = gs[:, 34:35]
                cp(lg, plg)
                S.op('dve', lambda e: e.max(out=m8, in_=lg), reads=[lg], writes=[m8])
                ts(nm1, m8[:, 0:1], -1.0, None, ALU.mult)
                act(ex, lg, AF.Exp, bias=nm1)
                act(e2, m8[:, 1:2], AF.Exp, bias=nm1)
                ts(mk, lg, m8[:, 1:2], None, ALU.is_ge)
                ts(e2, e2, 1.0, None, ALU.add)
                recip(rd, e2)
                tt(ex, ex, mk, ALU.mult)
                ts(gates[:, idx, :], ex, rd, None, ALU.mult)
        norm_to_hT(P2, i, 'ffn', hT, tiles, pp, extra=extra)
        P2.close()
        P2 = Pool()
        pg = [P2.ps("fpg%d" % k, [128, 512]) for k in range(2)]
        pu = [P2.ps("fpu%d" % k, [128, 512]) for k in range(2)]
        pd = [P2.ps("fpd%d" % k, [128, 1024]) for k in range(2)]
        groups = G_ if isinstance(G_, (list, tuple)) else [G_] * (dff // (G_ * 128))
        GM = max(groups)
        he = P2.sb("he", [128, GM, ntok], BF16)
        sg = [P2.sb("sg%d" % k, [128, 512]) for k in range(2)]
        Wg = [P2.sb("Wg%d" % k, [128, 8, GM * 128], BF16) for k in range(2)]
        Wu = [P2.sb("Wu%d" % k, [128, 8, GM * 128], BF16) for k in range(2)]
        Wd = [P2.sb("Wd%d" % k, [128, GM, 1024], BF16) for k in range(2)]
        blocks = []
        t0 = 0
        while t0 < ntok:
            n = min(512, ntok - t0)
            if t0 == 0 and ntok % 512 != 0:
                n = ntok % 512
            blocks.append((t0, n)); t0 += n
        it = 0
        first = True
        for e_i, (wg, wu, wd) in enumerate(experts):
            c0 = 0
            for Gc in groups:
                b = it % 2
                dma(Wg[b][:, :, 0:Gc * 128], wg[:, c0:c0 + Gc * 128].rearrange("(kc p) n -> p kc n", p=128), eng='pool')
                dma(Wu[b][:, :, 0:Gc * 128], wu[:, c0:c0 + Gc * 128].rearrange("(kc p) n -> p kc n", p=128), eng='pool')
                dma(Wd[b][:, 0:Gc, :], wd[c0:c0 + Gc * 128, :].rearrange("(c p) n -> p c n", p=128), eng='pool')
                c0 += Gc * 128
                k = 0
                for (t0, n) in blocks:
                    for c in range(Gc):
                        pg_ = pg[k % 2]; pu_ = pu[k % 2]; s_ = sg[k % 2]
                        for kc in range(8):
                            mm(pg_[:, 0:n], Wg[b][:, kc, c * 128:(c + 1) * 128], hT[:, kc, t0:t0 + n], start=(kc == 0), stop=(kc == 7))
                        for kc in range(8):
                            mm(pu_[:, 0:n], Wu[b][:, kc, c * 128:(c + 1) * 128], hT[:, kc, t0:t0 + n], start=(kc == 0), stop=(kc == 7))
                        act(s_[:, 0:n], pg_[:, 0:n], AF.Silu)
                        tt(he[:, c, t0:t0 + n], s_[:, 0:n], pu_[:, 0:n], ALU.mult)
                        k += 1
                for idx in range(ntl):
                    pd_ = pd[idx % 2]
                    for hf in range(2):
                        for c in range(Gc):
                            mm(pd_[:, hf * 512:(hf + 1) * 512], he[:, c, idx * 128:(idx + 1) * 128], Wd[b][:, c, hf * 512:(hf + 1) * 512],
                               start=(c == 0), stop=(c == Gc - 1))
                    a = acc[:, idx, :]
                    if gates is None:
                        if first:
                            cp(a, pd_, eng='dve')
                        else:
                            tt(a, pd_, a, ALU.add)
                    else:
                        gcol = gates[:, idx, e_i:e_i + 1]
                        if first:
                            ts(a, pd_, gcol, None, ALU.mult)
                        else:
                            stt(a, pd_, gcol, a, ALU.mult, ALU.add)
                first = False
                it += 1
        P2.close()
        P2 = Pool()
        pp = [P2.ps("fpp2_%d" % k, [128, 1024]) for k in range(2)]
        Gbc = [P2.sb("fGbc%d" % s, [128, 1024]) for s in range(2)]
        for s in range(2):
            make_bc(P2, Gbc[s], coef[i][:, 5, s, :], pp[s])
        rb = res_bufs(P2)
        for idx, n in enumerate(tiles):
            residual_update(P2, rb, idx, n, acc[:, idx, :], Gbc[1 if n < 2 else 0])
        P2.close()
        P.close()

    def phase_s5():
        P = Pool()
        uT = P.sb("uT", [128, 8, TOK], BF16)
        zT = P.sb("zT", [128, 8, 2048], BF16)
        pc = P.sb("s5pc", [128, 2, 3, 32])
        th = P.sb("s5th", [128, 2, 32]); mag = P.sb("s5mag", [128, 2, 32])
        co = P.sb("s5co", [128, 2, 2, 32])
        Bm = P.sb("s5B", [128, 2, 8, 2, 128], BF16)
        Cm = P.sb("s5C", [128, 2, 8, 2, 128], BF16)
        P2 = Pool()
        hT = P2.sb("shT", [128, 8, TOK], BF16)
        W = P2.sb("sW", [128, 8, 1024], BF16)
        for kc in range(8):
            dma(W[:, kc, :], D['od_w_in'][kc * 128:(kc + 1) * 128, :], eng='pool')
        pp = [P2.ps("spp%d" % k, [128, 1024]) for k in range(2)]
        norm_to_hT(P2, 1, 'mix', hT, list(range(NT)), pp)
        S.barrier()
        pa = [P2.ps("spa%d" % k, [128, 512]) for k in range(2)]
        blocks = [(0, 256), (256, 512), (768, 512), (1280, 512), (1792, 512)]
        k = 0
        for c in range(8):
            for (t0, n) in blocks:
                ps = pa[k % 2]
                for kc in range(8):
                    mm(ps[:, 0:n], W[:, kc, c * 128:(c + 1) * 128], hT[:, kc, t0:t0 + n], start=(kc == 0), stop=(kc == 7))
                cp(uT[:, c, t0:t0 + n], ps[:, 0:n], eng=('act' if k % 2 else 'dve'))
                k += 1
        P2.close()
        if upto < 4.2:
            return
        P2 = Pool()
        prm = P2.sb("prm", [32, 2, 3, 128])
        for d in range(2):
            dma(prm[:, d, 0, :], D['od_s5_lambda_re'][d]); dma(prm[:, d, 1, :], D['od_s5_lambda_im'][d])
            lsr = P2.sb("lsr%d" % d, [32, 2])
            dma(lsr, D['od_s5_log_step'][d])
            cp(v3(prm[:, d, 2, :], 64), bc_last(lsr, 64))
        ptp = P2.ps("sptp", [128, 512])
        for d in range(2):
            for q in range(3):
                tr(ptp[:, (d * 3 + q) * 32:(d * 3 + q + 1) * 32], prm[:, d, q, :], ident[0:32, 0:32])
        cp(pc, ptp[:, 0:192].rearrange("p (d q g) -> p d q g", d=2, q=3))
        w_ = [P2.sb("s5w%d" % k, [128, 2, 32]) for k in range(10)]
        lr, li, dt, ar, ai, den, nr, t1, t2, kk = w_
        ts(lr, pc[:, :, 0, :], -1e-4, None, ALU.min)
        cp(li, pc[:, :, 1, :])
        act(dt, pc[:, :, 2, :], AF.Exp)
        tt(t1, lr, dt, ALU.mult)
        act(mag, t1, AF.Exp)
        tt(th, li, dt, ALU.mult)
        ki = P2.sb("s5ki", [128, 2, 32], I32)
        ts(ki, th, 1.0 / (2 * PI), None, ALU.mult)
        cp(kk, ki)
        stt(t1, kk, -2 * PI, th, ALU.mult, ALU.add)
        ts(t1, t1, PI, -PI, ALU.min, ALU.max)
        act(ai, t1, AF.Sin)
        stt(t2, t1, -1.0, t1, ALU.mult, ALU.max)
        act(ar, t2, AF.Sin, bias=PI / 2, scale=-1.0)
        tt(ar, ar, mag, ALU.mult); tt(ai, ai, mag, ALU.mult)
        tt(den, lr, lr, ALU.mult); tt(t1, li, li, ALU.mult); tt(den, den, t1, ALU.add)
        recip(den, den)
        ts(nr, ar, -1.0, None, ALU.add)
        tt(t1, nr, lr, ALU.mult); tt(t2, ai, li, ALU.mult); tt(t1, t1, t2, ALU.add)
        tt(co[:, 0], t1, den, ALU.mult)
        tt(t1, ai, lr, ALU.mult); tt(t2, nr, li, ALU.mult); tt(t1, t1, t2, ALU.subtract)
        tt(co[:, 1], t1, den, ALU.mult)
        bin_ = [[P2.sb("s5bin%d%d" % (a, b), [128, 128]) for b in range(2)] for a in range(2)]
        bb = [[P2.sb("s5bb%d%d" % (a, b), [128, 128]) for b in range(2)] for a in range(2)]
        cin = [[P2.sb("s5cin%d%d" % (a, b), [128, 128]) for b in range(2)] for a in range(2)]
        craw = [[P2.sb("s5craw%d%d" % (a, b), [128, 64]) for b in range(2)] for a in range(2)]
        ptc = [P2.ps("sptc%d" % k, [128, 512]) for k in range(2)]
        for a in range(2):
            for b in range(2):
                memset(bin_[a][b], 0.0)
        it = 0
        for d in range(2):
            for c in range(8):
                q = it % 2
                for ri, key in enumerate(('od_s5_b_re', 'od_s5_b_im')):
                    for pr_ in range(2):
                        src = D[key][d][c * 8 + pr_:c * 8 + 8:2]
                        dst = bin_[q][ri][pr_ * 64:(pr_ + 1) * 64, :].rearrange("n (g two k) -> n g two k", two=2, k=16)[:, :, pr_, :]
                        dma(dst, src.rearrange("g n k -> n g k"))
                cr = co[:, 0, d, 4 * c:4 * c + 4].unsqueeze(2).to_broadcast([128, 4, 32])
                cim = co[:, 1, d, 4 * c:4 * c + 4].unsqueeze(2).to_broadcast([128, 4, 32])
                br_ = v3(bin_[q][0], 32); bi_ = v3(bin_[q][1], 32)
                o_re = v3(bb[q][0], 32); o_im = v3(bb[q][1], 32)
                tA = v3(cin[q][0], 32); tB = v3(cin[q][1], 32)
                tt(tA, br_, cr, ALU.mult); tt(tB, bi_, cim, ALU.mult); tt(o_re, tA, tB, ALU.subtract, eng='pool')
                tt(tA, bi_, cr, ALU.mult); tt(tB, br_, cim, ALU.mult); tt(o_im, tA, tB, ALU.add, eng='pool')
                pt_ = ptc[q]
                tr(pt_[:, 0:128], bb[q][0], ident); tr(pt_[:, 128:256], bb[q][1], ident)
                cp(Bm[:, d, c, :, :], v3(pt_[:, 0:256], 128), eng='act')
                for ri, key in enumerate(('od_s5_c_re', 'od_s5_c_im')):
                    dma(craw[q][ri], D[key][d][c * 128:(c + 1) * 128, :])
                    sgn = 1.0 if ri == 0 else -1.0
                    ts(cin[q][ri][:, 0:64], craw[q][ri], par[:, 0:1], sgn, ALU.mult, ALU.mult)
                    ts(cin[q][ri][:, 64:128], craw[q][ri], par[:, 1:2], sgn, ALU.mult, ALU.mult)
                tr(pt_[:, 256:384], cin[q][0], ident); tr(pt_[:, 384:512], cin[q][1], ident)
                cp(Cm[:, d, c, :, :], v3(pt_[:, 256:512], 128), eng='dve')
                it += 1
        P2.close()
        if upto < 4.3:
            return
        P2 = Pool()
        pos = P2.sb("s5pos", [128, TOK])
        rowmask = P2.sb("s5rm", [128, 4]); colmask = P2.sb("s5cm", [128, 4, 128])
        dma(rowmask, D['rowmask']); dma(colmask, D['colmask'])
        BmM = P2.sb("s5BmM", [128, 2, 128], BF16); CmM = P2.sb("s5CmM", [128, 2, 128], BF16)
        CmN = P2.sb("s5CmN", [128, 128], BF16)
        tht = P2.sb("s5tht", [128, 2, 32])
        ts(tht, th, 1.0 / (2 * PI), None, ALU.mult)
        dcol = colsB[:, 112:120]
        A_ = P2.sb("s5A", [128, TOK]); Y_ = P2.sb("s5Y", [128, TOK]); F1 = P2.sb("s5F1", [128, TOK])
        SINb = [P2.sb("s5SIN%d" % k, [128, TOK], BF16) for k in range(2)]
        COSb = [P2.sb("s5COS%d" % k, [128, TOK], BF16) for k in range(2)]
        BUr = P2.sb("s5BUr", [128, TOK], BF16); BUi = P2.sb("s5BUi", [128, TOK], BF16)
        Mre = P2.sb("s5Mre", [128, TOK], BF16); Mim = P2.sb("s5Mim", [128, TOK], BF16)
        Wre = P2.sb("s5Wre", [128, TOK], BF16); Wim = P2.sb("s5Wim", [128, TOK], BF16)
        T1 = P2.sb("s5T1", [128, TOK], BF16); T2 = P2.sb("s5T2", [128, TOK], BF16)
        T3 = P2.sb("s5T3", [128, TOK], BF16); T4 = P2.sb("s5T4", [128, TOK], BF16)
        Sre = P2.sb("s5Sre0", [128, 2048], BF16); Sim = P2.sb("s5Sim0", [128, 2048], BF16)
        pdr = [P2.ps("spdr%d" % k, [128, 512]) for k in range(4)]
        py = [P2.ps("spy%d" % k, [128, 512]) for k in range(4)]
        gl = [P2.sb("s5gl%d" % k, [128, 512]) for k in range(3)]
        blocks = [(0, 256), (256, 512), (768, 512), (1280, 512), (1792, 512)]
        MAGIC = 12582912.0

        def rev(ap, n):
            return bass.AP(ap.tensor, int(ap.offset) + n - 1, [list(ap.ap[0]), [-1, n]])

        tiles = [(c, d, m) for c in range(8) for d in range(2) for m in range(4)]

        S5X = ""

        def tablesA(k):
            c, d, m = tiles[k]
            if S5X == "notab" and k > 1:
                return
            if m == 0:
                dma(pos, D['pos'][:, d, :])
            thc = tht[:, d, 4 * c + m:4 * c + m + 1]
            act(A_, pos, AF.Identity, scale=thc)
            act(Y_, A_, AF.Identity, bias=MAGIC)

        def tablesB(k):
            SIN = SINb[k % 2]; COS = COSb[k % 2]
            if S5X == "notab" and k > 1:
                return
            stt(F1, Y_, -MAGIC, A_, ALU.add, ALU.subtract)
            act(SIN, F1, AF.Sin, scale=-6.283185)
            act(A_, F1, AF.Abs)
            act(COS, A_, AF.Sin, scale=-6.283185, bias=PI / 2)

        def drive(k):
            c, d, m = tiles[k]
            act(BmM, Bm[:, d, c, :, :], AF.Identity, scale=rowmask[:, m:m + 1])
            tt(CmM, Cm[:, d, c, :, :], bc_mid(colmask[:, m, :], 2), ALU.mult, eng='pool')
            ts(CmN, CmM[:, 0, :], -1.0, None, ALU.mult, eng='pool')
            for bi, (t0, n) in enumerate(blocks):
                pr_ = pdr[(bi % 2) * 2]; pi_ = pdr[(bi % 2) * 2 + 1]
                mm(pr_[:, 0:n], BmM[:, 0, :], uT[:, c, t0:t0 + n])
                mm(pi_[:, 0:n], BmM[:, 1, :], uT[:, c, t0:t0 + n])
                cp(BUr[:, t0:t0 + n], pr_[:, 0:n], eng='act')
                cp(BUi[:, t0:t0 + n], pi_[:, 0:n], eng='act')

        def stage2(k):
            c, d, m = tiles[k]
            SIN = SINb[k % 2]; COS = COSb[k % 2]
            rcol = mag[:, d, 4 * c + m:4 * c + m + 1]
            if not (S5X == "nomod" and k > 1):
                tt(T1, BUr, COS, ALU.mult); tt(T2, BUi, SIN, ALU.mult); tt(Mre, T1, T2, ALU.add)
                tt(T3, BUi, COS, ALU.mult); tt(T4, BUr, SIN, ALU.mult); tt(Mim, T3, T4, ALU.subtract)
            for (M_, W_) in ((Mre, Wre), (Mim, Wim)):
                for (t0, n) in ((0, 256), (256, 2048)):
                    if S5X == "noscan" and k > 1:
                        continue
                    if d == 0:
                        o_ = W_[:, t0:t0 + n]; i_ = M_[:, t0:t0 + n]
                        init = 0.0 if t0 == 0 else W_[:, 255:256]
                    else:
                        o_ = rev(W_[:, t0:t0 + n], n); i_ = rev(M_[:, t0:t0 + n], n)
                        init = 0.0 if t0 == 0 else W_[:, 0:1]
                    d0 = rcol.to_broadcast([128, n])
                    rd = [M_[:, t0:t0 + n], rcol] + ([init] if t0 else [])
                    S.op('dve', lambda e, o_=o_, i_=i_, d0=d0, init=init: e.tensor_tensor_scan(
                        out=o_, data0=d0, data1=i_, initial=init, op0=ALU.mult, op1=ALU.add),
                        reads=rd, writes=[W_[:, t0:t0 + n]])
            wl = Wre[:, 256:TOK]; wi = Wim[:, 256:TOK]; cl = COS[:, 256:TOK]; sl = SIN[:, 256:TOK]
            a1 = T1[:, 0:2048]; a2 = T2[:, 0:2048]; a3 = T3[:, 0:2048]; a4 = T4[:, 0:2048]
            a1 = Sre; a3 = Sim
            tt(a1, wl, cl, ALU.mult); tt(a2, wi, sl, ALU.mult)
            tt(a3, wl, sl, ALU.mult); tt(a4, wi, cl, ALU.mult)
            for b in range(4):
                bs = slice(b * 512, (b + 1) * 512)
                mm(py[b], CmM[:, 0, :], a1[:, bs], start=(d == 0 and m == 0), stop=False)
                mm(py[b], CmN, a2[:, bs], start=False, stop=False)
                mm(py[b], CmM[:, 1, :], a3[:, bs], start=False, stop=False)
                mm(py[b], CmM[:, 1, :], a4[:, bs], start=False, stop=(d == 1 and m == 3))
            if d == 1 and m == 3:
                for b in range(4):
                    y = gl[0]; a = gl[1]; b_ = gl[2]
                    stt(y, uT[:, c, 256 + b * 512:256 + (b + 1) * 512], dcol[:, c:c + 1], py[b], ALU.mult, ALU.add)
                    act(a, y, AF.Square)
                    ts(a, a, 0.044715, 1.0, ALU.mult, ALU.add)
                    tt(a, a, y, ALU.mult)
                    act(b_, a, AF.Sigmoid, scale=2.0 * math.sqrt(2.0 / PI))
                    tt(zT[:, c, b * 512:(b + 1) * 512], b_, y, ALU.mult)

        tablesA(0); tablesB(0)
        for k in range(len(tiles)):
            c, d, m = tiles[k]
            last_of_chunk = False
            if k + 1 < len(tiles) and not last_of_chunk:
                tablesA(k + 1)
            drive(k)
            if k + 1 < len(tiles) and not last_of_chunk:
                tablesB(k + 1)
            stage2(k)
            if k + 1 < len(tiles) and last_of_chunk:
                tablesA(k + 1); tablesB(k + 1)
        P2.close()
        if upto < 4.4:
            return
        P2 = Pool()
        Wa = P2.sb("Wa", [128, 8, 1024], BF16); Wb = P2.sb("Wb", [128, 8, 1024], BF16)
        for kc in range(8):
            dma(Wa[:, kc, :], D['od_glu_w_a'][kc * 128:(kc + 1) * 128, :], eng='pool')
            dma(Wb[:, kc, :], D['od_glu_w_b'][kc * 128:(kc + 1) * 128, :], eng='pool')
        ppa = [P2.ps("gpa%d" % k, [128, 1024]) for k in range(2)]
        ppb = [P2.ps("gpb%d" % k, [128, 1024]) for k in range(2)]
        Gbc = P2.sb("gGbc", [128, 1024])
        make_bc(P2, Gbc, coef[1][:, 2, 0, :], ppa[0])
        sgm = [P2.sb("gsg%d" % k, [128, 1024]) for k in range(2)]
        go = [P2.sb("ggo%d" % k, [128, 1024]) for k in range(2)]
        rb = res_bufs(P2)
        for idx in range(16):
            pa_ = ppa[idx % 2]; pb_ = ppb[idx % 2]
            for (pp_, Wx) in ((pa_, Wa), (pb_, Wb)):
                for hf in range(2):
                    for j in range(8):
                        mm(pp_[:, hf * 512:(hf + 1) * 512], zT[:, j, idx * 128:(idx + 1) * 128], Wx[:, j, hf * 512:(hf + 1) * 512],
                           start=(j == 0), stop=(j == 7))
            act(sgm[idx % 2], pb_, AF.Sigmoid)
            tt(go[idx % 2], pa_, sgm[idx % 2], ALU.mult)
            residual_update(P2, rb, idx, 2 + idx, go[idx % 2], Gbc)
        P2.close()
        P.close()

    if upto >= 1:
        phase_filters()
    if upto >= 2:
        phase_ada()
    if upto > 2:
        phase_even_mixer()
    if upto >= 4:
        phase_ffn(0, list(range(NT)), [(D['ev_ffn_w_gate'][0], D['ev_ffn_w_up'][0], D['ev_ffn_w_down'][0])], 2816, [5, 5, 4, 4, 4])
    if upto > 4:
        phase_s5()
    if upto >= 6:
        phase_ffn(1, list(range(2, NT)),
                  [(D['od_moe_w_gate'][e], D['od_moe_w_up'][e], D['od_moe_w_down'][e]) for e in range(8)],
                  3584, 4, router=D['od_router'])
    S.barrier()
    dma(xs_out, xs)
    S.barrier()
    S.emit()
    return nc


_NC = {}


def _prep_inputs(inputs, b):
    g = lambda k: np.ascontiguousarray(np.asarray(inputs[k], dtype=np.float32))
    m = {}
    m['x'] = g('x')[b]; m['c'] = g('c')[b]; m['ctx'] = g('ctx')[b]; m['c_ctx'] = g('c_ctx')
    for k in ('ada_w', 'ada_b', 'norm_mix_pre', 'norm_mix_post', 'norm_ffn_pre', 'norm_ffn_post',
              'ev_ffn_w_gate', 'ev_ffn_w_up', 'ev_ffn_w_down'):
        m[k] = g(k)
    for k in ('ev_w_in', 'ev_hy_conv_w', 'ev_hy_conv_b', 'ev_hy_f_w1', 'ev_hy_f_b1', 'ev_hy_f_w2', 'ev_hy_f_b2',
              'ev_hy_f_wout', 'ev_hy_freq', 'ev_q_norm', 'ev_k_norm', 'ev_w_out', 'od_w_in', 'od_s5_d',
              'od_glu_w_a', 'od_glu_w_b', 'od_router', 'od_moe_w_gate', 'od_moe_w_up', 'od_moe_w_down',
              'od_s5_b_re', 'od_s5_b_im'):
        m[k] = g(k)[0]
    m['ev_hy_skip'] = g('ev_hy_skip')[0].reshape(1024)
    m['od_s5_lambda_re'] = g('od_s5_lambda_re')[0].reshape(2, 32, 128)
    m['od_s5_lambda_im'] = g('od_s5_lambda_im')[0].reshape(2, 32, 128)
    m['od_s5_log_step'] = g('od_s5_log_step')[0].reshape(2, 32, 2)
    m['od_s5_c_re'] = g('od_s5_c_re')[0].reshape(2, 1024, 64)
    m['od_s5_c_im'] = g('od_s5_c_im')[0].reshape(2, 1024, 64)
    return m


def kernel(_upto=99, _cores=8, **inputs):
    if _upto not in _NC:
        _NC[_upto] = build(_upto)
    nc = _NC[_upto]
    consts = _consts()
    shared = None
    in_maps = []
    for b in range(_cores):
        m = _prep_inputs(inputs, b)
        if shared is None:
            shared = {k: v for k, v in m.items() if k not in ('x', 'c', 'ctx')}
        else:
            for k in shared:
                m[k] = shared[k]
        m.update(consts)
        in_maps.append(m)
    res = run_bass_kernel_spmd(nc, in_maps, core_ids=list(range(_cores)))
    outs = [np.asarray(r["xs"], dtype=np.float32) for r in res.results]
    if _upto < 99:
        return np.stack(outs, axis=0)
    return np.stack([o[256:] for o in outs], axis=0).astype(np.float32)
```

```python
import math
from contextlib import ExitStack
import numpy as np
import ml_dtypes
import concourse.bass as bass
import concourse.mybir as mybir
from concourse.bass_utils import run_bass_kernel_spmd

F32 = mybir.dt.float32
BF16 = mybir.dt.bfloat16
I32 = mybir.dt.int32
AF = mybir.ActivationFunctionType
ALU = mybir.AluOpType
AX = mybir.AxisListType
EPS = 1e-6
PI = math.pi
NT = 18
TOK = 2304


def _prod(xs):
    r = 1
    for x in xs:
        r *= int(x)
    return r


class Sched:
    ENG = ['pe', 'act', 'dve', 'pool', 'sp']

    def __init__(self, nc):
        self.nc = nc
        self.ops = {e: [] for e in self.ENG}
        self.seq = {e: 0 for e in self.ENG}
        self.sems = {e: nc.alloc_semaphore("sem_" + e) for e in self.ENG}
        self.dma_sems = {}
        self.dma_cnt = {}
        self.dma_slot = {}
        self.free_slots = []
        self.slot_cls = {}
        self.nslots = 0
        self.waited = {e: {} for e in self.ENG}
        self.recs = {}

    def _region(self, ap):
        t = ap.tensor
        name = ap.name
        pairs = [(int(s), int(c)) for s, c in ap.ap]
        off = int(ap.offset)
        if 'DRAM' in str(ap.space).upper():
            lo = hi = off
            for s, c in pairs:
                if s >= 0:
                    hi += s * (c - 1)
                else:
                    lo += s * (c - 1)
            return name, 0, 1, lo, hi
        rowsize = _prod(list(t.shape)[1:])
        p0 = off // rowsize
        f0 = off % rowsize
        ps, pc = pairs[0]
        if ps == 0:
            pc = 1
        lo = hi = f0
        for s, c in pairs[1:]:
            if s >= 0:
                hi += s * (c - 1)
            else:
                lo += s * (c - 1)
        if 'PSUM' in str(ap.space).upper():
            epb = 2048 // (2 if ap.dtype == BF16 else 4)
            lo = (lo // epb) * epb
            hi = (hi // epb + 1) * epb - 1
            q0 = (p0 // 32) * 32
            q1 = ((p0 + pc + 31) // 32) * 32
            return name, q0, q1, lo, hi
        return name, p0, p0 + pc, lo, hi

    def _deps_and_update(self, eng, tok, reads, writes):
        deps = []
        for ap in reads:
            name, p0, p1, f0, f1 = self._region(ap)
            lst = self.recs.setdefault(name, [])
            is_psum = 'PSUM' in str(ap.space).upper()
            for r in lst:
                if r[0] < p1 and p0 < r[1] and r[2] <= f1 and f0 <= r[3]:
                    if r[4] == 'w':
                        deps.append(r[5])
                    elif is_psum and r[6] != eng:
                        deps.append(r[5])
            found = False
            for i, r in enumerate(lst):
                if r[4] == 'r' and r[6] == eng and r[0] == p0 and r[1] == p1 and r[2] == f0 and r[3] == f1:
                    lst[i] = (p0, p1, f0, f1, 'r', tok, eng)
                    found = True
                    break
            if not found:
                lst.append((p0, p1, f0, f1, 'r', tok, eng))
        for ap in writes:
            name, p0, p1, f0, f1 = self._region(ap)
            lst = self.recs.setdefault(name, [])
            keep = []
            for r in lst:
                ov = r[0] < p1 and p0 < r[1] and r[2] <= f1 and f0 <= r[3]
                if ov:
                    if r[5] == tok:
                        keep.append(r)
                        continue
                    deps.append(r[5])
                    contained = r[0] >= p0 and r[1] <= p1 and r[2] >= f0 and r[3] <= f1
                    if not contained:
                        keep.append(r)
                else:
                    keep.append(r)
            keep.append((p0, p1, f0, f1, 'w', tok, eng))
            self.recs[name] = keep
        return deps

    def _resolve_waits(self, eng, deps):
        waits = []
        for d in deps:
            if d[0] == 'dma':
                key = d[1]
                val = 16 * self.dma_cnt[key]
                sem = self.dma_sems[key]
                wk = ('dma', key)
            else:
                e2, val = d
                if e2 == 'pe' and eng == 'pe':
                    continue
                sem = self.sems[e2]
                wk = e2
            if self.waited[eng].get(wk, 0) >= val:
                continue
            self.waited[eng][wk] = val
            waits.append((sem, val))
        return waits

    def op(self, eng, fn, reads=(), writes=()):
        self.seq[eng] += 1
        tok = (eng, self.seq[eng])
        deps = self._deps_and_update(eng, tok, list(reads), list(writes))
        waits = self._resolve_waits(eng, deps)
        self.ops[eng].append((waits, fn, self.sems[eng], 1))

    def dma(self, eng, out, in_, key=None, **kw):
        if key is None:
            key = out.name if 'DRAM' not in str(out.space).upper() else 'st_' + in_.name
        cls = 'sw' if eng == 'pool' else 'hw'
        key = (key, cls)
        if key not in self.dma_slot:
            fl = [x for x in self.free_slots if self.slot_cls[x] == cls]
            if fl:
                slot = fl[0]
                self.free_slots.remove(slot)
            else:
                slot = self.nslots
                self.nslots += 1
                self.dma_sems[slot] = self.nc.alloc_semaphore("dsem_%d" % slot)
                self.dma_cnt[slot] = 0
                self.slot_cls[slot] = cls
            self.dma_slot[key] = slot
        key = self.dma_slot[key]
        tok = ('dma', key)
        deps = self._deps_and_update(eng, tok, [in_], [out])
        if any(d == tok for d in deps):
            deps = [d for d in deps if d != tok] + [tok]
        waits = self._resolve_waits(eng, deps)
        self.dma_cnt[key] += 1
        fn = (lambda e, out=out, in_=in_, kw=kw: e.dma_start(out=out, in_=in_, **kw))
        self.ops[eng].append((waits, fn, self.dma_sems[key], 16))

    def barrier(self):
        for eng in self.ENG:
            deps = [(e2, self.seq[e2]) for e2 in self.ENG if self.seq[e2] > 0 and e2 != eng]
            deps += [('dma', k) for k in self.dma_sems if self.dma_cnt[k] > 0]
            waits = self._resolve_waits(eng, deps)
            if waits:
                self.ops[eng].append((waits, None, None, 0))
        self.recs = {}
        self.free_slots = sorted(set(self.free_slots) | set(self.dma_slot.values()), reverse=True)
        self.dma_slot = {}

    def emit(self):
        nc = self.nc
        ops = self.ops

        def run(engine, lst):
            for waits, fn, sem, inc in lst:
                for s, v in waits:
                    engine.wait_ge(s, v)
                if fn is not None:
                    ins = fn(engine)
                    ins.then_inc(sem, inc)

        with nc.Block() as block:
            @block.tensor
            def _(e):
                run(e, ops['pe'])

            @block.scalar
            def _(e):
                run(e, ops['act'])

            @block.vector
            def _(e):
                run(e, ops['dve'])

            @block.gpsimd
            def _(e):
                run(e, ops['pool'])

            @block.sync
            def _(e):
                run(e, ops['sp'])


_CONSTS = None


def _bf(a):
    return np.ascontiguousarray(a.astype(np.float32)).astype(ml_dtypes.bfloat16)


def _consts():
    global _CONSTS
    if _CONSTS is not None:
        return _CONSTS
    c = {}
    c['ident'] = np.eye(128, dtype=np.float32)
    c['ones'] = np.ones((128, 128), np.float32)
    par = np.zeros((128, 2), np.float32)
    for p in range(128):
        par[p, (p // 16) % 2] = 1.0
    c['par'] = par
    rm = np.zeros((128, 4), np.float32)
    cm = np.zeros((128, 4, 128), np.float32)
    for m in range(4):
        rm[32 * m:32 * m + 32, m] = 1.0
        cm[:, m, 32 * m:32 * m + 32] = 1.0
    c['rowmask'] = rm
    c['colmask'] = cm
    for nm, L in (('l', 2048), ('c', 256)):
        N = 2 * L
        nt = L // 128
        t = np.arange(L, dtype=np.float64)
        f = np.arange(L, dtype=np.float64) + 0.5
        ang = 2.0 * np.pi * np.outer(t, f) / N
        C = np.cos(ang)
        Sn = np.sin(ang)
        c['dfc_' + nm] = _bf(C.reshape(nt, 128, nt, 128).transpose(2, 1, 0, 3))
        c['dfs_' + nm] = _bf(Sn.reshape(nt, 128, nt, 128).transpose(2, 1, 0, 3))
        sc = 2.0 / N
        c['dic_' + nm] = _bf((sc * C).reshape(nt, 128, nt, 128).transpose(0, 3, 2, 1))
        c['dis_' + nm] = _bf((sc * Sn).reshape(nt, 128, nt, 128).transpose(0, 3, 2, 1))
        tl = np.linspace(0.0, 1.0, L, dtype=np.float32)[:, None]
        bands = np.linspace(1e-4, 15, 16, dtype=np.float32)
        phase = (np.float32(2.0 * math.pi / L) * np.arange(L, dtype=np.float32)[:, None]) * bands
        z = np.concatenate([tl, np.cos(phase), -np.sin(phase)], axis=-1).astype(np.float32)
        c['zT_' + nm] = np.ascontiguousarray(z.T)
        slow = -math.log(1e-2) / 1.5
        fast = -math.log(1e-2) / 0.3
        deltas = np.linspace(slow, fast, 512, dtype=np.float32)
        dec = np.exp(-tl * deltas).astype(np.float32)
        c['dec_' + nm] = np.ascontiguousarray(dec.reshape(nt, 128, 512).transpose(1, 0, 2))
    rows = 2048 // 64
    row = np.repeat(np.arange(rows, dtype=np.float32), 64)
    col = np.tile(np.arange(64, dtype=np.float32), rows)
    inv = (10000.0 ** (-np.arange(16, dtype=np.float32) / 16)).astype(np.float32)
    ang = np.concatenate([row[:, None] * inv, col[:, None] * inv], axis=-1)
    c['ropec'] = np.ascontiguousarray(np.cos(ang).astype(np.float32).reshape(16, 128, 32).transpose(1, 0, 2))
    c['ropes'] = np.ascontiguousarray(np.sin(ang).astype(np.float32).reshape(16, 128, 32).transpose(1, 0, 2))
    posf = np.arange(TOK, dtype=np.float32)
    posb = np.concatenate([255.0 - np.arange(256), 256.0 + 2047.0 - np.arange(2048)]).astype(np.float32)
    c['pos'] = np.ascontiguousarray(np.stack([np.tile(posf, (128, 1)), np.tile(posb, (128, 1))], axis=1))
    _CONSTS = c
    return c


_CONST_DT = {'dfc_l': BF16, 'dfs_l': BF16, 'dic_l': BF16, 'dis_l': BF16,
             'dfc_c': BF16, 'dfs_c': BF16, 'dic_c': BF16, 'dis_c': BF16}

_IN_SHAPES = {
    'x': [2048, 1024], 'c': [1024], 'ctx': [256, 1024], 'c_ctx': [1024],
    'ada_w': [2, 1024, 6144], 'ada_b': [2, 6144],
    'norm_mix_pre': [2, 1024], 'norm_mix_post': [2, 1024], 'norm_ffn_pre': [2, 1024], 'norm_ffn_post': [2, 1024],
    'ev_w_in': [1024, 2304], 'ev_hy_conv_w': [3, 1536], 'ev_hy_conv_b': [1536],
    'ev_hy_f_w1': [33, 64], 'ev_hy_f_b1': [64], 'ev_hy_f_w2': [64, 64], 'ev_hy_f_b2': [64],
    'ev_hy_f_wout': [64, 2048], 'ev_hy_freq': [64], 'ev_hy_skip': [1024],
    'ev_q_norm': [64], 'ev_k_norm': [64], 'ev_w_out': [1024, 1024],
    'ev_ffn_w_gate': [1, 1024, 2816], 'ev_ffn_w_up': [1, 1024, 2816], 'ev_ffn_w_down': [1, 2816, 1024],
    'od_w_in': [1024, 1024], 'od_s5_lambda_re': [2, 32, 128], 'od_s5_lambda_im': [2, 32, 128],
    'od_s5_log_step': [2, 32, 2], 'od_s5_b_re': [2, 64, 64, 16], 'od_s5_b_im': [2, 64, 64, 16],
    'od_s5_c_re': [2, 1024, 64], 'od_s5_c_im': [2, 1024, 64], 'od_s5_d': [1024],
    'od_glu_w_a': [1024, 1024], 'od_glu_w_b': [1024, 1024], 'od_router': [1024, 8],
    'od_moe_w_gate': [8, 1024, 3584], 'od_moe_w_up': [8, 1024, 3584], 'od_moe_w_down': [8, 3584, 1024],
}


def build(upto=99):
    nc = bass.Bass("TRN2", target_bir_lowering=False)
    S = Sched(nc)
    D = {}
    for k, shp in _IN_SHAPES.items():
        D[k] = nc.dram_tensor(k, list(shp), F32, kind="ExternalInput").ap()
    for k, v in _consts().items():
        D[k] = nc.dram_tensor(k, list(v.shape), _CONST_DT.get(k, F32), kind="ExternalInput").ap()
    xs_out = nc.dram_tensor("xs", [TOK, 1024], F32, kind="ExternalOutput").ap()
    xs = nc.dram_tensor("xs_scr", [TOK, 1024], F32, kind="Internal").ap()
    kscr = {nm: nc.dram_tensor("kscr_" + nm, [2, 2, L, 512], BF16, kind="Internal").ap()
            for nm, L in (('l', 2048), ('c', 256))}

    uid = [0]

    class Pool:
        def __init__(self):
            self.st = ExitStack()

        def sb(self, name, shape, dt=F32):
            uid[0] += 1
            t = self.st.enter_context(nc.sbuf_tensor("%s_%d" % (name, uid[0]), list(shape), dt))
            return t.ap()

        def ps(self, name, shape, dt=F32):
            uid[0] += 1
            epb = 2048 // (2 if dt == BF16 else 4)
            shape = [shape[0], ((shape[1] + epb - 1) // epb) * epb]
            t = self.st.enter_context(nc.psum_tensor("%s_%d" % (name, uid[0]), list(shape), dt))
            return t.ap()

        def close(self):
            S.barrier()
            self.st.close()

    def mm(out, lhsT, rhs, start=True, stop=True):
        S.op('pe', lambda e: e.matmul(out, lhsT=lhsT, rhs=rhs, start=start, stop=stop), reads=[lhsT, rhs], writes=[out])

    def tr(out, in_, ident):
        S.op('pe', lambda e: e.transpose(out, in_, ident), reads=[in_, ident], writes=[out])

    def _isap(x):
        return not isinstance(x, (int, float)) and x is not None

    def act(out, in_, func, bias=None, scale=None, accum=None):
        kw = {}
        rd = [in_]
        if bias is not None:
            kw['bias'] = bias
            if _isap(bias):
                rd.append(bias)
        if scale is not None:
            kw['scale'] = scale
            if _isap(scale):
                rd.append(scale)
        wr = [out]
        if accum is not None:
            kw['accum_out'] = accum
            wr.append(accum)
        S.op('act', lambda e: e.activation(out=out, in_=in_, func=func, **kw), reads=rd, writes=wr)

    def ts(out, in0, s1, s2=None, op0=ALU.mult, op1=None, eng='dve'):
        rd = [in0] + [s for s in (s1, s2) if _isap(s)]
        if op1 is None:
            S.op(eng, lambda e: e.tensor_scalar(out=out, in0=in0, scalar1=s1, scalar2=None, op0=op0), reads=rd, writes=[out])
        else:
            S.op(eng, lambda e: e.tensor_scalar(out=out, in0=in0, scalar1=s1, scalar2=s2, op0=op0, op1=op1), reads=rd, writes=[out])

    def tt(out, in0, in1, op, eng='dve'):
        S.op(eng, lambda e: e.tensor_tensor(out=out, in0=in0, in1=in1, op=op), reads=[in0, in1], writes=[out])

    def stt(out, in0, scalar, in1, op0, op1):
        rd = [in0, in1] + ([scalar] if _isap(scalar) else [])
        S.op('dve', lambda e: e.scalar_tensor_tensor(out=out, in0=in0, scalar=scalar, in1=in1, op0=op0, op1=op1), reads=rd, writes=[out])

    def cp(out, in_, eng='dve'):
        if eng == 'act':
            S.op('act', lambda e: e.copy(out=out, in_=in_), reads=[in_], writes=[out])
        else:
            S.op(eng, lambda e: e.tensor_copy(out=out, in_=in_), reads=[in_], writes=[out])

    def recip(out, in_):
        S.op('dve', lambda e: e.reciprocal(out=out, in_=in_), reads=[in_], writes=[out])

    def memset(ap, val, eng='pool'):
        S.op(eng, lambda e: e.memset(ap, val), writes=[ap])

    def dma(out, in_, eng='sp', **kw):
        S.dma(eng, out, in_, **kw)

    def v3(ap, b):
        return ap.rearrange("p (a b) -> p a b", b=b)

    def bc_last(ap, n):
        return ap.unsqueeze(2).to_broadcast([ap.shape[0], ap.shape[1], n])

    def bc_mid(ap, n):
        return ap.unsqueeze(1).to_broadcast([ap.shape[0], n, ap.shape[1]])

    G = Pool()
    ident = G.sb("ident", [128, 128])
    identb = G.sb("identb", [128, 128], BF16)
    ones = G.sb("ones", [128, 128])
    par = G.sb("par", [128, 2])
    colsA = G.sb("colsA", [128, 112])
    colsB = G.sb("colsB", [128, 120])
    coef = [G.sb("coef%d" % i, [128, 6, 2, 8]) for i in range(2)]
    dma(ident, D['ident'])
    dma(ones, D['ones'])
    dma(par, D['par'])
    cp(identb, ident)
    dma(xs[0:256, :], D['ctx'])
    dma(xs[256:TOK, :], D['x'])

    def phase_filters():
        P = Pool()
        w1 = P.sb("fw1", [33, 64]); w2 = P.sb("fw2", [64, 64]); wout = P.sb("fwout", [64, 2048])
        cols = P.sb("fcols", [64, 3]); frb = P.sb("ffrb", [64, 2])
        dma(w1, D['ev_hy_f_w1']); dma(w2, D['ev_hy_f_w2']); dma(wout, D['ev_hy_f_wout'])
        for i, k in enumerate(('ev_hy_f_b1', 'ev_hy_f_b2', 'ev_hy_freq')):
            dma(cols[:, i:i + 1], D[k].rearrange("(p o) -> p o", o=1))
        tt(frb[:, 0:1], cols[:, 0:1], cols[:, 2:3], ALU.mult)
        tt(frb[:, 1:2], cols[:, 1:2], cols[:, 2:3], ALU.mult)
        psm = [P.ps("fps%d" % i, [128, 512]) for i in range(4)]
        for nm, L in (('l', 2048), ('c', 256)):
            nt = L // 128
            zT = P.sb("fzT", [33, L]); h1 = P.sb("fh1", [64, L]); h2 = P.sb("fh2", [64, L])
            tmp = [P.sb("ftmp%d" % i, [64, 512]) for i in range(2)]
            tki = P.sb("ftki", [64, 512], I32); tkf = P.sb("ftkf", [64, 512])
            dec = P.sb("fdec", [128, nt, 512])
            dma(zT, D['zT_' + nm]); dma(dec, D['dec_' + nm])
            for (wm, src, dst, bcol) in ((w1, zT, h1, 0), (w2, h1, h2, 1)):
                for bi, b0 in enumerate(range(0, L, 512)):
                    n = min(512, L - b0)
                    ps = psm[bi % 2]
                    mm(ps[0:64, 0:n], wm, src[:, b0:b0 + n])
                    t_ = tmp[bi % 2]
                    ts(t_[:, 0:n], ps[0:64, 0:n], cols[:, 2:3], frb[:, bcol:bcol + 1], ALU.mult, ALU.add)
                    ts(tki[:, 0:n], t_[:, 0:n], 1.0 / (2 * PI), None, ALU.mult)
                    cp(tkf[:, 0:n], tki[:, 0:n])
                    stt(t_[:, 0:n], tkf[:, 0:n], -2 * PI, t_[:, 0:n], ALU.mult, ALU.add)
                    ts(t_[:, 0:n], t_[:, 0:n], PI, -PI, ALU.min, ALU.max)
                    act(dst[:, b0:b0 + n], t_[:, 0:n], AF.Sin)
            tf = [P.sb("ftf%d" % i, [128, 512]) for i in range(2)]
            tb = [P.sb("ftb%d" % i, [128, 512]) for i in range(2)]
            dft = [[P.sb("fdft%d%d" % (i, j), [128, nt, 128], BF16) for j in range(2)] for i in range(2)]
            kst = [[P.sb("fkst%d%d" % (i, j), [128, 512], BF16) for j in range(2)] for i in range(2)]
            for o in range(2):
                hs = P.sb("fhs", [128, nt, 512], BF16); hd = P.sb("fhd", [128, nt, 512], BF16)
                for t_i in range(nt):
                    pa = psm[0 + (t_i % 2) * 2]; pb = psm[1 + (t_i % 2) * 2]
                    mm(pa, h2[:, t_i * 128:(t_i + 1) * 128], wout[:, o * 512:(o + 1) * 512])
                    mm(pb, h2[:, t_i * 128:(t_i + 1) * 128], wout[:, 1024 + o * 512:1024 + (o + 1) * 512])
                    a = tf[t_i % 2]; b = tb[t_i % 2]
                    tt(a, pa, dec[:, t_i, :], ALU.mult)
                    tt(b, pb, dec[:, t_i, :], ALU.mult)
                    if t_i == 0:
                        memset(b[0:1, :], 0.0, eng='dve')
                    tt(hs[:, t_i, :], a, b, ALU.add, eng='pool')
                    tt(hd[:, t_i, :], a, b, ALU.subtract, eng='pool')
                for fc in range(nt):
                    cf = dft[fc % 2][0]; sf = dft[fc % 2][1]
                    dma(cf, D['dfc_' + nm][fc]); dma(sf, D['dfs_' + nm][fc])
                    pr = psm[(fc % 2) * 2]; pi_ = psm[(fc % 2) * 2 + 1]
                    for tc in range(nt):
                        mm(pr, cf[:, tc, :], hs[:, tc, :], start=(tc == 0), stop=(tc == nt - 1))
                    for tc in range(nt):
                        mm(pi_, sf[:, tc, :], hd[:, tc, :], start=(tc == 0), stop=(tc == nt - 1))
                    kr = kst[fc % 2][0]; ki = kst[fc % 2][1]
                    cp(kr, pr, eng='dve'); cp(ki, pi_, eng='act')
                    dma(kscr[nm][o, 0, fc * 128:(fc + 1) * 128, :], kr, key='kst')
                    dma(kscr[nm][o, 1, fc * 128:(fc + 1) * 128, :], ki, key='kst')
        P.close()

    def phase_ada():
        P = Pool()
        vecA = P.sb("vecA", [112, 128]); vecB = P.sb("vecB", [120, 128])
        dma(vecA[0:8, :], D['c'].rearrange("(r p) -> r p", p=128))
        dma(vecA[8:16, :], D['c_ctx'].rearrange("(r p) -> r p", p=128))
        for i in range(2):
            dma(vecA[16 + 48 * i:64 + 48 * i, :], D['ada_b'][i].rearrange("(r p) -> r p", p=128))
        r0 = 0
        for k in ('norm_mix_pre', 'norm_mix_post', 'norm_ffn_pre', 'norm_ffn_post'):
            dma(vecB[r0:r0 + 16, :], D[k].rearrange("i (r p) -> (i r) p", p=128))
            r0 += 16
        dma(vecB[64:100, :], D['ev_hy_conv_w'].rearrange("k (r p) -> (k r) p", p=128))
        dma(vecB[100:112, :], D['ev_hy_conv_b'].rearrange("(r p) -> r p", p=128))
        dma(vecB[112:120, :], D['od_s5_d'].rearrange("(r p) -> r p", p=128))
        pt = P.ps("apt", [128, 512])
        tr(pt[:, 0:112], vecA, ident[0:112, 0:112])
        cp(colsA, pt[:, 0:112])
        pt2 = P.ps("apt2", [128, 512])
        tr(pt2[:, 0:120], vecB, ident[0:120, 0:120])
        cp(colsB, pt2[:, 0:120])
        sc2 = P.sb("sc2", [128, 8, 2])
        act(sc2[:, :, 0], colsA[:, 0:8], AF.Silu)
        act(sc2[:, :, 1], colsA[:, 8:16], AF.Silu)
        aw = [P.sb("aw%d" % i, [128, 8, 512]) for i in range(3)]
        pm = [P.ps("apm%d" % i, [128, 96]) for i in range(2)]
        prow = [P.ps("aprow%d" % i, [128, 512]) for i in range(2)]
        rows = P.sb("arows", [2, 6144])
        mod = P.sb("mod", [128, 48, 2])
        for i in range(2):
            for nb in range(12):
                a = aw[nb % 3]
                dma(a, D['ada_w'][i][:, nb * 512:(nb + 1) * 512].rearrange("(kc p) n -> p kc n", p=128))
                pr = prow[nb % 2]
                for kc in range(8):
                    mm(pr[0:2, :], sc2[:, kc, :], a[:, kc, :], start=(kc == 0), stop=(kc == 7))
                cp(rows[:, nb * 512:(nb + 1) * 512], pr[0:2, :], eng=('act' if nb % 2 else 'dve'))
            for m in range(48):
                tr(pm[i][:, m * 2:m * 2 + 2], rows[:, m * 128:(m + 1) * 128], ident[0:2, 0:2])
            tt(mod, v3(pm[i][:, 0:96], 2), bc_last(colsA[:, 16 + 48 * i:64 + 48 * i], 2), ALU.add)
            nmp = colsB[:, 0 + 8 * i:8 + 8 * i]; nmpost = colsB[:, 16 + 8 * i:24 + 8 * i]
            nfp = colsB[:, 32 + 8 * i:40 + 8 * i]; nfpost = colsB[:, 48 + 8 * i:56 + 8 * i]
            for s in range(2):
                stt(coef[i][:, 0, s, :], mod[:, 8:16, s], 1.0, nmp, ALU.add, ALU.mult)
                cp(coef[i][:, 1, s, :], mod[:, 0:8, s])
                tt(coef[i][:, 2, s, :], mod[:, 16:24, s], nmpost, ALU.mult)
                stt(coef[i][:, 3, s, :], mod[:, 32:40, s], 1.0, nfp, ALU.add, ALU.mult)
                cp(coef[i][:, 4, s, :], mod[:, 24:32, s])
                tt(coef[i][:, 5, s, :], mod[:, 40:48, s], nfpost, ALU.mult)
        P.close()

    def make_bc(P, dst, col, pp):
        dg = [P.sb("dg%d" % i, [128, 128]) for i in range(2)]
        for j in range(8):
            d = dg[j % 2]
            ts(d, ident, col[:, j:j + 1], None, ALU.mult)
            mm(pp[:, j * 128:(j + 1) * 128], ones, d)
        cp(dst, pp)

    def norm_to_hT(P, i, kind, hT, tiles, pp, extra=None):
        xin = [P.sb("nx%d" % k, [128, 1024]) for k in range(3)]
        xn = [P.sb("nxn%d" % k, [128, 1024]) for k in range(3)]
        junk = P.sb("njunk", [128, 1024])
        st = P.sb("nst", [128, 3 * NT])
        ka = 0 if kind == 'mix' else 3
        DBG = 9
        for idx, n in enumerate(tiles):
            s = 1 if n < 2 else 0
            xt = xin[idx % 3]; xo = xn[idx % 3]; p2 = pp[idx % 2]
            dma(xt, xs[n * 128:(n + 1) * 128, :])
            if DBG < 1:
                continue
            act(junk, xt, AF.Square, accum=st[:, n:n + 1])
            act(st[:, NT + n:NT + n + 1], st[:, n:n + 1], AF.Sqrt, bias=EPS, scale=1.0 / 1024)
            recip(st[:, 2 * NT + n:2 * NT + n + 1], st[:, NT + n:NT + n + 1])
            act(xo, xt, AF.Identity, scale=st[:, 2 * NT + n:2 * NT + n + 1])
            if DBG < 2:
                continue
            for j in range(8):
                tr(p2[:, j * 128:(j + 1) * 128], xo[:, j * 128:(j + 1) * 128], ident)
            if DBG < 4:
                continue
            for j in range(8):
                A = coef[i][:, ka, s, j:j + 1]; B = coef[i][:, ka + 1, s, j:j + 1]
                o = hT[:, j, idx * 128:(idx + 1) * 128]
                if j < 4:
                    ts(o, p2[:, j * 128:(j + 1) * 128], A, B, ALU.mult, ALU.add)
                else:
                    act(o, p2[:, j * 128:(j + 1) * 128], AF.Identity, bias=B, scale=A)
            if extra is not None:
                extra(idx, n, p2, coef[i][:, ka, s, :], coef[i][:, ka + 1, s, :])

    def residual_update(P, bufs, idx, n, src, Gbc):
        xt, tmp, st, junk = bufs
        xt = xt[idx % 3]; tmp = tmp[idx % 3]
        c0 = (idx % 3) * 3
        dma(xt, xs[n * 128:(n + 1) * 128, :])
        act(junk, src, AF.Square, accum=st[:, c0:c0 + 1])
        if upto < 2.82:
            return
        act(st[:, c0 + 1:c0 + 2], st[:, c0:c0 + 1], AF.Sqrt, bias=EPS, scale=1.0 / 1024)
        recip(st[:, c0 + 2:c0 + 3], st[:, c0 + 1:c0 + 2])
        if upto < 2.83:
            return
        stt(tmp, src, st[:, c0 + 2:c0 + 3], Gbc, ALU.mult, ALU.mult)
        if upto < 2.84:
            return
        tt(tmp, tmp, xt, ALU.add, eng='pool')
        if upto < 2.85:
            return
        dma(xs[n * 128:(n + 1) * 128, :], tmp)

    def res_bufs(P):
        return ([P.sb("rx%d" % k, [128, 1024]) for k in range(3)], [P.sb("rt%d" % k, [128, 1024]) for k in range(3)],
                P.sb("rst", [128, 9]), P.sb("rjunk", [128, 1024]))

    def phase_even_mixer():
        P = Pool()
        z_tok = P.sb("z_tok", [128, NT, 1536], BF16)
        y_at = P.sb("y_at", [128, NT, 512], BF16)
        PH = Pool()
        hT = PH.sb("hT", [128, 8, TOK], BF16)
        P3 = Pool()
        pp = [P3.ps("pp%d" % k, [128, 1024]) for k in range(2)]
        norm_to_hT(P3, 0, 'mix', hT, list(range(NT)), pp)
        P3.close()
        if upto < 2.2:
            return
        PQ = Pool()
        QT = PQ.sb("QT", [64, 8, TOK], BF16)
        KT = PQ.sb("KT", [64, 2, TOK], BF16)
        Va = PQ.sb("Va", [128, NT, 2, 65], BF16)
        memset(Va, 1.0)
        P3 = Pool()
        W = P3.sb("Wqkv", [128, 8, 768], BF16)
        for kc in range(8):
            dma(W[:, kc, :], D['ev_w_in'][kc * 128:(kc + 1) * 128, 1536:2304], eng='pool')
        pp = [P3.ps("pp%d" % k, [128, 1024]) for k in range(2)]
        ptb = [P3.ps("ptb%d" % k, [128, 1024], BF16) for k in range(2)]
        gq = P3.sb("gq", [128, 64]); gk = P3.sb("gk", [128, 64])
        dma(gq, D['ev_q_norm'].partition_broadcast(128)); dma(gk, D['ev_k_norm'].partition_broadcast(128))
        qkg = P3.sb("qkg", [128, 10, 64])
        cp(qkg[:, 0:8, :], bc_mid(gq, 8)); cp(qkg[:, 8:10, :], bc_mid(gk, 2))
        ropec = P3.sb("ropec", [128, 16, 32]); ropes = P3.sb("ropes", [128, 16, 32])
        dma(ropec, D['ropec']); dma(ropes, D['ropes'])
        sq = P3.sb("sq", [128, 640]); sst = P3.sb("sst", [128, 30])
        qn = P3.sb("qn", [128, 10, 64]); qr = [P3.sb("qr%d" % k, [128, 10, 64], BF16) for k in range(2)]
        rt = [P3.sb("rt%d" % k, [128, 10, 32]) for k in range(4)]
        for n in range(NT):
            pq = pp[n % 2]
            for kc in range(8):
                mm(pq[:, 0:512], hT[:, kc, n * 128:(n + 1) * 128], W[:, kc, 0:512], start=(kc == 0), stop=(kc == 7))
            for kc in range(8):
                mm(pq[:, 512:768], hT[:, kc, n * 128:(n + 1) * 128], W[:, kc, 512:768], start=(kc == 0), stop=(kc == 7))
            act(sq, pq[:, 0:640], AF.Square)
            S.op('dve', lambda e, o=sst[:, 0:10], i_=v3(sq, 64): e.tensor_reduce(out=o, in_=i_, axis=AX.X, op=ALU.add),
                 reads=[sq], writes=[sst[:, 0:10]])
            act(sst[:, 10:20], sst[:, 0:10], AF.Sqrt, bias=EPS, scale=1.0 / 64)
            recip(sst[:, 20:30], sst[:, 10:20])
            tt(qn, v3(pq[:, 0:640], 64), bc_last(sst[:, 20:30], 64), ALU.mult)
            q_ = qr[n % 2]
            if n >= 2:
                tt(qn, qn, qkg, ALU.mult)
                cc = bc_mid(ropec[:, n - 2, :], 10); ss_ = bc_mid(ropes[:, n - 2, :], 10)
                x1 = qn[:, :, 0:32]; x2 = qn[:, :, 32:64]
                tt(rt[0], x1, cc, ALU.mult); tt(rt[1], x2, ss_, ALU.mult)
                tt(q_[:, :, 0:32], rt[0], rt[1], ALU.subtract, eng='pool')
                tt(rt[2], x1, ss_, ALU.mult); tt(rt[3], x2, cc, ALU.mult)
                tt(q_[:, :, 32:64], rt[2], rt[3], ALU.add, eng='pool')
            else:
                tt(q_, qn, qkg, ALU.mult)
            cp(Va[:, n, :, 0:64], v3(pq[:, 640:768], 64), eng='act')
            pt0 = ptb[0]; pt1 = ptb[1]
            for h in range(8):
                tr(pt0[0:64, h * 128:(h + 1) * 128], q_[:, h, :], identb)
            for h in range(2):
                tr(pt1[0:64, h * 128:(h + 1) * 128], q_[:, 8 + h, :], identb)
            cp(QT[:, :, n * 128:(n + 1) * 128], v3(pt0[0:64, :], 128), eng='act')
            cp(KT[:, :, n * 128:(n + 1) * 128], v3(pt1[0:64, 0:256], 128), eng='dve')
        P3.close()
        if upto < 2.3:
            return
        P3 = Pool()
        psc = [P3.ps("psc%d" % k, [128, 512]) for k in range(4)]
        po = [P3.ps("po%d" % k, [128, 512]) for k in range(2)]
        PT = [P3.sb("PT%d" % k, [128, NT, 512], BF16) for k in range(2)]
        rc = P3.sb("rc", [128, 8])
        it = 0
        for h in range(8):
            g = h // 4
            jobs = [(0, 256, [0, 1], 0)] + [(256 + qb * 512, 512, list(range(NT)), 2 + qb * 4) for qb in range(4)]
            for (q0, nq, kcs, tile0) in jobs:
                pt_ = PT[it % 2]
                for kc in kcs:
                    ps = psc[kc % 4]
                    mm(ps[:, 0:nq], KT[:, g, kc * 128:(kc + 1) * 128], QT[:, h, q0:q0 + nq])
                    act(pt_[:, kc, 0:nq], ps[:, 0:nq], AF.Exp, scale=0.125)
                pov = po[it % 2]
                nqt = nq // 128
                for qt in range(nqt):
                    for ki, kc in enumerate(kcs):
                        mm(pov[:, qt * 65:(qt + 1) * 65], pt_[:, kc, qt * 128:(qt + 1) * 128], Va[:, kc, g, :],
                           start=(ki == 0), stop=(ki == len(kcs) - 1))
                pv = v3(pov[:, 0:nqt * 65], 65)
                r_ = rc[:, (it % 2) * 4:(it % 2) * 4 + nqt]
                recip(r_, pv[:, :, 64])
                tt(y_at[:, tile0:tile0 + nqt, h * 64:(h + 1) * 64], pv[:, :, 0:64], bc_last(r_, 64), ALU.mult)
                it += 1
        P3.close()
        PQ.close()
        if upto < 2.4:
            return
        P3 = Pool()
        W = P3.sb("Why", [128, 8, 1536], BF16)
        for kc in range(8):
            dma(W[:, kc, :], D['ev_w_in'][kc * 128:(kc + 1) * 128, 0:1536], eng='pool')
        pa = [P3.ps("pa%d" % k, [128, 512]) for k in range(2)]
        ptb = [P3.ps("ptb%d" % k, [128, 1024], BF16) for k in range(2)]
        pc_c = [P3.sb("pc_c%d" % k, [128, 258]) for k in range(2)]
        pc_l = [P3.sb("pc_l%d" % k, [128, 2050]) for k in range(2)]
        for k in range(2):
            memset(pc_c[k], 0.0); memset(pc_l[k], 0.0)
        zf = P3.sb("zf", [128, TOK])
        zc = [P3.sb("zc%d" % k, [128, TOK], BF16) for k in range(2)]
        blocks = [(0, 256), (256, 512), (768, 512), (1280, 512), (1792, 512)]
        for c in range(12):
            pcc = pc_c[c % 2]; pcl = pc_l[c % 2]
            for bi, (t0, n) in enumerate(blocks):
                ps = pa[bi % 2]
                for kc in range(8):
                    mm(ps[:, 0:n], W[:, kc, c * 128:(c + 1) * 128], hT[:, kc, t0:t0 + n], start=(kc == 0), stop=(kc == 7))
                dst = pcc[:, 1:257] if t0 == 0 else pcl[:, 1 + t0 - 256:1 + t0 - 256 + n]
                cp(dst, ps[:, 0:n], eng='act')
            w0 = colsB[:, 64 + c:65 + c]; w1c = colsB[:, 76 + c:77 + c]; w2c = colsB[:, 88 + c:89 + c]
            bcl = colsB[:, 100 + c:101 + c]
            zcc = zc[c % 2]
            for (pc, L, off) in ((pcc, 256, 0), (pcl, 2048, 256)):
                ts(zf[:, off:off + L], pc[:, 1:L + 1], w1c, bcl, ALU.mult, ALU.add)
                stt(zf[:, off:off + L], pc[:, 0:L], w0, zf[:, off:off + L], ALU.mult, ALU.add)
                stt(zcc[:, off:off + L], pc[:, 2:L + 2], w2c, zf[:, off:off + L], ALU.mult, ALU.add)
            for gi, n0 in enumerate((0, 8, 16)):
                cnt = min(8, NT - n0)
                pt = ptb[gi % 2]
                for k in range(cnt):
                    tr(pt[:, k * 128:(k + 1) * 128], zcc[:, (n0 + k) * 128:(n0 + k + 1) * 128], identb)
                cp(z_tok[:, n0:n0 + cnt, c * 128:(c + 1) * 128], v3(pt[:, 0:cnt * 128], 128), eng=('act' if gi % 2 else 'dve'))
        P3.close()
        PH.close()
        if upto < 2.5:
            return
        P3 = Pool()
        skip = P3.sb("skip", [128, 1024])
        dma(skip, D['ev_hy_skip'].partition_broadcast(128))
        psm = [P3.ps("hps%d" % k, [128, 512]) for k in range(6)]
        for nm, L, tile0 in (('l', 2048, 2), ('c', 256, 0)):
            nt = L // 128
            Yr = P3.sb("Yr_" + nm, [128, nt, 512], BF16); Yi = P3.sb("Yi_" + nm, [128, nt, 512], BF16)
            v1 = P3.sb("v1_" + nm, [128, nt, 512], BF16)
            dft = [[P3.sb("hdft%s%d%d" % (nm, i, j), [128, nt, 128], BF16) for j in range(2)] for i in range(2)]
            tm = [P3.sb("htm%s%d" % (nm, i), [128, 512]) for i in range(6)]
            for o in range(2):
                vsrc = (lambda t_: z_tok[:, tile0 + t_, 0:512]) if o == 0 else (lambda t_: v1[:, t_, :])
                vdst = (lambda t_: v1[:, t_, :]) if o == 0 else (lambda t_: z_tok[:, tile0 + t_, 0:512])
                dma(Yr, kscr[nm][o, 0].rearrange("(f p) c -> p f c", p=128))
                dma(Yi, kscr[nm][o, 1].rearrange("(f p) c -> p f c", p=128))
                for fc in range(nt):
                    cf = dft[fc % 2][0]; sf = dft[fc % 2][1]
                    dma(cf, D['dfc_' + nm][fc]); dma(sf, D['dfs_' + nm][fc])
                    pr = psm[(fc % 2) * 2]; pi_ = psm[(fc % 2) * 2 + 1]
                    for tc in range(nt):
                        mm(pr, cf[:, tc, :], vsrc(tc), start=(tc == 0), stop=(tc == nt - 1))
                    for tc in range(nt):
                        mm(pi_, sf[:, tc, :], vsrc(tc), start=(tc == 0), stop=(tc == nt - 1))
                    tt(tm[0], pr, Yr[:, fc, :], ALU.mult); tt(tm[1], pi_, Yi[:, fc, :], ALU.mult)
                    tt(tm[2], pr, Yi[:, fc, :], ALU.mult); tt(tm[3], pi_, Yr[:, fc, :], ALU.mult)
                    tt(Yr[:, fc, :], tm[0], tm[1], ALU.subtract, eng='pool')
                    tt(Yi[:, fc, :], tm[2], tm[3], ALU.add, eng='pool')
                for t_i in range(nt):
                    ci = dft[t_i % 2][0]; si = dft[t_i % 2][1]
                    dma(ci, D['dic_' + nm][t_i]); dma(si, D['dis_' + nm][t_i])
                    py = psm[4 + t_i % 2]
                    for fc in range(nt):
                        mm(py, ci[:, fc, :], Yr[:, fc, :], start=(fc == 0), stop=False)
                    for fc in range(nt):
                        mm(py, si[:, fc, :], Yi[:, fc, :], start=False, stop=(fc == nt - 1))
                    a = tm[4 + t_i % 2]
                    tt(a, vsrc(t_i), skip[:, o * 512:(o + 1) * 512], ALU.mult)
                    tt(a, a, py, ALU.add)
                    tt(vdst(t_i), a, z_tok[:, tile0 + t_i, (o + 1) * 512:(o + 2) * 512], ALU.mult)
        P3.close()
        if upto < 2.6:
            return
        P3 = Pool()
        Wo = P3.sb("Wo", [128, 8, 1024], BF16)
        for kc in range(8):
            dma(Wo[:, kc, :], D['ev_w_out'][kc * 128:(kc + 1) * 128, :], eng='pool')
        pp = [P3.ps("opp%d" % k, [128, 1024]) for k in range(2)]
        ptb = [P3.ps("optb%d" % k, [128, 1024], BF16) for k in range(2)]
        Gbc = [P3.sb("Gbc%d" % s, [128, 1024]) for s in range(2)]
        for s in range(2):
            make_bc(P3, Gbc[s], coef[0][:, 2, s, :], pp[s])
        mixT = [P3.sb("mixT%d" % k, [128, 8, 128], BF16) for k in range(2)]
        rb = res_bufs(P3)
        if upto < 2.7:
            return
        for n in range(NT):
            pt = ptb[n % 2]; mt = mixT[n % 2]
            for j in range(4):
                tr(pt[:, j * 128:(j + 1) * 128], z_tok[:, n, j * 128:(j + 1) * 128], identb)
            for j in range(4):
                tr(pt[:, (4 + j) * 128:(5 + j) * 128], y_at[:, n, j * 128:(j + 1) * 128], identb)
            cp(mt, v3(pt, 128), eng='act')
            po_ = pp[n % 2]
            for hf in range(2):
                for j in range(8):
                    mm(po_[:, hf * 512:(hf + 1) * 512], mt[:, j, :], Wo[:, j, hf * 512:(hf + 1) * 512], start=(j == 0), stop=(j == 7))
            if upto >= 2.8:
                residual_update(P3, rb, n, n, po_, Gbc[1 if n < 2 else 0])
        P3.close()
        P.close()

    def phase_ffn(i, tiles, experts, dff, G_, router=None):
        P = Pool()
        ntl = len(tiles)
        ntok = ntl * 128
        hT = P.sb("fhT", [128, 8, ntok], BF16)
        acc = P.sb("facc", [128, ntl, 1024])
        gates = None
        P2 = Pool()
        pp = [P2.ps("fpp%d" % k, [128, 1024]) for k in range(2)]
        extra = None
        if router is not None:
            gates = P.sb("gates", [128, ntl, 8])
            rw = P2.sb("rw", [128, 8, 8])
            dma(rw, router.rearrange("(kc p) e -> p kc e", p=128))
            h32 = [P2.sb("h32_%d" % k, [128, 8, 128]) for k in range(2)]
            pl = [P2.ps("fpl%d" % k, [128, 8]) for k in range(2)]
            gs = P2.sb("gs", [128, 48])

            def extra(idx, n, p2, Acol, Bcol):
                h = h32[idx % 2]
                for j in range(8):
                    ts(h[:, j, :], p2[:, j * 128:(j + 1) * 128], Acol[:, j:j + 1], Bcol[:, j:j + 1], ALU.mult, ALU.add)
                plg = pl[idx % 2][:, 0:8]
                for j in range(8):
                    mm(plg, h[:, j, :], rw[:, j, :], start=(j == 0), stop=(j == 7))
                lg = gs[:, 0:8]; m8 = gs[:, 8:16]; ex = gs[:, 16:24]; mk = gs[:, 24:32]
                nm1 = gs[:, 32:33]; e2 = gs[:, 33:34]; rd = gs[:, 34:35]
                cp(lg, plg)
                S.op('dve', lambda e: e.max(out=m8, in_=lg), reads=[lg], writes=[m8])
                ts(nm1, m8[:, 0:1], -1.0, None, ALU.mult)
                act(ex, lg, AF.Exp, bias=nm1)
                act(e2, m8[:, 1:2], AF.Exp, bias=nm1)
                ts(mk, lg, m8[:, 1:2], None, ALU.is_ge)
                ts(e2, e2, 1.0, None, ALU.add)
                recip(rd, e2)
                tt(ex, ex, mk, ALU.mult)
                ts(gates[:, idx, :], ex, rd, None, ALU.mult)
        norm_to_hT(P2, i, 'ffn', hT, tiles, pp, extra=extra)
        P2.close()
        P2 = Pool()
        pg = [P2.ps("fpg%d" % k, [128, 512]) for k in range(2)]
        pu = [P2.ps("fpu%d" % k, [128, 512]) for k in range(2)]
        pd = [P2.ps("fpd%d" % k, [128, 1024]) for k in range(2)]
        groups = G_ if isinstance(G_, (list, tuple)) else [G_] * (dff // (G_ * 128))
        GM = max(groups)
        he = P2.sb("he", [128, GM, ntok], BF16)
        sg = [P2.sb("sg%d" % k, [128, 512]) for k in range(2)]
        Wg = [P2.sb("Wg%d" % k, [128, 8, GM * 128], BF16) for k in range(2)]
        Wu = [P2.sb("Wu%d" % k, [128, 8, GM * 128], BF16) for k in range(2)]
        Wd = [P2.sb("Wd%d" % k, [128, GM, 1024], BF16) for k in range(2)]
        blocks = []
        t0 = 0
        while t0 < ntok:
            n = min(512, ntok - t0)
            if t0 == 0 and ntok % 512 != 0:
                n = ntok % 512
            blocks.append((t0, n)); t0 += n
        it = 0
        first = True
        for e_i, (wg, wu, wd) in enumerate(experts):
            c0 = 0
            for Gc in groups:
                b = it % 2
                dma(Wg[b][:, :, 0:Gc * 128], wg[:, c0:c0 + Gc * 128].rearrange("(kc p) n -> p kc n", p=128), eng='pool')
                dma(Wu[b][:, :, 0:Gc * 128], wu[:, c0:c0 + Gc * 128].rearrange("(kc p) n -> p kc n", p=128), eng='pool')
                dma(Wd[b][:, 0:Gc, :], wd[c0:c0 + Gc * 128, :].rearrange("(c p) n -> p c n", p=128), eng='pool')
                c0 += Gc * 128
                k = 0
                for (t0, n) in blocks:
                    for c in range(Gc):
                        pg_ = pg[k % 2]; pu_ = pu[k % 2]; s_ = sg[k % 2]
                        for kc in range(8):
                            mm(pg_[:, 0:n], Wg[b][:, kc, c * 128:(c + 1) * 128], hT[:, kc, t0:t0 + n], start=(kc == 0), stop=(kc == 7))
                        for kc in range(8):
                            mm(pu_[:, 0:n], Wu[b][:, kc, c * 128:(c + 1) * 128], hT[:, kc, t0:t0 + n], start=(kc == 0), stop=(kc == 7))
                        act(s_[:, 0:n], pg_[:, 0:n], AF.Silu)
                        tt(he[:, c, t0:t0 + n], s_[:, 0:n], pu_[:, 0:n], ALU.mult)
                        k += 1
                for idx in range(ntl):
                    pd_ = pd[idx % 2]
                    for hf in range(2):
                        for c in range(Gc):
                            mm(pd_[:, hf * 512:(hf + 1) * 512], he[:, c, idx * 128:(idx + 1) * 128], Wd[b][:, c, hf * 512:(hf + 1) * 512],
                               start=(c == 0), stop=(c == Gc - 1))
                    a = acc[:, idx, :]
                    if gates is None:
                        if first:
                            cp(a, pd_, eng='dve')
                        else:
                            tt(a, pd_, a, ALU.add)
                    else:
                        gcol = gates[:, idx, e_i:e_i + 1]
                        if first:
                            ts(a, pd_, gcol, None, ALU.mult)
                        else:
                            stt(a, pd_, gcol, a, ALU.mult, ALU.add)
                first = False
                it += 1
        P2.close()
        P2 = Pool()
        pp = [P2.ps("fpp2_%d" % k, [128, 1024]) for k in range(2)]
        Gbc = [P2.sb("fGbc%d" % s, [128, 1024]) for s in range(2)]
        for s in range(2):
            make_bc(P2, Gbc[s], coef[i][:, 5, s, :], pp[s])
        rb = res_bufs(P2)
        for idx, n in enumerate(tiles):
            residual_update(P2, rb, idx, n, acc[:, idx, :], Gbc[1 if n < 2 else 0])
        P2.close()
        P.close()

    def phase_s5():
        P = Pool()
        uT = P.sb("uT", [128, 8, TOK], BF16)
        zT = P.sb("zT", [128, 8, 2048], BF16)
        pc = P.sb("s5pc", [128, 2, 3, 32])
        th = P.sb("s5th", [128, 2, 32]); mag = P.sb("s5mag", [128, 2, 32])
        co = P.sb("s5co", [128, 2, 2, 32])
        Bm = P.sb("s5B", [128, 2, 8, 2, 128], BF16)
        Cm = P.sb("s5C", [128, 2, 8, 2, 128], BF16)
        P2 = Pool()
        hT = P2.sb("shT", [128, 8, TOK], BF16)
        W = P2.sb("sW", [128, 8, 1024], BF16)
        for kc in range(8):
            dma(W[:, kc, :], D['od_w_in'][kc * 128:(kc + 1) * 128, :], eng='pool')
        pp = [P2.ps("spp%d" % k, [128, 1024]) for k in range(2)]
        norm_to_hT(P2, 1, 'mix', hT, list(range(NT)), pp)
        S.barrier()
        pa = [P2.ps("spa%d" % k, [128, 512]) for k in range(2)]
        blocks = [(0, 256), (256, 512), (768, 512), (1280, 512), (1792, 512)]
        k = 0
        for c in range(8):
            for (t0, n) in blocks:
                ps = pa[k % 2]
                for kc in range(8):
                    mm(ps[:, 0:n], W[:, kc, c * 128:(c + 1) * 128], hT[:, kc, t0:t0 + n], start=(kc == 0), stop=(kc == 7))
                cp(uT[:, c, t0:t0 + n], ps[:, 0:n], eng=('act' if k % 2 else 'dve'))
                k += 1
        P2.close()
        if upto < 4.2:
            return
        P2 = Pool()
        prm = P2.sb("prm", [32, 2, 3, 128])
        for d in range(2):
            dma(prm[:, d, 0, :], D['od_s5_lambda_re'][d]); dma(prm[:, d, 1, :], D['od_s5_lambda_im'][d])
            lsr = P2.sb("lsr%d" % d, [32, 2])
            dma(lsr, D['od_s5_log_step'][d])
            cp(v3(prm[:, d, 2, :], 64), bc_last(lsr, 64))
        ptp = P2.ps("sptp", [128, 512])
        for d in range(2):
            for q in range(3):
                tr(ptp[:, (d * 3 + q) * 32:(d * 3 + q + 1) * 32], prm[:, d, q, :], ident[0:32, 0:32])
        cp(pc, ptp[:, 0:192].rearrange("p (d q g) -> p d q g", d=2, q=3))
        w_ = [P2.sb("s5w%d" % k, [128, 2, 32]) for k in range(10)]
        lr, li, dt, ar, ai, den, nr, t1, t2, kk = w_
        ts(lr, pc[:, :, 0, :], -1e-4, None, ALU.min)
        cp(li, pc[:, :, 1, :])
        act(dt, pc[:, :, 2, :], AF.Exp)
        tt(t1, lr, dt, ALU.mult)
        act(mag, t1, AF.Exp)
        tt(th, li, dt, ALU.mult)
        ki = P2.sb("s5ki", [128, 2, 32], I32)
        ts(ki, th, 1.0 / (2 * PI), None, ALU.mult)
        cp(kk, ki)
        stt(t1, kk, -2 * PI, th, ALU.mult, ALU.add)
        ts(t1, t1, PI, -PI, ALU.min, ALU.max)
        act(ai, t1, AF.Sin)
        stt(t2, t1, -1.0, t1, ALU.mult, ALU.max)
        act(ar, t2, AF.Sin, bias=PI / 2, scale=-1.0)
        tt(ar, ar, mag, ALU.mult); tt(ai, ai, mag, ALU.mult)
        tt(den, lr, lr, ALU.mult); tt(t1, li, li, ALU.mult); tt(den, den, t1, ALU.add)
        recip(den, den)
        ts(nr, ar, -1.0, None, ALU.add)
        tt(t1, nr, lr, ALU.mult); tt(t2, ai, li, ALU.mult); tt(t1, t1, t2, ALU.add)
        tt(co[:, 0], t1, den, ALU.mult)
        tt(t1, ai, lr, ALU.mult); tt(t2, nr, li, ALU.mult); tt(t1, t1, t2, ALU.subtract)
        tt(co[:, 1], t1, den, ALU.mult)
        bin_ = [[P2.sb("s5bin%d%d" % (a, b), [128, 128]) for b in range(2)] for a in range(2)]
        bb = [[P2.sb("s5bb%d%d" % (a, b), [128, 128]) for b in range(2)] for a in range(2)]
        cin = [[P2.sb("s5cin%d%d" % (a, b), [128, 128]) for b in range(2)] for a in range(2)]
        craw = [[P2.sb("s5craw%d%d" % (a, b), [128, 64]) for b in range(2)] for a in range(2)]
        ptc = [P2.ps("sptc%d" % k, [128, 512]) for k in range(2)]
        for a in range(2):
            for b in range(2):
                memset(bin_[a][b], 0.0)
        it = 0
        for d in range(2):
            for c in range(8):
                q = it % 2
                for ri, key in enumerate(('od_s5_b_re', 'od_s5_b_im')):
                    for pr_ in range(2):
                        src = D[key][d][c * 8 + pr_:c * 8 + 8:2]
                        dst = bin_[q][ri][pr_ * 64:(pr_ + 1) * 64, :].rearrange("n (g two k) -> n g two k", two=2, k=16)[:, :, pr_, :]
                        dma(dst, src.rearrange("g n k -> n g k"))
                cr = co[:, 0, d, 4 * c:4 * c + 4].unsqueeze(2).to_broadcast([128, 4, 32])
                cim = co[:, 1, d, 4 * c:4 * c + 4].unsqueeze(2).to_broadcast([128, 4, 32])
                br_ = v3(bin_[q][0], 32); bi_ = v3(bin_[q][1], 32)
                o_re = v3(bb[q][0], 32); o_im = v3(bb[q][1], 32)
                tA = v3(cin[q][0], 32); tB = v3(cin[q][1], 32)
                tt(tA, br_, cr, ALU.mult); tt(tB, bi_, cim, ALU.mult); tt(o_re, tA, tB, ALU.subtract, eng='pool')
                tt(tA, bi_, cr, ALU.mult); tt(tB, br_, cim, ALU.mult); tt(o_im, tA, tB, ALU.add, eng='pool')
                pt_ = ptc[q]
                tr(pt_[:, 0:128], bb[q][0], ident); tr(pt_[:, 128:256], bb[q][1], ident)
                cp(Bm[:, d, c, :, :], v3(pt_[:, 0:256], 128), eng='act')
                for ri, key in enumerate(('od_s5_c_re', 'od_s5_c_im')):
                    dma(craw[q][ri], D[key][d][c * 128:(c + 1) * 128, :])
                    sgn = 1.0 if ri == 0 else -1.0
                    ts(cin[q][ri][:, 0:64], craw[q][ri], par[:, 0:1], sgn, ALU.mult, ALU.mult)
                    ts(cin[q][ri][:, 64:128], craw[q][ri], par[:, 1:2], sgn, ALU.mult, ALU.mult)
                tr(pt_[:, 256:384], cin[q][0], ident); tr(pt_[:, 384:512], cin[q][1], ident)
                cp(Cm[:, d, c, :, :], v3(pt_[:, 256:512], 128), eng='dve')
                it += 1
        P2.close()
        if upto < 4.3:
            return
        P2 = Pool()
        pos = P2.sb("s5pos", [128, TOK])
        rowmask = P2.sb("s5rm", [128, 4]); colmask = P2.sb("s5cm", [128, 4, 128])
        dma(rowmask, D['rowmask']); dma(colmask, D['colmask'])
        BmM = P2.sb("s5BmM", [128, 2, 128], BF16); CmM = P2.sb("s5CmM", [128, 2, 128], BF16)
        CmN = P2.sb("s5CmN", [128, 128], BF16)
        tht = P2.sb("s5tht", [128, 2, 32])
        ts(tht, th, 1.0 / (2 * PI), None, ALU.mult)
        dcol = colsB[:, 112:120]
        A_ = P2.sb("s5A", [128, TOK]); Y_ = P2.sb("s5Y", [128, TOK]); F1 = P2.sb("s5F1", [128, TOK])
        SINb = [P2.sb("s5SIN%d" % k, [128, TOK], BF16) for k in range(2)]
        COSb = [P2.sb("s5COS%d" % k, [128, TOK], BF16) for k in range(2)]
        BUr = P2.sb("s5BUr", [128, TOK], BF16); BUi = P2.sb("s5BUi", [128, TOK], BF16)
        Mre = P2.sb("s5Mre", [128, TOK], BF16); Mim = P2.sb("s5Mim", [128, TOK], BF16)
        Wre = P2.sb("s5Wre", [128, TOK], BF16); Wim = P2.sb("s5Wim", [128, TOK], BF16)
        T1 = P2.sb("s5T1", [128, TOK], BF16); T2 = P2.sb("s5T2", [128, TOK], BF16)
        T3 = P2.sb("s5T3", [128, TOK], BF16); T4 = P2.sb("s5T4", [128, TOK], BF16)
        Sre = P2.sb("s5Sre0", [128, 2048], BF16); Sim = P2.sb("s5Sim0", [128, 2048], BF16)
        pdr = [P2.ps("spdr%d" % k, [128, 512]) for k in range(4)]
        py = [P2.ps("spy%d" % k, [128, 512]) for k in range(4)]
        gl = [P2.sb("s5gl%d" % k, [128, 512]) for k in range(3)]
        blocks = [(0, 256), (256, 512), (768, 512), (1280, 512), (1792, 512)]
        MAGIC = 12582912.0

        def rev(ap, n):
            return bass.AP(ap.tensor, int(ap.offset) + n - 1, [list(ap.ap[0]), [-1, n]])

        tiles = [(c, d, m) for c in range(8) for d in range(2) for m in range(4)]

        S5X = ""

        def tablesA(k):
            c, d, m = tiles[k]
            if S5X == "notab" and k > 1:
                return
            if m == 0:
                dma(pos, D['pos'][:, d, :])
            thc = tht[:, d, 4 * c + m:4 * c + m + 1]
            act(A_, pos, AF.Identity, scale=thc)
            act(Y_, A_, AF.Identity, bias=MAGIC)

        def tablesB(k):
            SIN = SINb[k % 2]; COS = COSb[k % 2]
            if S5X == "notab" and k > 1:
                return
            stt(F1, Y_, -MAGIC, A_, ALU.add, ALU.subtract)
            act(SIN, F1, AF.Sin, scale=-6.283185)
            act(A_, F1, AF.Abs)
            act(COS, A_, AF.Sin, scale=-6.283185, bias=PI / 2)

        def drive(k):
            c, d, m = tiles[k]
            act(BmM, Bm[:, d, c, :, :], AF.Identity, scale=rowmask[:, m:m + 1])
            tt(CmM, Cm[:, d, c, :, :], bc_mid(colmask[:, m, :], 2), ALU.mult, eng='pool')
            ts(CmN, CmM[:, 0, :], -1.0, None, ALU.mult, eng='pool')
            for bi, (t0, n) in enumerate(blocks):
                pr_ = pdr[(bi % 2) * 2]; pi_ = pdr[(bi % 2) * 2 + 1]
                mm(pr_[:, 0:n], BmM[:, 0, :], uT[:, c, t0:t0 + n])
                mm(pi_[:, 0:n], BmM[:, 1, :], uT[:, c, t0:t0 + n])
                cp(BUr[:, t0:t0 + n], pr_[:, 0:n], eng='act')
                cp(BUi[:, t0:t0 + n], pi_[:, 0:n], eng='act')

        def stage2(k):
            c, d, m = tiles[k]
            SIN = SINb[k % 2]; COS = COSb[k % 2]
            rcol = mag[:, d, 4 * c + m:4 * c + m + 1]
            if not (S5X == "nomod" and k > 1):
                tt(T1, BUr, COS, ALU.mult); tt(T2, BUi, SIN, ALU.mult); tt(Mre, T1, T2, ALU.add)
                tt(T3, BUi, COS, ALU.mult); tt(T4, BUr, SIN, ALU.mult); tt(Mim, T3, T4, ALU.subtract)
            for (M_, W_) in ((Mre, Wre), (Mim, Wim)):
                for (t0, n) in ((0, 256), (256, 2048)):
                    if S5X == "noscan" and k > 1:
                        continue
                    if d == 0:
                        o_ = W_[:, t0:t0 + n]; i_ = M_[:, t0:t0 + n]
                        init = 0.0 if t0 == 0 else W_[:, 255:256]
                    else:
                        o_ = rev(W_[:, t0:t0 + n], n); i_ = rev(M_[:, t0:t0 + n], n)
                        init = 0.0 if t0 == 0 else W_[:, 0:1]
                    d0 = rcol.to_broadcast([128, n])
                    rd = [M_[:, t0:t0 + n], rcol] + ([init] if t0 else [])
                    S.op('dve', lambda e, o_=o_, i_=i_, d0=d0, init=init: e.tensor_tensor_scan(
                        out=o_, data0=d0, data1=i_, initial=init, op0=ALU.mult, op1=ALU.add),
                        reads=rd, writes=[W_[:, t0:t0 + n]])
            wl = Wre[:, 256:TOK]; wi = Wim[:, 256:TOK]; cl = COS[:, 256:TOK]; sl = SIN[:, 256:TOK]
            a1 = T1[:, 0:2048]; a2 = T2[:, 0:2048]; a3 = T3[:, 0:2048]; a4 = T4[:, 0:2048]
            a1 = Sre; a3 = Sim
            tt(a1, wl, cl, ALU.mult); tt(a2, wi, sl, ALU.mult)
            tt(a3, wl, sl, ALU.mult); tt(a4, wi, cl, ALU.mult)
            for b in range(4):
                bs = slice(b * 512, (b + 1) * 512)
                mm(py[b], CmM[:, 0, :], a1[:, bs], start=(d == 0 and m == 0), stop=False)
                mm(py[b], CmN, a2[:, bs], start=False, stop=False)
                mm(py[b], CmM[:, 1, :], a3[:, bs], start=False, stop=False)
                mm(py[b], CmM[:, 1, :], a4[:, bs], start=False, stop=(d == 1 and m == 3))
            if d == 1 and m == 3:
                for b in range(4):
                    y = gl[0]; a = gl[1]; b_ = gl[2]
                    stt(y, uT[:, c, 256 + b * 512:256 + (b + 1) * 512], dcol[:, c:c + 1], py[b], ALU.mult, ALU.add)
                    act(a, y, AF.Square)
                    ts(a, a, 0.044715, 1.0, ALU.mult, ALU.add)
                    tt(a, a, y, ALU.mult)
                    act(b_, a, AF.Sigmoid, scale=2.0 * math.sqrt(2.0 / PI))
                    tt(zT[:, c, b * 512:(b + 1) * 512], b_, y, ALU.mult)

        tablesA(0); tablesB(0)
        for k in range(len(tiles)):
            c, d, m = tiles[k]
            last_of_chunk = False
            if k + 1 < len(tiles) and not last_of_chunk:
                tablesA(k + 1)
            drive(k)
            if k + 1 < len(tiles) and not last_of_chunk:
                tablesB(k + 1)
            stage2(k)
            if k + 1 < len(tiles) and last_of_chunk:
                tablesA(k + 1); tablesB(k + 1)
        P2.close()
        if upto < 4.4:
            return
        P2 = Pool()
        Wa = P2.sb("Wa", [128, 8, 1024], BF16); Wb = P2.sb("Wb", [128, 8, 1024], BF16)
        for kc in range(8):
            dma(Wa[:, kc, :], D['od_glu_w_a'][kc * 128:(kc + 1) * 128, :], eng='pool')
            dma(Wb[:, kc, :], D['od_glu_w_b'][kc * 128:(kc + 1) * 128, :], eng='pool')
        ppa = [P2.ps("gpa%d" % k, [128, 1024]) for k in range(2)]
        ppb = [P2.ps("gpb%d" % k, [128, 1024]) for k in range(2)]
        Gbc = P2.sb("gGbc", [128, 1024])
        make_bc(P2, Gbc, coef[1][:, 2, 0, :], ppa[0])
        sgm = [P2.sb("gsg%d" % k, [128, 1024]) for k in range(2)]
        go = [P2.sb("ggo%d" % k, [128, 1024]) for k in range(2)]
        rb = res_bufs(P2)
        for idx in range(16):
            pa_ = ppa[idx % 2]; pb_ = ppb[idx % 2]
            for (pp_, Wx) in ((pa_, Wa), (pb_, Wb)):
                for hf in range(2):
                    for j in range(8):
                        mm(pp_[:, hf * 512:(hf + 1) * 512], zT[:, j, idx * 128:(idx + 1) * 128], Wx[:, j, hf * 512:(hf + 1) * 512],
                           start=(j == 0), stop=(j == 7))
            act(sgm[idx % 2], pb_, AF.Sigmoid)
            tt(go[idx % 2], pa_, sgm[idx % 2], ALU.mult)
            residual_update(P2, rb, idx, 2 + idx, go[idx % 2], Gbc)
        P2.close()
        P.close()

    if upto >= 1:
        phase_filters()
    if upto >= 2:
        phase_ada()
    if upto > 2:
        phase_even_mixer()
    if upto >= 4:
        phase_ffn(0, list(range(NT)), [(D['ev_ffn_w_gate'][0], D['ev_ffn_w_up'][0], D['ev_ffn_w_down'][0])], 2816, [5, 5, 4, 4, 4])
    if upto > 4:
        phase_s5()
    if upto >= 6:
        phase_ffn(1, list(range(2, NT)),
                  [(D['od_moe_w_gate'][e], D['od_moe_w_up'][e], D['od_moe_w_down'][e]) for e in range(8)],
                  3584, 4, router=D['od_router'])
    S.barrier()
    dma(xs_out, xs)
    S.barrier()
    S.emit()
    return nc


_NC = {}


def _prep_inputs(inputs, b):
    g = lambda k: np.ascontiguousarray(np.asarray(inputs[k], dtype=np.float32))
    m = {}
    m['x'] = g('x')[b]; m['c'] = g('c')[b]; m['ctx'] = g('ctx')[b]; m['c_ctx'] = g('c_ctx')
    for k in ('ada_w', 'ada_b', 'norm_mix_pre', 'norm_mix_post', 'norm_ffn_pre', 'norm_ffn_post',
              'ev_ffn_w_gate', 'ev_ffn_w_up', 'ev_ffn_w_down'):
        m[k] = g(k)
    for k in ('ev_w_in', 'ev_hy_conv_w', 'ev_hy_conv_b', 'ev_hy_f_w1', 'ev_hy_f_b1', 'ev_hy_f_w2', 'ev_hy_f_b2',
              'ev_hy_f_wout', 'ev_hy_freq', 'ev_q_norm', 'ev_k_norm', 'ev_w_out', 'od_w_in', 'od_s5_d',
              'od_glu_w_a', 'od_glu_w_b', 'od_router', 'od_moe_w_gate', 'od_moe_w_up', 'od_moe_w_down',
              'od_s5_b_re', 'od_s5_b_im'):
        m[k] = g(k)[0]
    m['ev_hy_skip'] = g('ev_hy_skip')[0].reshape(1024)
    m['od_s5_lambda_re'] = g('od_s5_lambda_re')[0].reshape(2, 32, 128)
    m['od_s5_lambda_im'] = g('od_s5_lambda_im')[0].reshape(2, 32, 128)
    m['od_s5_log_step'] = g('od_s5_log_step')[0].reshape(2, 32, 2)
    m['od_s5_c_re'] = g('od_s5_c_re')[0].reshape(2, 1024, 64)
    m['od_s5_c_im'] = g('od_s5_c_im')[0].reshape(2, 1024, 64)
    return m


def kernel(_upto=99, _cores=8, **inputs):
    if _upto not in _NC:
        _NC[_upto] = build(_upto)
    nc = _NC[_upto]
    consts = _consts()
    shared = None
    in_maps = []
    for b in range(_cores):
        m = _prep_inputs(inputs, b)
        if shared is None:
            shared = {k: v for k, v in m.items() if k not in ('x', 'c', 'ctx')}
        else:
            for k in shared:
                m[k] = shared[k]
        m.update(consts)
        in_maps.append(m)
    res = run_bass_kernel_spmd(nc, in_maps, core_ids=list(range(_cores)))
    outs = [np.asarray(r["xs"], dtype=np.float32) for r in res.results]
    if _upto < 99:
        return np.stack(outs, axis=0)
    return np.stack([o[256:] for o in outs], axis=0).astype(np.float32)
```

```python
import math
from contextlib import ExitStack
import numpy as np
import ml_dtypes
import concourse.bass as bass
import concourse.mybir as mybir
from concourse.bass_utils import run_bass_kernel_spmd

F32 = mybir.dt.float32
BF16 = mybir.dt.bfloat16
I32 = mybir.dt.int32
AF = mybir.ActivationFunctionType
ALU = mybir.AluOpType
AX = mybir.AxisListType
EPS = 1e-6
PI = math.pi
NT = 18
TOK = 2304


def _prod(xs):
    r = 1
    for x in xs:
        r *= int(x)
    return r


class Sched:
    ENG = ['pe', 'act', 'dve', 'pool', 'sp']

    def __init__(self, nc):
        self.nc = nc
        self.ops = {e: [] for e in self.ENG}
        self.seq = {e: 0 for e in self.ENG}
        self.sems = {e: nc.alloc_semaphore("sem_" + e) for e in self.ENG}
        self.dma_sems = {}
        self.dma_cnt = {}
        self.dma_slot = {}
        self.free_slots = []
        self.slot_cls = {}
        self.nslots = 0
        self.waited = {e: {} for e in self.ENG}
        self.recs = {}

    def _region(self, ap):
        t = ap.tensor
        name = ap.name
        pairs = [(int(s), int(c)) for s, c in ap.ap]
        off = int(ap.offset)
        if 'DRAM' in str(ap.space).upper():
            lo = hi = off
            for s, c in pairs:
                if s >= 0:
                    hi += s * (c - 1)
                else:
                    lo += s * (c - 1)
            return name, 0, 1, lo, hi
        rowsize = _prod(list(t.shape)[1:])
        p0 = off // rowsize
        f0 = off % rowsize
        ps, pc = pairs[0]
        if ps == 0:
            pc = 1
        lo = hi = f0
        for s, c in pairs[1:]:
            if s >= 0:
                hi += s * (c - 1)
            else:
                lo += s * (c - 1)
        if 'PSUM' in str(ap.space).upper():
            epb = 2048 // (2 if ap.dtype == BF16 else 4)
            lo = (lo // epb) * epb
            hi = (hi // epb + 1) * epb - 1
            q0 = (p0 // 32) * 32
            q1 = ((p0 + pc + 31) // 32) * 32
            return name, q0, q1, lo, hi
        return name, p0, p0 + pc, lo, hi

    def _deps_and_update(self, eng, tok, reads, writes):
        deps = []
        for ap in reads:
            name, p0, p1, f0, f1 = self._region(ap)
            lst = self.recs.setdefault(name, [])
            is_psum = 'PSUM' in str(ap.space).upper()
            for r in lst:
                if r[0] < p1 and p0 < r[1] and r[2] <= f1 and f0 <= r[3]:
                    if r[4] == 'w':
                        deps.append(r[5])
                    elif is_psum and r[6] != eng:
                        deps.append(r[5])
            found = False
            for i, r in enumerate(lst):
                if r[4] == 'r' and r[6] == eng and r[0] == p0 and r[1] == p1 and r[2] == f0 and r[3] == f1:
                    lst[i] = (p0, p1, f0, f1, 'r', tok, eng)
                    found = True
                    break
            if not found:
                lst.append((p0, p1, f0, f1, 'r', tok, eng))
        for ap in writes:
            name, p0, p1, f0, f1 = self._region(ap)
            lst = self.recs.setdefault(name, [])
            keep = []
            for r in lst:
                ov = r[0] < p1 and p0 < r[1] and r[2] <= f1 and f0 <= r[3]
                if ov:
                    if r[5] == tok:
                        keep.append(r)
                        continue
                    deps.append(r[5])
                    contained = r[0] >= p0 and r[1] <= p1 and r[2] >= f0 and r[3] <= f1
                    if not contained:
                        keep.append(r)
                else:
                    keep.append(r)
            keep.append((p0, p1, f0, f1, 'w', tok, eng))
            self.recs[name] = keep
        return deps

    def _resolve_waits(self, eng, deps):
        waits = []
        for d in deps:
            if d[0] == 'dma':
                key = d[1]
                val = 16 * self.dma_cnt[key]
                sem = self.dma_sems[key]
                wk = ('dma', key)
            else:
                e2, val = d
                if e2 == 'pe' and eng == 'pe':
                    continue
                sem = self.sems[e2]
                wk = e2
            if self.waited[eng].get(wk, 0) >= val:
                continue
            self.waited[eng][wk] = val
            waits.append((sem, val))
        return waits

    def op(self, eng, fn, reads=(), writes=()):
        self.seq[eng] += 1
        tok = (eng, self.seq[eng])
        deps = self._deps_and_update(eng, tok, list(reads), list(writes))
        waits = self._resolve_waits(eng, deps)
        self.ops[eng].append((waits, fn, self.sems[eng], 1))

    def dma(self, eng, out, in_, key=None, **kw):
        if key is None:
            key = out.name if 'DRAM' not in str(out.space).upper() else 'st_' + in_.name
        cls = 'sw' if eng == 'pool' else 'hw'
        key = (key, cls)
        if key not in self.dma_slot:
            fl = [x for x in self.free_slots if self.slot_cls[x] == cls]
            if fl:
                slot = fl[0]
                self.free_slots.remove(slot)
            else:
                slot = self.nslots
                self.nslots += 1
                self.dma_sems[slot] = self.nc.alloc_semaphore("dsem_%d" % slot)
                self.dma_cnt[slot] = 0
                self.slot_cls[slot] = cls
            self.dma_slot[key] = slot
        key = self.dma_slot[key]
        tok = ('dma', key)
        deps = self._deps_and_update(eng, tok, [in_], [out])
        if any(d == tok for d in deps):
            deps = [d for d in deps if d != tok] + [tok]
        waits = self._resolve_waits(eng, deps)
        self.dma_cnt[key] += 1
        fn = (lambda e, out=out, in_=in_, kw=kw: e.dma_start(out=out, in_=in_, **kw))
        self.ops[eng].append((waits, fn, self.dma_sems[key], 16))

    def barrier(self):
        for eng in self.ENG:
            deps = [(e2, self.seq[e2]) for e2 in self.ENG if self.seq[e2] > 0 and e2 != eng]
            deps += [('dma', k) for k in self.dma_sems if self.dma_cnt[k] > 0]
            waits = self._resolve_waits(eng, deps)
            if waits:
                self.ops[eng].append((waits, None, None, 0))
        self.recs = {}
        self.free_slots = sorted(set(self.free_slots) | set(self.dma_slot.values()), reverse=True)
        self.dma_slot = {}

    def emit(self):
        nc = self.nc
        ops = self.ops

        def run(engine, lst):
            for waits, fn, sem, inc in lst:
                for s, v in waits:
                    engine.wait_ge(s, v)
                if fn is not None:
                    ins = fn(engine)
                    ins.then_inc(sem, inc)

        with nc.Block() as block:
            @block.tensor
            def _(e):
                run(e, ops['pe'])

            @block.scalar
            def _(e):
                run(e, ops['act'])

            @block.vector
            def _(e):
                run(e, ops['dve'])

            @block.gpsimd
            def _(e):
                run(e, ops['pool'])

            @block.sync
            def _(e):
                run(e, ops['sp'])


_CONSTS = None


def _bf(a):
    return np.ascontiguousarray(a.astype(np.float32)).astype(ml_dtypes.bfloat16)


def _consts():
    global _CONSTS
    if _CONSTS is not None:
        return _CONSTS
    c = {}
    c['ident'] = np.eye(128, dtype=np.float32)
    c['ones'] = np.ones((128, 128), np.float32)
    par = np.zeros((128, 2), np.float32)
    for p in range(128):
        par[p, (p // 16) % 2] = 1.0
    c['par'] = par
    rm = np.zeros((128, 4), np.float32)
    cm = np.zeros((128, 4, 128), np.float32)
    for m in range(4):
        rm[32 * m:32 * m + 32, m] = 1.0
        cm[:, m, 32 * m:32 * m + 32] = 1.0
    c['rowmask'] = rm
    c['colmask'] = cm
    for nm, L in (('l', 2048), ('c', 256)):
        N = 2 * L
        nt = L // 128
        t = np.arange(L, dtype=np.float64)
        f = np.arange(L, dtype=np.float64) + 0.5
        ang = 2.0 * np.pi * np.outer(t, f) / N
        C = np.cos(ang)
        Sn = np.sin(ang)
        c['dfc_' + nm] = _bf(C.reshape(nt, 128, nt, 128).transpose(2, 1, 0, 3))
        c['dfs_' + nm] = _bf(Sn.reshape(nt, 128, nt, 128).transpose(2, 1, 0, 3))
        sc = 2.0 / N
        c['dic_' + nm] = _bf((sc * C).reshape(nt, 128, nt, 128).transpose(0, 3, 2, 1))
        c['dis_' + nm] = _bf((sc * Sn).reshape(nt, 128, nt, 128).transpose(0, 3, 2, 1))
        tl = np.linspace(0.0, 1.0, L, dtype=np.float32)[:, None]
        bands = np.linspace(1e-4, 15, 16, dtype=np.float32)
        phase = (np.float32(2.0 * math.pi / L) * np.arange(L, dtype=np.float32)[:, None]) * bands
        z = np.concatenate([tl, np.cos(phase), -np.sin(phase)], axis=-1).astype(np.float32)
        c['zT_' + nm] = np.ascontiguousarray(z.T)
        slow = -math.log(1e-2) / 1.5
        fast = -math.log(1e-2) / 0.3
        deltas = np.linspace(slow, fast, 512, dtype=np.float32)
        dec = np.exp(-tl * deltas).astype(np.float32)
        c['dec_' + nm] = np.ascontiguousarray(dec.reshape(nt, 128, 512).transpose(1, 0, 2))
    rows = 2048 // 64
    row = np.repeat(np.arange(rows, dtype=np.float32), 64)
    col = np.tile(np.arange(64, dtype=np.float32), rows)
    inv = (10000.0 ** (-np.arange(16, dtype=np.float32) / 16)).astype(np.float32)
    ang = np.concatenate([row[:, None] * inv, col[:, None] * inv], axis=-1)
    c['ropec'] = np.ascontiguousarray(np.cos(ang).astype(np.float32).reshape(16, 128, 32).transpose(1, 0, 2))
    c['ropes'] = np.ascontiguousarray(np.sin(ang).astype(np.float32).reshape(16, 128, 32).transpose(1, 0, 2))
    posf = np.arange(TOK, dtype=np.float32)
    posb = np.concatenate([255.0 - np.arange(256), 256.0 + 2047.0 - np.arange(2048)]).astype(np.float32)
    c['pos'] = np.ascontiguousarray(np.stack([np.tile(posf, (128, 1)), np.tile(posb, (128, 1))], axis=1))
    _CONSTS = c
    return c


_CONST_DT = {'dfc_l': BF16, 'dfs_l': BF16, 'dic_l': BF16, 'dis_l': BF16,
             'dfc_c': BF16, 'dfs_c': BF16, 'dic_c': BF16, 'dis_c': BF16}

_IN_SHAPES = {
    'x': [2048, 1024], 'c': [1024], 'ctx': [256, 1024], 'c_ctx': [1024],
    'ada_w': [2, 1024, 6144], 'ada_b': [2, 6144],
    'norm_mix_pre': [2, 1024], 'norm_mix_post': [2, 1024], 'norm_ffn_pre': [2, 1024], 'norm_ffn_post': [2, 1024],
    'ev_w_in': [1024, 2304], 'ev_hy_conv_w': [3, 1536], 'ev_hy_conv_b': [1536],
    'ev_hy_f_w1': [33, 64], 'ev_hy_f_b1': [64], 'ev_hy_f_w2': [64, 64], 'ev_hy_f_b2': [64],
    'ev_hy_f_wout': [64, 2048], 'ev_hy_freq': [64], 'ev_hy_skip': [1024],
    'ev_q_norm': [64], 'ev_k_norm': [64], 'ev_w_out': [1024, 1024],
    'ev_ffn_w_gate': [1, 1024, 2816], 'ev_ffn_w_up': [1, 1024, 2816], 'ev_ffn_w_down': [1, 2816, 1024],
    'od_w_in': [1024, 1024], 'od_s5_lambda_re': [2, 32, 128], 'od_s5_lambda_im': [2, 32, 128],
    'od_s5_log_step': [2, 32, 2], 'od_s5_b_re': [2, 64, 64, 16], 'od_s5_b_im': [2, 64, 64, 16],
    'od_s5_c_re': [2, 1024, 64], 'od_s5_c_im': [2, 1024, 64], 'od_s5_d': [1024],
    'od_glu_w_a': [1024, 1024], 'od_glu_w_b': [1024, 1024], 'od_router': [1024, 8],
    'od_moe_w_gate': [8, 1024, 3584], 'od_moe_w_up': [8, 1024, 3584], 'od_moe_w_down': [8, 3584, 1024],
}


def build(upto=99):
    nc = bass.Bass("TRN2", target_bir_lowering=False)
    S = Sched(nc)
    D = {}
    for k, shp in _IN_SHAPES.items():
        D[k] = nc.dram_tensor(k, list(shp), F32, kind="ExternalInput").ap()
    for k, v in _consts().items():
        D[k] = nc.dram_tensor(k, list(v.shape), _CONST_DT.get(k, F32), kind="ExternalInput").ap()
    xs_out = nc.dram_tensor("xs", [TOK, 1024], F32, kind="ExternalOutput").ap()
    xs = nc.dram_tensor("xs_scr", [TOK, 1024], F32, kind="Internal").ap()
    kscr = {nm: nc.dram_tensor("kscr_" + nm, [2, 2, L, 512], BF16, kind="Internal").ap()
            for nm, L in (('l', 2048), ('c', 256))}

    uid = [0]

    class Pool:
        def __init__(self):
            self.st = ExitStack()

        def sb(self, name, shape, dt=F32):
            uid[0] += 1
            t = self.st.enter_context(nc.sbuf_tensor("%s_%d" % (name, uid[0]), list(shape), dt))
            return t.ap()

        def ps(self, name, shape, dt=F32):
            uid[0] += 1
            epb = 2048 // (2 if dt == BF16 else 4)
            shape = [shape[0], ((shape[1] + epb - 1) // epb) * epb]
            t = self.st.enter_context(nc.psum_tensor("%s_%d" % (name, uid[0]), list(shape), dt))
            return t.ap()

        def close(self):
            S.barrier()
            self.st.close()

    def mm(out, lhsT, rhs, start=True, stop=True):
        S.op('pe', lambda e: e.matmul(out, lhsT=lhsT, rhs=rhs, start=start, stop=stop), reads=[lhsT, rhs], writes=[out])

    def tr(out, in_, ident):
        S.op('pe', lambda e: e.transpose(out, in_, ident), reads=[in_, ident], writes=[out])

    def _isap(x):
        return not isinstance(x, (int, float)) and x is not None

    def act(out, in_, func, bias=None, scale=None, accum=None):
        kw = {}
        rd = [in_]
        if bias is not None:
            kw['bias'] = bias
            if _isap(bias):
                rd.append(bias)
        if scale is not None:
            kw['scale'] = scale
            if _isap(scale):
                rd.append(scale)
        wr = [out]
        if accum is not None:
            kw['accum_out'] = accum
            wr.append(accum)
        S.op('act', lambda e: e.activation(out=out, in_=in_, func=func, **kw), reads=rd, writes=wr)

    def ts(out, in0, s1, s2=None, op0=ALU.mult, op1=None, eng='dve'):
        rd = [in0] + [s for s in (s1, s2) if _isap(s)]
        if op1 is None:
            S.op(eng, lambda e: e.tensor_scalar(out=out, in0=in0, scalar1=s1, scalar2=None, op0=op0), reads=rd, writes=[out])
        else:
            S.op(eng, lambda e: e.tensor_scalar(out=out, in0=in0, scalar1=s1, scalar2=s2, op0=op0, op1=op1), reads=rd, writes=[out])

    def tt(out, in0, in1, op, eng='dve'):
        S.op(eng, lambda e: e.tensor_tensor(out=out, in0=in0, in1=in1, op=op), reads=[in0, in1], writes=[out])

    def stt(out, in0, scalar, in1, op0, op1):
        rd = [in0, in1] + ([scalar] if _isap(scalar) else [])
        S.op('dve', lambda e: e.scalar_tensor_tensor(out=out, in0=in0, scalar=scalar, in1=in1, op0=op0, op1=op1), reads=rd, writes=[out])

    def cp(out, in_, eng='dve'):
        if eng == 'act':
            S.op('act', lambda e: e.copy(out=out, in_=in_), reads=[in_], writes=[out])
        else:
            S.op(eng, lambda e: e.tensor_copy(out=out, in_=in_), reads=[in_], writes=[out])

    def recip(out, in_):
        S.op('dve', lambda e: e.reciprocal(out=out, in_=in_), reads=[in_], writes=[out])

    def memset(ap, val, eng='pool'):
        S.op(eng, lambda e: e.memset(ap, val), writes=[ap])

    def dma(out, in_, eng='sp', **kw):
        S.dma(eng, out, in_, **kw)

    def v3(ap, b):
        return ap.rearrange("p (a b) -> p a b", b=b)

    def bc_last(ap, n):
        return ap.unsqueeze(2).to_broadcast([ap.shape[0], ap.shape[1], n])

    def bc_mid(ap, n):
        return ap.unsqueeze(1).to_broadcast([ap.shape[0], n, ap.shape[1]])

    G = Pool()
    ident = G.sb("ident", [128, 128])
    identb = G.sb("identb", [128, 128], BF16)
    ones = G.sb("ones", [128, 128])
    par = G.sb("par", [128, 2])
    colsA = G.sb("colsA", [128, 112])
    colsB = G.sb("colsB", [128, 120])
    coef = [G.sb("coef%d" % i, [128, 6, 2, 8]) for i in range(2)]
    dma(ident, D['ident'])
    dma(ones, D['ones'])
    dma(par, D['par'])
    cp(identb, ident)
    dma(xs[0:256, :], D['ctx'])
    dma(xs[256:TOK, :], D['x'])

    def phase_filters():
        P = Pool()
        w1 = P.sb("fw1", [33, 64]); w2 = P.sb("fw2", [64, 64]); wout = P.sb("fwout", [64, 2048])
        cols = P.sb("fcols", [64, 3]); frb = P.sb("ffrb", [64, 2])
        dma(w1, D['ev_hy_f_w1']); dma(w2, D['ev_hy_f_w2']); dma(wout, D['ev_hy_f_wout'])
        for i, k in enumerate(('ev_hy_f_b1', 'ev_hy_f_b2', 'ev_hy_freq')):
            dma(cols[:, i:i + 1], D[k].rearrange("(p o) -> p o", o=1))
        tt(frb[:, 0:1], cols[:, 0:1], cols[:, 2:3], ALU.mult)
        tt(frb[:, 1:2], cols[:, 1:2], cols[:, 2:3], ALU.mult)
        psm = [P.ps("fps%d" % i, [128, 512]) for i in range(4)]
        for nm, L in (('l', 2048), ('c', 256)):
            nt = L // 128
            zT = P.sb("fzT", [33, L]); h1 = P.sb("fh1", [64, L]); h2 = P.sb("fh2", [64, L])
            tmp = [P.sb("ftmp%d" % i, [64, 512]) for i in range(2)]
            tki = P.sb("ftki", [64, 512], I32); tkf = P.sb("ftkf", [64, 512])
            dec = P.sb("fdec", [128, nt, 512])
            dma(zT, D['zT_' + nm]); dma(dec, D['dec_' + nm])
            for (wm, src, dst, bcol) in ((w1, zT, h1, 0), (w2, h1, h2, 1)):
                for bi, b0 in enumerate(range(0, L, 512)):
                    n = min(512, L - b0)
                    ps = psm[bi % 2]
                    mm(ps[0:64, 0:n], wm, src[:, b0:b0 + n])
                    t_ = tmp[bi % 2]
                    ts(t_[:, 0:n], ps[0:64, 0:n], cols[:, 2:3], frb[:, bcol:bcol + 1], ALU.mult, ALU.add)
                    ts(tki[:, 0:n], t_[:, 0:n], 1.0 / (2 * PI), None, ALU.mult)
                    cp(tkf[:, 0:n], tki[:, 0:n])
                    stt(t_[:, 0:n], tkf[:, 0:n], -2 * PI, t_[:, 0:n], ALU.mult, ALU.add)
                    ts(t_[:, 0:n], t_[:, 0:n], PI, -PI, ALU.min, ALU.max)
                    act(dst[:, b0:b0 + n], t_[:, 0:n], AF.Sin)
            tf = [P.sb("ftf%d" % i, [128, 512]) for i in range(2)]
            tb = [P.sb("ftb%d" % i, [128, 512]) for i in range(2)]
            dft = [[P.sb("fdft%d%d" % (i, j), [128, nt, 128], BF16) for j in range(2)] for i in range(2)]
            kst = [[P.sb("fkst%d%d" % (i, j), [128, 512], BF16) for j in range(2)] for i in range(2)]
            for o in range(2):
                hs = P.sb("fhs", [128, nt, 512], BF16); hd = P.sb("fhd", [128, nt, 512], BF16)
                for t_i in range(nt):
                    pa = psm[0 + (t_i % 2) * 2]; pb = psm[1 + (t_i % 2) * 2]
                    mm(pa, h2[:, t_i * 128:(t_i + 1) * 128], wout[:, o * 512:(o + 1) * 512])
                    mm(pb, h2[:, t_i * 128:(t_i + 1) * 128], wout[:, 1024 + o * 512:1024 + (o + 1) * 512])
                    a = tf[t_i % 2]; b = tb[t_i % 2]
                    tt(a, pa, dec[:, t_i, :], ALU.mult)
                    tt(b, pb, dec[:, t_i, :], ALU.mult)
                    if t_i == 0:
                        memset(b[0:1, :], 0.0, eng='dve')
                    tt(hs[:, t_i, :], a, b, ALU.add, eng='pool')
                    tt(hd[:, t_i, :], a, b, ALU.subtract, eng='pool')
                for fc in range(nt):
                    cf = dft[fc % 2][0]; sf = dft[fc % 2][1]
                    dma(cf, D['dfc_' + nm][fc]); dma(sf, D['dfs_' + nm][fc])
                    pr = psm[(fc % 2) * 2]; pi_ = psm[(fc % 2) * 2 + 1]
                    for tc in range(nt):
                        mm(pr, cf[:, tc, :], hs[:, tc, :], start=(tc == 0), stop=(tc == nt - 1))
                    for tc in range(nt):
                        mm(pi_, sf[:, tc, :], hd[:, tc, :], start=(tc == 0), stop=(tc == nt - 1))
                    kr = kst[fc % 2][0]; ki = kst[fc % 2][1]
                    cp(kr, pr, eng='dve'); cp(ki, pi_, eng='act')
                    dma(kscr[nm][o, 0, fc * 128:(fc + 1) * 128, :], kr, key='kst')
                    dma(kscr[nm][o, 1, fc * 128:(fc + 1) * 128, :], ki, key='kst')
        P.close()

    def phase_ada():
        P = Pool()
        vecA = P.sb("vecA", [112, 128]); vecB = P.sb("vecB", [120, 128])
        dma(vecA[0:8, :], D['c'].rearrange("(r p) -> r p", p=128))
        dma(vecA[8:16, :], D['c_ctx'].rearrange("(r p) -> r p", p=128))
        for i in range(2):
            dma(vecA[16 + 48 * i:64 + 48 * i, :], D['ada_b'][i].rearrange("(r p) -> r p", p=128))
        r0 = 0
        for k in ('norm_mix_pre', 'norm_mix_post', 'norm_ffn_pre', 'norm_ffn_post'):
            dma(vecB[r0:r0 + 16, :], D[k].rearrange("i (r p) -> (i r) p", p=128))
            r0 += 16
        dma(vecB[64:100, :], D['ev_hy_conv_w'].rearrange("k (r p) -> (k r) p", p=128))
        dma(vecB[100:112, :], D['ev_hy_conv_b'].rearrange("(r p) -> r p", p=128))
        dma(vecB[112:120, :], D['od_s5_d'].rearrange("(r p) -> r p", p=128))
        pt = P.ps("apt", [128, 512])
        tr(pt[:, 0:112], vecA, ident[0:112, 0:112])
        cp(colsA, pt[:, 0:112])
        pt2 = P.ps("apt2", [128, 512])
        tr(pt2[:, 0:120], vecB, ident[0:120, 0:120])
        cp(colsB, pt2[:, 0:120])
        sc2 = P.sb("sc2", [128, 8, 2])
        act(sc2[:, :, 0], colsA[:, 0:8], AF.Silu)
        act(sc2[:, :, 1], colsA[:, 8:16], AF.Silu)
        aw = [P.sb("aw%d" % i, [128, 8, 512]) for i in range(3)]
        pm = [P.ps("apm%d" % i, [128, 96]) for i in range(2)]
        prow = [P.ps("aprow%d" % i, [128, 512]) for i in range(2)]
        rows = P.sb("arows", [2, 6144])
        mod = P.sb("mod", [128, 48, 2])
        for i in range(2):
            for nb in range(12):
                a = aw[nb % 3]
                dma(a, D['ada_w'][i][:, nb * 512:(nb + 1) * 512].rearrange("(kc p) n -> p kc n", p=128))
                pr = prow[nb % 2]
                for kc in range(8):
                    mm(pr[0:2, :], sc2[:, kc, :], a[:, kc, :], start=(kc == 0), stop=(kc == 7))
                cp(rows[:, nb * 512:(nb + 1) * 512], pr[0:2, :], eng=('act' if nb % 2 else 'dve'))
            for m in range(48):
                tr(pm[i][:, m * 2:m * 2 + 2], rows[:, m * 128:(m + 1) * 128], ident[0:2, 0:2])
            tt(mod, v3(pm[i][:, 0:96], 2), bc_last(colsA[:, 16 + 48 * i:64 + 48 * i], 2), ALU.add)
            nmp = colsB[:, 0 + 8 * i:8 + 8 * i]; nmpost = colsB[:, 16 + 8 * i:24 + 8 * i]
            nfp = colsB[:, 32 + 8 * i:40 + 8 * i]; nfpost = colsB[:, 48 + 8 * i:56 + 8 * i]
            for s in range(2):
                stt(coef[i][:, 0, s, :], mod[:, 8:16, s], 1.0, nmp, ALU.add, ALU.mult)
                cp(coef[i][:, 1, s, :], mod[:, 0:8, s])
                tt(coef[i][:, 2, s, :], mod[:, 16:24, s], nmpost, ALU.mult)
                stt(coef[i][:, 3, s, :], mod[:, 32:40, s], 1.0, nfp, ALU.add, ALU.mult)
                cp(coef[i][:, 4, s, :], mod[:, 24:32, s])
                tt(coef[i][:, 5, s, :], mod[:, 40:48, s], nfpost, ALU.mult)
        P.close()

    def make_bc(P, dst, col, pp):
        dg = [P.sb("dg%d" % i, [128, 128]) for i in range(2)]
        for j in range(8):
            d = dg[j % 2]
            ts(d, ident, col[:, j:j + 1], None, ALU.mult)
            mm(pp[:, j * 128:(j + 1) * 128], ones, d)
        cp(dst, pp)

    def norm_to_hT(P, i, kind, hT, tiles, pp, extra=None):
        xin = [P.sb("nx%d" % k, [128, 1024]) for k in range(3)]
        xn = [P.sb("nxn%d" % k, [128, 1024]) for k in range(3)]
        junk = P.sb("njunk", [128, 1024])
        st = P.sb("nst", [128, 3 * NT])
        ka = 0 if kind == 'mix' else 3
        DBG = 9
        for idx, n in enumerate(tiles):
            s = 1 if n < 2 else 0
            xt = xin[idx % 3]; xo = xn[idx % 3]; p2 = pp[idx % 2]
            dma(xt, xs[n * 128:(n + 1) * 128, :])
            if DBG < 1:
                continue
            act(junk, xt, AF.Square, accum=st[:, n:n + 1])
            act(st[:, NT + n:NT + n + 1], st[:, n:n + 1], AF.Sqrt, bias=EPS, scale=1.0 / 1024)
            recip(st[:, 2 * NT + n:2 * NT + n + 1], st[:, NT + n:NT + n + 1])
            act(xo, xt, AF.Identity, scale=st[:, 2 * NT + n:2 * NT + n + 1])
            if DBG < 2:
                continue
            for j in range(8):
                tr(p2[:, j * 128:(j + 1) * 128], xo[:, j * 128:(j + 1) * 128], ident)
            if DBG < 4:
                continue
            for j in range(8):
                A = coef[i][:, ka, s, j:j + 1]; B = coef[i][:, ka + 1, s, j:j + 1]
                o = hT[:, j, idx * 128:(idx + 1) * 128]
                if j < 4:
                    ts(o, p2[:, j * 128:(j + 1) * 128], A, B, ALU.mult, ALU.add)
                else:
                    act(o, p2[:, j * 128:(j + 1) * 128], AF.Identity, bias=B, scale=A)
            if extra is not None:
                extra(idx, n, p2, coef[i][:, ka, s, :], coef[i][:, ka + 1, s, :])

    def residual_update(P, bufs, idx, n, src, Gbc):
        xt, tmp, st, junk = bufs
        xt = xt[idx % 3]; tmp = tmp[idx % 3]
        c0 = (idx % 3) * 3
        dma(xt, xs[n * 128:(n + 1) * 128, :])
        act(junk, src, AF.Square, accum=st[:, c0:c0 + 1])
        if upto < 2.82:
            return
        act(st[:, c0 + 1:c0 + 2], st[:, c0:c0 + 1], AF.Sqrt, bias=EPS, scale=1.0 / 1024)
        recip(st[:, c0 + 2:c0 + 3], st[:, c0 + 1:c0 + 2])
        if upto < 2.83:
            return
        stt(tmp, src, st[:, c0 + 2:c0 + 3], Gbc, ALU.mult, ALU.mult)
        if upto < 2.84:
            return
        tt(tmp, tmp, xt, ALU.add, eng='pool')
        if upto < 2.85:
            return
        dma(xs[n * 128:(n + 1) * 128, :], tmp)

    def res_bufs(P):
        return ([P.sb("rx%d" % k, [128, 1024]) for k in range(3)], [P.sb("rt%d" % k, [128, 1024]) for k in range(3)],
                P.sb("rst", [128, 9]), P.sb("rjunk", [128, 1024]))

    def phase_even_mixer():
        P = Pool()
        z_tok = P.sb("z_tok", [128, NT, 1536], BF16)
        y_at = P.sb("y_at", [128, NT, 512], BF16)
        PH = Pool()
        hT = PH.sb("hT", [128, 8, TOK], BF16)
        P3 = Pool()
        pp = [P3.ps("pp%d" % k, [128, 1024]) for k in range(2)]
        norm_to_hT(P3, 0, 'mix', hT, list(range(NT)), pp)
        P3.close()
        if upto < 2.2:
            return
        PQ = Pool()
        QT = PQ.sb("QT", [64, 8, TOK], BF16)
        KT = PQ.sb("KT", [64, 2, TOK], BF16)
        Va = PQ.sb("Va", [128, NT, 2, 65], BF16)
        memset(Va, 1.0)
        P3 = Pool()
        W = P3.sb("Wqkv", [128, 8, 768], BF16)
        for kc in range(8):
            dma(W[:, kc, :], D['ev_w_in'][kc * 128:(kc + 1) * 128, 1536:2304], eng='pool')
        pp = [P3.ps("pp%d" % k, [128, 1024]) for k in range(2)]
        ptb = [P3.ps("ptb%d" % k, [128, 1024], BF16) for k in range(2)]
        gq = P3.sb("gq", [128, 64]); gk = P3.sb("gk", [128, 64])
        dma(gq, D['ev_q_norm'].partition_broadcast(128)); dma(gk, D['ev_k_norm'].partition_broadcast(128))
        qkg = P3.sb("qkg", [128, 10, 64])
        cp(qkg[:, 0:8, :], bc_mid(gq, 8)); cp(qkg[:, 8:10, :], bc_mid(gk, 2))
        ropec = P3.sb("ropec", [128, 16, 32]); ropes = P3.sb("ropes", [128, 16, 32])
        dma(ropec, D['ropec']); dma(ropes, D['ropes'])
        sq2 = [P3.sb("sq%d" % k, [128, 640]) for k in range(2)]; sst2 = [P3.sb("sst%d" % k, [128, 30]) for k in range(2)]
        qn2 = [P3.sb("qn%d" % k, [128, 10, 64]) for k in range(2)]; qr = [P3.sb("qr%d" % k, [128, 10, 64], BF16) for k in range(2)]
        rt2 = [[P3.sb("rt%d_%d" % (k, j), [128, 10, 32]) for k in range(4)] for j in range(2)]
        def stageA(n):
            sq = sq2[n % 2]; sst = sst2[n % 2]; qn = qn2[n % 2]; rt = rt2[n % 2]
            pq = pp[n % 2]
            for kc in range(8):
                mm(pq[:, 0:512], hT[:, kc, n * 128:(n + 1) * 128], W[:, kc, 0:512], start=(kc == 0), stop=(kc == 7))
            for kc in range(8):
                mm(pq[:, 512:768], hT[:, kc, n * 128:(n + 1) * 128], W[:, kc, 512:768], start=(kc == 0), stop=(kc == 7))
            act(sq, pq[:, 0:640], AF.Square)
            S.op('dve', lambda e, o=sst[:, 0:10], i_=v3(sq, 64): e.tensor_reduce(out=o, in_=i_, axis=AX.X, op=ALU.add),
                 reads=[sq], writes=[sst[:, 0:10]])
            act(sst[:, 10:20], sst[:, 0:10], AF.Sqrt, bias=EPS, scale=1.0 / 64)
            recip(sst[:, 20:30], sst[:, 10:20])
            tt(qn, v3(pq[:, 0:640], 64), bc_last(sst[:, 20:30], 64), ALU.mult)
            cp(Va[:, n, :, 0:64], v3(pq[:, 640:768], 64), eng='act')
            q_ = qr[n % 2]
            if n >= 2:
                tt(qn, qn, qkg, ALU.mult)
                cc = bc_mid(ropec[:, n - 2, :], 10); ss_ = bc_mid(ropes[:, n - 2, :], 10)
                x1 = qn[:, :, 0:32]; x2 = qn[:, :, 32:64]
                tt(rt[0], x1, cc, ALU.mult); tt(rt[1], x2, ss_, ALU.mult)
                tt(q_[:, :, 0:32], rt[0], rt[1], ALU.subtract, eng='pool')
                tt(rt[2], x1, ss_, ALU.mult); tt(rt[3], x2, cc, ALU.mult)
                tt(q_[:, :, 32:64], rt[2], rt[3], ALU.add, eng='pool')
            else:
                tt(q_, qn, qkg, ALU.mult)

        def stageB(n):
            q_ = qr[n % 2]
            pt0 = ptb[0]; pt1 = ptb[1]
            for h in range(8):
                tr(pt0[0:64, h * 128:(h + 1) * 128], q_[:, h, :], identb)
            for h in range(2):
                tr(pt1[0:64, h * 128:(h + 1) * 128], q_[:, 8 + h, :], identb)
            cp(QT[:, :, n * 128:(n + 1) * 128], v3(pt0[0:64, :], 128), eng='act')
            cp(KT[:, :, n * 128:(n + 1) * 128], v3(pt1[0:64, 0:256], 128), eng='dve')

        stageA(0)
        for n in range(1, NT):
            stageA(n)
            stageB(n - 1)
        stageB(NT - 1)
        P3.close()
        if upto < 2.3:
            return
        P3 = Pool()
        psc = [P3.ps("psc%d" % k, [128, 512]) for k in range(4)]
        po = [P3.ps("po%d" % k, [128, 512]) for k in range(2)]
        PT = [P3.sb("PT%d" % k, [128, NT, 512], BF16) for k in range(2)]
        rc = P3.sb("rc", [128, 8])
        it = 0
        for h in range(8):
            g = h // 4
            jobs = [(0, 256, [0, 1], 0)] + [(256 + qb * 512, 512, list(range(NT)), 2 + qb * 4) for qb in range(4)]
            for (q0, nq, kcs, tile0) in jobs:
                pt_ = PT[it % 2]
                for kc in kcs:
                    ps = psc[kc % 4]
                    mm(ps[:, 0:nq], KT[:, g, kc * 128:(kc + 1) * 128], QT[:, h, q0:q0 + nq])
                    act(pt_[:, kc, 0:nq], ps[:, 0:nq], AF.Exp, scale=0.125)
                pov = po[it % 2]
                nqt = nq // 128
                for qt in range(nqt):
                    for ki, kc in enumerate(kcs):
                        mm(pov[:, qt * 65:(qt + 1) * 65], pt_[:, kc, qt * 128:(qt + 1) * 128], Va[:, kc, g, :],
                           start=(ki == 0), stop=(ki == len(kcs) - 1))
                pv = v3(pov[:, 0:nqt * 65], 65)
                r_ = rc[:, (it % 2) * 4:(it % 2) * 4 + nqt]
                recip(r_, pv[:, :, 64])
                tt(y_at[:, tile0:tile0 + nqt, h * 64:(h + 1) * 64], pv[:, :, 0:64], bc_last(r_, 64), ALU.mult)
                it += 1
        P3.close()
        PQ.close()
        if upto < 2.4:
            return
        P3 = Pool()
        W = P3.sb("Why", [128, 8, 1536], BF16)
        for kc in range(8):
            dma(W[:, kc, :], D['ev_w_in'][kc * 128:(kc + 1) * 128, 0:1536], eng='pool')
        pa = [P3.ps("pa%d" % k, [128, 512]) for k in range(2)]
        ptb = [P3.ps("ptb%d" % k, [128, 1024], BF16) for k in range(2)]
        pc_c = [P3.sb("pc_c%d" % k, [128, 258]) for k in range(2)]
        pc_l = [P3.sb("pc_l%d" % k, [128, 2050]) for k in range(2)]
        for k in range(2):
            memset(pc_c[k], 0.0); memset(pc_l[k], 0.0)
        zf = P3.sb("zf", [128, TOK])
        zc = [P3.sb("zc%d" % k, [128, TOK], BF16) for k in range(2)]
        blocks = [(0, 256), (256, 512), (768, 512), (1280, 512), (1792, 512)]
        for c in range(12):
            pcc = pc_c[c % 2]; pcl = pc_l[c % 2]
            for bi, (t0, n) in enumerate(blocks):
                ps = pa[bi % 2]
                for kc in range(8):
                    mm(ps[:, 0:n], W[:, kc, c * 128:(c + 1) * 128], hT[:, kc, t0:t0 + n], start=(kc == 0), stop=(kc == 7))
                dst = pcc[:, 1:257] if t0 == 0 else pcl[:, 1 + t0 - 256:1 + t0 - 256 + n]
                cp(dst, ps[:, 0:n], eng='act')
            w0 = colsB[:, 64 + c:65 + c]; w1c = colsB[:, 76 + c:77 + c]; w2c = colsB[:, 88 + c:89 + c]
            bcl = colsB[:, 100 + c:101 + c]
            zcc = zc[c % 2]
            for (pc, L, off) in ((pcc, 256, 0), (pcl, 2048, 256)):
                ts(zf[:, off:off + L], pc[:, 1:L + 1], w1c, bcl, ALU.mult, ALU.add)
                stt(zf[:, off:off + L], pc[:, 0:L], w0, zf[:, off:off + L], ALU.mult, ALU.add)
                stt(zcc[:, off:off + L], pc[:, 2:L + 2], w2c, zf[:, off:off + L], ALU.mult, ALU.add)
            for gi, n0 in enumerate((0, 8, 16)):
                cnt = min(8, NT - n0)
                pt = ptb[gi % 2]
                for k in range(cnt):
                    tr(pt[:, k * 128:(k + 1) * 128], zcc[:, (n0 + k) * 128:(n0 + k + 1) * 128], identb)
                cp(z_tok[:, n0:n0 + cnt, c * 128:(c + 1) * 128], v3(pt[:, 0:cnt * 128], 128), eng=('act' if gi % 2 else 'dve'))
        P3.close()
        PH.close()
        if upto < 2.5:
            return
        P3 = Pool()
        skip = P3.sb("skip", [128, 1024])
        dma(skip, D['ev_hy_skip'].partition_broadcast(128))
        psm = [P3.ps("hps%d" % k, [128, 512]) for k in range(6)]
        for nm, L, tile0 in (('l', 2048, 2), ('c', 256, 0)):
            nt = L // 128
            Yr = P3.sb("Yr_" + nm, [128, nt, 512], BF16); Yi = P3.sb("Yi_" + nm, [128, nt, 512], BF16)
            v1 = P3.sb("v1_" + nm, [128, nt, 512], BF16)
            dft = [[P3.sb("hdft%s%d%d" % (nm, i, j), [128, nt, 128], BF16) for j in range(2)] for i in range(2)]
            tm = [P3.sb("htm%s%d" % (nm, i), [128, 512]) for i in range(6)]
            for o in range(2):
                vsrc = (lambda t_: z_tok[:, tile0 + t_, 0:512]) if o == 0 else (lambda t_: v1[:, t_, :])
                vdst = (lambda t_: v1[:, t_, :]) if o == 0 else (lambda t_: z_tok[:, tile0 + t_, 0:512])
                dma(Yr, kscr[nm][o, 0].rearrange("(f p) c -> p f c", p=128))
                dma(Yi, kscr[nm][o, 1].rearrange("(f p) c -> p f c", p=128))
                for fc in range(nt):
                    cf = dft[fc % 2][0]; sf = dft[fc % 2][1]
                    dma(cf, D['dfc_' + nm][fc]); dma(sf, D['dfs_' + nm][fc])
                    pr = psm[(fc % 2) * 2]; pi_ = psm[(fc % 2) * 2 + 1]
                    for tc in range(nt):
                        mm(pr, cf[:, tc, :], vsrc(tc), start=(tc == 0), stop=(tc == nt - 1))
                    for tc in range(nt):
                        mm(pi_, sf[:, tc, :], vsrc(tc), start=(tc == 0), stop=(tc == nt - 1))
                    tt(tm[0], pr, Yr[:, fc, :], ALU.mult); tt(tm[1], pi_, Yi[:, fc, :], ALU.mult)
                    tt(tm[2], pr, Yi[:, fc, :], ALU.mult); tt(tm[3], pi_, Yr[:, fc, :], ALU.mult)
                    tt(Yr[:, fc, :], tm[0], tm[1], ALU.subtract, eng='pool')
                    tt(Yi[:, fc, :], tm[2], tm[3], ALU.add, eng='pool')
                for t_i in range(nt):
                    ci = dft[t_i % 2][0]; si = dft[t_i % 2][1]
                    dma(ci, D['dic_' + nm][t_i]); dma(si, D['dis_' + nm][t_i])
                    py = psm[4 + t_i % 2]
                    for fc in range(nt):
                        mm(py, ci[:, fc, :], Yr[:, fc, :], start=(fc == 0), stop=False)
                    for fc in range(nt):
                        mm(py, si[:, fc, :], Yi[:, fc, :], start=False, stop=(fc == nt - 1))
                    a = tm[4 + t_i % 2]
                    tt(a, vsrc(t_i), skip[:, o * 512:(o + 1) * 512], ALU.mult)
                    tt(a, a, py, ALU.add)
                    tt(vdst(t_i), a, z_tok[:, tile0 + t_i, (o + 1) * 512:(o + 2) * 512], ALU.mult)
        P3.close()
        if upto < 2.6:
            return
        P3 = Pool()
        Wo = P3.sb("Wo", [128, 8, 1024], BF16)
        for kc in range(8):
            dma(Wo[:, kc, :], D['ev_w_out'][kc * 128:(kc + 1) * 128, :], eng='pool')
        pp = [P3.ps("opp%d" % k, [128, 1024]) for k in range(2)]
        ptb = [P3.ps("optb%d" % k, [128, 1024], BF16) for k in range(2)]
        Gbc = [P3.sb("Gbc%d" % s, [128, 1024]) for s in range(2)]
        for s in range(2):
            make_bc(P3, Gbc[s], coef[0][:, 2, s, :], pp[s])
        mixT = [P3.sb("mixT%d" % k, [128, 8, 128], BF16) for k in range(2)]
        rb = res_bufs(P3)
        if upto < 2.7:
            return
        for n in range(NT):
            pt = ptb[n % 2]; mt = mixT[n % 2]
            for j in range(4):
                tr(pt[:, j * 128:(j + 1) * 128], z_tok[:, n, j * 128:(j + 1) * 128], identb)
            for j in range(4):
                tr(pt[:, (4 + j) * 128:(5 + j) * 128], y_at[:, n, j * 128:(j + 1) * 128], identb)
            cp(mt, v3(pt, 128), eng='act')
            po_ = pp[n % 2]
            for hf in range(2):
                for j in range(8):
                    mm(po_[:, hf * 512:(hf + 1) * 512], mt[:, j, :], Wo[:, j, hf * 512:(hf + 1) * 512], start=(j == 0), stop=(j == 7))
            if upto >= 2.8:
                residual_update(P3, rb, n, n, po_, Gbc[1 if n < 2 else 0])
        P3.close()
        P.close()

    def phase_ffn(i, tiles, experts, dff, G_, router=None):
        P = Pool()
        ntl = len(tiles)
        ntok = ntl * 128
        hT = P.sb("fhT", [128, 8, ntok], BF16)
        acc = P.sb("facc", [128, ntl, 1024])
        gates = None
        P2 = Pool()
        pp = [P2.ps("fpp%d" % k, [128, 1024]) for k in range(2)]
        extra = None
        if router is not None:
            gates = P.sb("gates", [128, ntl, 8])
            rw = P2.sb("rw", [128, 8, 8])
            dma(rw, router.rearrange("(kc p) e -> p kc e", p=128))
            h32 = [P2.sb("h32_%d" % k, [128, 8, 128]) for k in range(2)]
            pl = [P2.ps("fpl%d" % k, [128, 8]) for k in range(2)]
            gs = P2.sb("gs", [128, 48])

            def extra(idx, n, p2, Acol, Bcol):
                h = h32[idx % 2]
                for j in range(8):
                    ts(h[:, j, :], p2[:, j * 128:(j + 1) * 128], Acol[:, j:j + 1], Bcol[:, j:j + 1], ALU.mult, ALU.add)
                plg = pl[idx % 2][:, 0:8]
                for j in range(8):
                    mm(plg, h[:, j, :], rw[:, j, :], start=(j == 0), stop=(j == 7))
                lg = gs[:, 0:8]; m8 = gs[:, 8:16]; ex = gs[:, 16:24]; mk = gs[:, 24:32]
                nm1 = gs[:, 32:33]; e2 = gs[:, 33:34]; rd = gs[:, 34:35]
                cp(lg, plg)
                S.op('dve', lambda e: e.max(out=m8, in_=lg), reads=[lg], writes=[m8])
                ts(nm1, m8[:, 0:1], -1.0, None, ALU.mult)
                act(ex, lg, AF.Exp, bias=nm1)
                act(e2, m8[:, 1:2], AF.Exp, bias=nm1)
                ts(mk, lg, m8[:, 1:2], None, ALU.is_ge)
                ts(e2, e2, 1.0, None, ALU.add)
                recip(rd, e2)
                tt(ex, ex, mk, ALU.mult)
                ts(gates[:, idx, :], ex, rd, None, ALU.mult)
        norm_to_hT(P2, i, 'ffn', hT, tiles, pp, extra=extra)
        P2.close()
        P2 = Pool()
        pg = [P2.ps("fpg%d" % k, [128, 512]) for k in range(2)]
        pu = [P2.ps("fpu%d" % k, [128, 512]) for k in range(2)]
        pd = [P2.ps("fpd%d" % k, [128, 1024]) for k in range(2)]
        groups = G_ if isinstance(G_, (list, tuple)) else [G_] * (dff // (G_ * 128))
        GM = max(groups)
        he = P2.sb("he", [128, GM, ntok], BF16)
        sg = [P2.sb("sg%d" % k, [128, 512]) for k in range(2)]
        Wg = [P2.sb("Wg%d" % k, [128, 8, GM * 128], BF16) for k in range(2)]
        Wu = [P2.sb("Wu%d" % k, [128, 8, GM * 128], BF16) for k in range(2)]
        Wd = [P2.sb("Wd%d" % k, [128, GM, 1024], BF16) for k in range(2)]
        blocks = []
        t0 = 0
        while t0 < ntok:
            n = min(512, ntok - t0)
            if t0 == 0 and ntok % 512 != 0:
                n = ntok % 512
            blocks.append((t0, n)); t0 += n
        it = 0
        first = True
        for e_i, (wg, wu, wd) in enumerate(experts):
            c0 = 0
            for Gc in groups:
                b = it % 2
                dma(Wg[b][:, :, 0:Gc * 128], wg[:, c0:c0 + Gc * 128].rearrange("(kc p) n -> p kc n", p=128), eng='pool')
                dma(Wu[b][:, :, 0:Gc * 128], wu[:, c0:c0 + Gc * 128].rearrange("(kc p) n -> p kc n", p=128), eng='pool')
                dma(Wd[b][:, 0:Gc, :], wd[c0:c0 + Gc * 128, :].rearrange("(c p) n -> p c n", p=128), eng='pool')
                c0 += Gc * 128
                k = 0
                for (t0, n) in blocks:
                    for c in range(Gc):
                        pg_ = pg[k % 2]; pu_ = pu[k % 2]; s_ = sg[k % 2]
                        for kc in range(8):
                            mm(pg_[:, 0:n], Wg[b][:, kc, c * 128:(c + 1) * 128], hT[:, kc, t0:t0 + n], start=(kc == 0), stop=(kc == 7))
                        for kc in range(8):
                            mm(pu_[:, 0:n], Wu[b][:, kc, c * 128:(c + 1) * 128], hT[:, kc, t0:t0 + n], start=(kc == 0), stop=(kc == 7))
                        act(s_[:, 0:n], pg_[:, 0:n], AF.Silu)
                        tt(he[:, c, t0:t0 + n], s_[:, 0:n], pu_[:, 0:n], ALU.mult)
                        k += 1
                for idx in range(ntl):
                    pd_ = pd[idx % 2]
                    for hf in range(2):
                        for c in range(Gc):
                            mm(pd_[:, hf * 512:(hf + 1) * 512], he[:, c, idx * 128:(idx + 1) * 128], Wd[b][:, c, hf * 512:(hf + 1) * 512],
                               start=(c == 0), stop=(c == Gc - 1))
                    a = acc[:, idx, :]
                    if gates is None:
                        if first:
                            cp(a, pd_, eng='dve')
                        else:
                            tt(a, pd_, a, ALU.add)
                    else:
                        gcol = gates[:, idx, e_i:e_i + 1]
                        if first:
                            ts(a, pd_, gcol, None, ALU.mult)
                        else:
                            stt(a, pd_, gcol, a, ALU.mult, ALU.add)
                first = False
                it += 1
        P2.close()
        P2 = Pool()
        pp = [P2.ps("fpp2_%d" % k, [128, 1024]) for k in range(2)]
        Gbc = [P2.sb("fGbc%d" % s, [128, 1024]) for s in range(2)]
        for s in range(2):
            make_bc(P2, Gbc[s], coef[i][:, 5, s, :], pp[s])
        rb = res_bufs(P2)
        for idx, n in enumerate(tiles):
            residual_update(P2, rb, idx, n, acc[:, idx, :], Gbc[1 if n < 2 else 0])
        P2.close()
        P.close()

    def phase_s5():
        P = Pool()
        uT = P.sb("uT", [128, 8, TOK], BF16)
        zT = P.sb("zT", [128, 8, 2048], BF16)
        pc = P.sb("s5pc", [128, 2, 3, 32])
        th = P.sb("s5th", [128, 2, 32]); mag = P.sb("s5mag", [128, 2, 32])
        co = P.sb("s5co", [128, 2, 2, 32])
        Bm = P.sb("s5B", [128, 2, 8, 2, 128], BF16)
        Cm = P.sb("s5C", [128, 2, 8, 2, 128], BF16)
        P2 = Pool()
        hT = P2.sb("shT", [128, 8, TOK], BF16)
        W = P2.sb("sW", [128, 8, 1024], BF16)
        for kc in range(8):
            dma(W[:, kc, :], D['od_w_in'][kc * 128:(kc + 1) * 128, :], eng='pool')
        pp = [P2.ps("spp%d" % k, [128, 1024]) for k in range(2)]
        norm_to_hT(P2, 1, 'mix', hT, list(range(NT)), pp)
        S.barrier()
        pa = [P2.ps("spa%d" % k, [128, 512]) for k in range(2)]
        blocks = [(0, 256), (256, 512), (768, 512), (1280, 512), (1792, 512)]
        k = 0
        for c in range(8):
            for (t0, n) in blocks:
                ps = pa[k % 2]
                for kc in range(8):
                    mm(ps[:, 0:n], W[:, kc, c * 128:(c + 1) * 128], hT[:, kc, t0:t0 + n], start=(kc == 0), stop=(kc == 7))
                cp(uT[:, c, t0:t0 + n], ps[:, 0:n], eng=('act' if k % 2 else 'dve'))
                k += 1
        P2.close()
        if upto < 4.2:
            return
        P2 = Pool()
        prm = P2.sb("prm", [32, 2, 3, 128])
        for d in range(2):
            dma(prm[:, d, 0, :], D['od_s5_lambda_re'][d]); dma(prm[:, d, 1, :], D['od_s5_lambda_im'][d])
            lsr = P2.sb("lsr%d" % d, [32, 2])
            dma(lsr, D['od_s5_log_step'][d])
            cp(v3(prm[:, d, 2, :], 64), bc_last(lsr, 64))
        ptp = P2.ps("sptp", [128, 512])
        for d in range(2):
            for q in range(3):
                tr(ptp[:, (d * 3 + q) * 32:(d * 3 + q + 1) * 32], prm[:, d, q, :], ident[0:32, 0:32])
        cp(pc, ptp[:, 0:192].rearrange("p (d q g) -> p d q g", d=2, q=3))
        w_ = [P2.sb("s5w%d" % k, [128, 2, 32]) for k in range(10)]
        lr, li, dt, ar, ai, den, nr, t1, t2, kk = w_
        ts(lr, pc[:, :, 0, :], -1e-4, None, ALU.min)
        cp(li, pc[:, :, 1, :])
        act(dt, pc[:, :, 2, :], AF.Exp)
        tt(t1, lr, dt, ALU.mult)
        act(mag, t1, AF.Exp)
        tt(th, li, dt, ALU.mult)
        ki = P2.sb("s5ki", [128, 2, 32], I32)
        ts(ki, th, 1.0 / (2 * PI), None, ALU.mult)
        cp(kk, ki)
        stt(t1, kk, -2 * PI, th, ALU.mult, ALU.add)
        ts(t1, t1, PI, -PI, ALU.min, ALU.max)
        act(ai, t1, AF.Sin)
        stt(t2, t1, -1.0, t1, ALU.mult, ALU.max)
        act(ar, t2, AF.Sin, bias=PI / 2, scale=-1.0)
        tt(ar, ar, mag, ALU.mult); tt(ai, ai, mag, ALU.mult)
        tt(den, lr, lr, ALU.mult); tt(t1, li, li, ALU.mult); tt(den, den, t1, ALU.add)
        recip(den, den)
        ts(nr, ar, -1.0, None, ALU.add)
        tt(t1, nr, lr, ALU.mult); tt(t2, ai, li, ALU.mult); tt(t1, t1, t2, ALU.add)
        tt(co[:, 0], t1, den, ALU.mult)
        tt(t1, ai, lr, ALU.mult); tt(t2, nr, li, ALU.mult); tt(t1, t1, t2, ALU.subtract)
        tt(co[:, 1], t1, den, ALU.mult)
        bin_ = [[P2.sb("s5bin%d%d" % (a, b), [128, 128]) for b in range(2)] for a in range(2)]
        bb = [[P2.sb("s5bb%d%d" % (a, b), [128, 128]) for b in range(2)] for a in range(2)]
        cin = [[P2.sb("s5cin%d%d" % (a, b), [128, 128]) for b in range(2)] for a in range(2)]
        craw = [[P2.sb("s5craw%d%d" % (a, b), [128, 64]) for b in range(2)] for a in range(2)]
        ptc = [P2.ps("sptc%d" % k, [128, 512]) for k in range(2)]
        for a in range(2):
            for b in range(2):
                memset(bin_[a][b], 0.0)
        it = 0
        for d in range(2):
            for c in range(8):
                q = it % 2
                for ri, key in enumerate(('od_s5_b_re', 'od_s5_b_im')):
                    for pr_ in range(2):
                        src = D[key][d][c * 8 + pr_:c * 8 + 8:2]
                        dst = bin_[q][ri][pr_ * 64:(pr_ + 1) * 64, :].rearrange("n (g two k) -> n g two k", two=2, k=16)[:, :, pr_, :]
                        dma(dst, src.rearrange("g n k -> n g k"))
                cr = co[:, 0, d, 4 * c:4 * c + 4].unsqueeze(2).to_broadcast([128, 4, 32])
                cim = co[:, 1, d, 4 * c:4 * c + 4].unsqueeze(2).to_broadcast([128, 4, 32])
                br_ = v3(bin_[q][0], 32); bi_ = v3(bin_[q][1], 32)
                o_re = v3(bb[q][0], 32); o_im = v3(bb[q][1], 32)
                tA = v3(cin[q][0], 32); tB = v3(cin[q][1], 32)
                tt(tA, br_, cr, ALU.mult); tt(tB, bi_, cim, ALU.mult); tt(o_re, tA, tB, ALU.subtract, eng='pool')
                tt(tA, bi_, cr, ALU.mult); tt(tB, br_, cim, ALU.mult); tt(o_im, tA, tB, ALU.add, eng='pool')
                pt_ = ptc[q]
                tr(pt_[:, 0:128], bb[q][0], ident); tr(pt_[:, 128:256], bb[q][1], ident)
                cp(Bm[:, d, c, :, :], v3(pt_[:, 0:256], 128), eng='act')
                for ri, key in enumerate(('od_s5_c_re', 'od_s5_c_im')):
                    dma(craw[q][ri], D[key][d][c * 128:(c + 1) * 128, :])
                    sgn = 1.0 if ri == 0 else -1.0
                    ts(cin[q][ri][:, 0:64], craw[q][ri], par[:, 0:1], sgn, ALU.mult, ALU.mult)
                    ts(cin[q][ri][:, 64:128], craw[q][ri], par[:, 1:2], sgn, ALU.mult, ALU.mult)
                tr(pt_[:, 256:384], cin[q][0], ident); tr(pt_[:, 384:512], cin[q][1], ident)
                cp(Cm[:, d, c, :, :], v3(pt_[:, 256:512], 128), eng='dve')
                it += 1
        P2.close()
        if upto < 4.3:
            return
        P2 = Pool()
        pos = P2.sb("s5pos", [128, TOK])
        rowmask = P2.sb("s5rm", [128, 4]); colmask = P2.sb("s5cm", [128, 4, 128])
        dma(rowmask, D['rowmask']); dma(colmask, D['colmask'])
        BmM = P2.sb("s5BmM", [128, 2, 128], BF16); CmM = P2.sb("s5CmM", [128, 2, 128], BF16)
        CmN = P2.sb("s5CmN", [128, 128], BF16)
        tht = P2.sb("s5tht", [128, 2, 32])
        ts(tht, th, 1.0 / (2 * PI), None, ALU.mult)
        dcol = colsB[:, 112:120]
        A_ = P2.sb("s5A", [128, TOK]); Y_ = P2.sb("s5Y", [128, TOK]); F1 = P2.sb("s5F1", [128, TOK])
        SINb = [P2.sb("s5SIN%d" % k, [128, TOK], BF16) for k in range(2)]
        COSb = [P2.sb("s5COS%d" % k, [128, TOK], BF16) for k in range(2)]
        BUr = P2.sb("s5BUr", [128, TOK], BF16); BUi = P2.sb("s5BUi", [128, TOK], BF16)
        Mre = P2.sb("s5Mre", [128, TOK], BF16); Mim = P2.sb("s5Mim", [128, TOK], BF16)
        Wre = P2.sb("s5Wre", [128, TOK], BF16); Wim = P2.sb("s5Wim", [128, TOK], BF16)
        T1 = P2.sb("s5T1", [128, TOK], BF16); T2 = P2.sb("s5T2", [128, TOK], BF16)
        T3 = P2.sb("s5T3", [128, TOK], BF16); T4 = P2.sb("s5T4", [128, TOK], BF16)
        Sre = P2.sb("s5Sre0", [128, 2048], BF16); Sim = P2.sb("s5Sim0", [128, 2048], BF16)
        pdr = [P2.ps("spdr%d" % k, [128, 512]) for k in range(4)]
        py = [P2.ps("spy%d" % k, [128, 512]) for k in range(4)]
        gl = [P2.sb("s5gl%d" % k, [128, 512]) for k in range(3)]
        blocks = [(0, 256), (256, 512), (768, 512), (1280, 512), (1792, 512)]
        MAGIC = 12582912.0

        def rev(ap, n):
            return bass.AP(ap.tensor, int(ap.offset) + n - 1, [list(ap.ap[0]), [-1, n]])

        tiles = [(c, d, m) for c in range(8) for d in range(2) for m in range(4)]

        S5X = ""

        def tablesA(k):
            c, d, m = tiles[k]
            if S5X == "notab" and k > 1:
                return
            if m == 0:
                dma(pos, D['pos'][:, d, :])
            thc = tht[:, d, 4 * c + m:4 * c + m + 1]
            act(A_, pos, AF.Identity, scale=thc)
            act(Y_, A_, AF.Identity, bias=MAGIC)

        def tablesB(k):
            SIN = SINb[k % 2]; COS = COSb[k % 2]
            if S5X == "notab" and k > 1:
                return
            stt(F1, Y_, -MAGIC, A_, ALU.add, ALU.subtract)
            act(SIN, F1, AF.Sin, scale=-6.283185)
            act(A_, F1, AF.Abs)
            act(COS, A_, AF.Sin, scale=-6.283185, bias=PI / 2)

        def drive(k):
            c, d, m = tiles[k]
            act(BmM, Bm[:, d, c, :, :], AF.Identity, scale=rowmask[:, m:m + 1])
            tt(CmM, Cm[:, d, c, :, :], bc_mid(colmask[:, m, :], 2), ALU.mult, eng='pool')
            ts(CmN, CmM[:, 0, :], -1.0, None, ALU.mult, eng='pool')
            for bi, (t0, n) in enumerate(blocks):
                pr_ = pdr[(bi % 2) * 2]; pi_ = pdr[(bi % 2) * 2 + 1]
                mm(pr_[:, 0:n], BmM[:, 0, :], uT[:, c, t0:t0 + n])
                mm(pi_[:, 0:n], BmM[:, 1, :], uT[:, c, t0:t0 + n])
                cp(BUr[:, t0:t0 + n], pr_[:, 0:n], eng='act')
                cp(BUi[:, t0:t0 + n], pi_[:, 0:n], eng='act')

        def stage2(k):
            c, d, m = tiles[k]
            SIN = SINb[k % 2]; COS = COSb[k % 2]
            rcol = mag[:, d, 4 * c + m:4 * c + m + 1]
            if not (S5X == "nomod" and k > 1):
                tt(T1, BUr, COS, ALU.mult); tt(T2, BUi, SIN, ALU.mult); tt(Mre, T1, T2, ALU.add)
                tt(T3, BUi, COS, ALU.mult); tt(T4, BUr, SIN, ALU.mult); tt(Mim, T3, T4, ALU.subtract)
            for (M_, W_) in ((Mre, Wre), (Mim, Wim)):
                for (t0, n) in ((0, 256), (256, 2048)):
                    if S5X == "noscan" and k > 1:
                        continue
                    if d == 0:
                        o_ = W_[:, t0:t0 + n]; i_ = M_[:, t0:t0 + n]
                        init = 0.0 if t0 == 0 else W_[:, 255:256]
                    else:
                        o_ = rev(W_[:, t0:t0 + n], n); i_ = rev(M_[:, t0:t0 + n], n)
                        init = 0.0 if t0 == 0 else W_[:, 0:1]
                    d0 = rcol.to_broadcast([128, n])
                    rd = [M_[:, t0:t0 + n], rcol] + ([init] if t0 else [])
                    S.op('dve', lambda e, o_=o_, i_=i_, d0=d0, init=init: e.tensor_tensor_scan(
                        out=o_, data0=d0, data1=i_, initial=init, op0=ALU.mult, op1=ALU.add),
                        reads=rd, writes=[W_[:, t0:t0 + n]])
            wl = Wre[:, 256:TOK]; wi = Wim[:, 256:TOK]; cl = COS[:, 256:TOK]; sl = SIN[:, 256:TOK]
            a1 = T1[:, 0:2048]; a2 = T2[:, 0:2048]; a3 = T3[:, 0:2048]; a4 = T4[:, 0:2048]
            a1 = Sre; a3 = Sim
            tt(a1, wl, cl, ALU.mult); tt(a2, wi, sl, ALU.mult)
            tt(a3, wl, sl, ALU.mult); tt(a4, wi, cl, ALU.mult)
            for b in range(4):
                bs = slice(b * 512, (b + 1) * 512)
                mm(py[b], CmM[:, 0, :], a1[:, bs], start=(d == 0 and m == 0), stop=False)
                mm(py[b], CmN, a2[:, bs], start=False, stop=False)
                mm(py[b], CmM[:, 1, :], a3[:, bs], start=False, stop=False)
                mm(py[b], CmM[:, 1, :], a4[:, bs], start=False, stop=(d == 1 and m == 3))
            if d == 1 and m == 3:
                for b in range(4):
                    y = gl[0]; a = gl[1]; b_ = gl[2]
                    stt(y, uT[:, c, 256 + b * 512:256 + (b + 1) * 512], dcol[:, c:c + 1], py[b], ALU.mult, ALU.add)
                    act(a, y, AF.Square)
                    ts(a, a, 0.044715, 1.0, ALU.mult, ALU.add)
                    tt(a, a, y, ALU.mult)
                    act(b_, a, AF.Sigmoid, scale=2.0 * math.sqrt(2.0 / PI))
                    tt(zT[:, c, b * 512:(b + 1) * 512], b_, y, ALU.mult)

        tablesA(0); tablesB(0)
        for k in range(len(tiles)):
            c, d, m = tiles[k]
            last_of_chunk = False
            if k + 1 < len(tiles) and not last_of_chunk:
                tablesA(k + 1)
            drive(k)
            if k + 1 < len(tiles) and not last_of_chunk:
                tablesB(k + 1)
            stage2(k)
            if k + 1 < len(tiles) and last_of_chunk:
                tablesA(k + 1); tablesB(k + 1)
        P2.close()
        if upto < 4.4:
            return
        P2 = Pool()
        Wa = P2.sb("Wa", [128, 8, 1024], BF16); Wb = P2.sb("Wb", [128, 8, 1024], BF16)
        for kc in range(8):
            dma(Wa[:, kc, :], D['od_glu_w_a'][kc * 128:(kc + 1) * 128, :], eng='pool')
            dma(Wb[:, kc, :], D['od_glu_w_b'][kc * 128:(kc + 1) * 128, :], eng='pool')
        ppa = [P2.ps("gpa%d" % k, [128, 1024]) for k in range(2)]
        ppb = [P2.ps("gpb%d" % k, [128, 1024]) for k in range(2)]
        Gbc = P2.sb("gGbc", [128, 1024])
        make_bc(P2, Gbc, coef[1][:, 2, 0, :], ppa[0])
        sgm = [P2.sb("gsg%d" % k, [128, 1024]) for k in range(2)]
        go = [P2.sb("ggo%d" % k, [128, 1024]) for k in range(2)]
        rb = res_bufs(P2)
        for idx in range(16):
            pa_ = ppa[idx % 2]; pb_ = ppb[idx % 2]
            for (pp_, Wx) in ((pa_, Wa), (pb_, Wb)):
                for hf in range(2):
                    for j in range(8):
                        mm(pp_[:, hf * 512:(hf + 1) * 512], zT[:, j, idx * 128:(idx + 1) * 128], Wx[:, j, hf * 512:(hf + 1) * 512],
                           start=(j == 0), stop=(j == 7))
            act(sgm[idx % 2], pb_, AF.Sigmoid)
            tt(go[idx % 2], pa_, sgm[idx % 2], ALU.mult)
            residual_update(P2, rb, idx, 2 + idx, go[idx % 2], Gbc)
        P2.close()
        P.close()

    if upto >= 1:
        phase_filters()
    if upto >= 2:
        phase_ada()
    if upto > 2:
        phase_even_mixer()
    if upto >= 4:
        phase_ffn(0, list(range(NT)), [(D['ev_ffn_w_gate'][0], D['ev_ffn_w_up'][0], D['ev_ffn_w_down'][0])], 2816, [5, 5, 4, 4, 4])
    if upto > 4:
        phase_s5()
    if upto >= 6:
        phase_ffn(1, list(range(2, NT)),
                  [(D['od_moe_w_gate'][e], D['od_moe_w_up'][e], D['od_moe_w_down'][e]) for e in range(8)],
                  3584, 4, router=D['od_router'])
    S.barrier()
    dma(xs_out, xs)
    S.barrier()
    S.emit()
    return nc


_NC = {}


def _prep_inputs(inputs, b):
    g = lambda k: np.ascontiguousarray(np.asarray(inputs[k], dtype=np.float32))
    m = {}
    m['x'] = g('x')[b]; m['c'] = g('c')[b]; m['ctx'] = g('ctx')[b]; m['c_ctx'] = g('c_ctx')
    for k in ('ada_w', 'ada_b', 'norm_mix_pre', 'norm_mix_post', 'norm_ffn_pre', 'norm_ffn_post',
              'ev_ffn_w_gate', 'ev_ffn_w_up', 'ev_ffn_w_down'):
        m[k] = g(k)
    for k in ('ev_w_in', 'ev_hy_conv_w', 'ev_hy_conv_b', 'ev_hy_f_w1', 'ev_hy_f_b1', 'ev_hy_f_w2', 'ev_hy_f_b2',
              'ev_hy_f_wout', 'ev_hy_freq', 'ev_q_norm', 'ev_k_norm', 'ev_w_out', 'od_w_in', 'od_s5_d',
              'od_glu_w_a', 'od_glu_w_b', 'od_router', 'od_moe_w_gate', 'od_moe_w_up', 'od_moe_w_down',
              'od_s5_b_re', 'od_s5_b_im'):
        m[k] = g(k)[0]
    m['ev_hy_skip'] = g('ev_hy_skip')[0].reshape(1024)
    m['od_s5_lambda_re'] = g('od_s5_lambda_re')[0].reshape(2, 32, 128)
    m['od_s5_lambda_im'] = g('od_s5_lambda_im')[0].reshape(2, 32, 128)
    m['od_s5_log_step'] = g('od_s5_log_step')[0].reshape(2, 32, 2)
    m['od_s5_c_re'] = g('od_s5_c_re')[0].reshape(2, 1024, 64)
    m['od_s5_c_im'] = g('od_s5_c_im')[0].reshape(2, 1024, 64)
    return m


def kernel(_upto=99, _cores=8, **inputs):
    if _upto not in _NC:
        _NC[_upto] = build(_upto)
    nc = _NC[_upto]
    consts = _consts()
    shared = None
    in_maps = []
    for b in range(_cores):
        m = _prep_inputs(inputs, b)
        if shared is None:
            shared = {k: v for k, v in m.items() if k not in ('x', 'c', 'ctx')}
        else:
            for k in shared:
                m[k] = shared[k]
        m.update(consts)
        in_maps.append(m)
    res = run_bass_kernel_spmd(nc, in_maps, core_ids=list(range(_cores)))
    outs = [np.asarray(r["xs"], dtype=np.float32) for r in res.results]
    if _upto < 99:
        return np.stack(outs, axis=0)
    return np.stack([o[256:] for o in outs], axis=0).astype(np.float32)
```

```python
import math
from contextlib import ExitStack
import numpy as np
import ml_dtypes
import concourse.bass as bass
import concourse.mybir as mybir
from concourse.bass_utils import run_bass_kernel_spmd

F32 = mybir.dt.float32
BF16 = mybir.dt.bfloat16
I32 = mybir.dt.int32
AF = mybir.ActivationFunctionType
ALU = mybir.AluOpType
AX = mybir.AxisListType
EPS = 1e-6
PI = math.pi
NT = 18
TOK = 2304


def _prod(xs):
    r = 1
    for x in xs:
        r *= int(x)
    return r


class Sched:
    ENG = ['pe', 'act', 'dve', 'pool', 'sp']

    def __init__(self, nc):
        self.nc = nc
        self.ops = {e: [] for e in self.ENG}
        self.seq = {e: 0 for e in self.ENG}
        self.sems = {e: nc.alloc_semaphore("sem_" + e) for e in self.ENG}
        self.dma_sems = {}
        self.dma_cnt = {}
        self.dma_slot = {}
        self.free_slots = []
        self.slot_cls = {}
        self.nslots = 0
        self.waited = {e: {} for e in self.ENG}
        self.recs = {}

    def _region(self, ap):
        t = ap.tensor
        name = ap.name
        pairs = [(int(s), int(c)) for s, c in ap.ap]
        off = int(ap.offset)
        if 'DRAM' in str(ap.space).upper():
            lo = hi = off
            for s, c in pairs:
                if s >= 0:
                    hi += s * (c - 1)
                else:
                    lo += s * (c - 1)
            return name, 0, 1, lo, hi
        rowsize = _prod(list(t.shape)[1:])
        p0 = off // rowsize
        f0 = off % rowsize
        ps, pc = pairs[0]
        if ps == 0:
            pc = 1
        lo = hi = f0
        for s, c in pairs[1:]:
            if s >= 0:
                hi += s * (c - 1)
            else:
                lo += s * (c - 1)
        if 'PSUM' in str(ap.space).upper():
            epb = 2048 // (2 if ap.dtype == BF16 else 4)
            lo = (lo // epb) * epb
            hi = (hi // epb + 1) * epb - 1
            q0 = (p0 // 32) * 32
            q1 = ((p0 + pc + 31) // 32) * 32
            return name, q0, q1, lo, hi
        return name, p0, p0 + pc, lo, hi

    def _deps_and_update(self, eng, tok, reads, writes):
        deps = []
        for ap in reads:
            name, p0, p1, f0, f1 = self._region(ap)
            lst = self.recs.setdefault(name, [])
            is_psum = 'PSUM' in str(ap.space).upper()
            for r in lst:
                if r[0] < p1 and p0 < r[1] and r[2] <= f1 and f0 <= r[3]:
                    if r[4] == 'w':
                        deps.append(r[5])
                    elif is_psum and r[6] != eng:
                        deps.append(r[5])
            found = False
            for i, r in enumerate(lst):
                if r[4] == 'r' and r[6] == eng and r[0] == p0 and r[1] == p1 and r[2] == f0 and r[3] == f1:
                    lst[i] = (p0, p1, f0, f1, 'r', tok, eng)
                    found = True
                    break
            if not found:
                lst.append((p0, p1, f0, f1, 'r', tok, eng))
        for ap in writes:
            name, p0, p1, f0, f1 = self._region(ap)
            lst = self.recs.setdefault(name, [])
            keep = []
            for r in lst:
                ov = r[0] < p1 and p0 < r[1] and r[2] <= f1 and f0 <= r[3]
                if ov:
                    if r[5] == tok:
                        keep.append(r)
                        continue
                    deps.append(r[5])
                    contained = r[0] >= p0 and r[1] <= p1 and r[2] >= f0 and r[3] <= f1
                    if not contained:
                        keep.append(r)
                else:
                    keep.append(r)
            keep.append((p0, p1, f0, f1, 'w', tok, eng))
            self.recs[name] = keep
        return deps

    def _resolve_waits(self, eng, deps):
        waits = []
        for d in deps:
            if d[0] == 'dma':
                key = d[1]
                val = 16 * self.dma_cnt[key]
                sem = self.dma_sems[key]
                wk = ('dma', key)
            else:
                e2, val = d
                if e2 == 'pe' and eng == 'pe':
                    continue
                sem = self.sems[e2]
                wk = e2
            if self.waited[eng].get(wk, 0) >= val:
                continue
            self.waited[eng][wk] = val
            waits.append((sem, val))
        return waits

    def op(self, eng, fn, reads=(), writes=()):
        self.seq[eng] += 1
        tok = (eng, self.seq[eng])
        deps = self._deps_and_update(eng, tok, list(reads), list(writes))
        waits = self._resolve_waits(eng, deps)
        self.ops[eng].append((waits, fn, self.sems[eng], 1))

    def dma(self, eng, out, in_, key=None, **kw):
        if key is None:
            key = out.name if 'DRAM' not in str(out.space).upper() else 'st_' + in_.name
        cls = 'sw' if eng == 'pool' else 'hw'
        key = (key, cls)
        if key not in self.dma_slot:
            fl = [x for x in self.free_slots if self.slot_cls[x] == cls]
            if fl:
                slot = fl[0]
                self.free_slots.remove(slot)
            else:
                slot = self.nslots
                self.nslots += 1
                self.dma_sems[slot] = self.nc.alloc_semaphore("dsem_%d" % slot)
                self.dma_cnt[slot] = 0
                self.slot_cls[slot] = cls
            self.dma_slot[key] = slot
        key = self.dma_slot[key]
        tok = ('dma', key)
        deps = self._deps_and_update(eng, tok, [in_], [out])
        if any(d == tok for d in deps):
            deps = [d for d in deps if d != tok] + [tok]
        waits = self._resolve_waits(eng, deps)
        self.dma_cnt[key] += 1
        fn = (lambda e, out=out, in_=in_, kw=kw: e.dma_start(out=out, in_=in_, **kw))
        self.ops[eng].append((waits, fn, self.dma_sems[key], 16))

    def barrier(self):
        for eng in self.ENG:
            deps = [(e2, self.seq[e2]) for e2 in self.ENG if self.seq[e2] > 0 and e2 != eng]
            deps += [('dma', k) for k in self.dma_sems if self.dma_cnt[k] > 0]
            waits = self._resolve_waits(eng, deps)
            if waits:
                self.ops[eng].append((waits, None, None, 0))
        self.recs = {}
        self.free_slots = sorted(set(self.free_slots) | set(self.dma_slot.values()), reverse=True)
        self.dma_slot = {}

    def emit(self):
        nc = self.nc
        ops = self.ops

        def run(engine, lst):
            for waits, fn, sem, inc in lst:
                for s, v in waits:
                    engine.wait_ge(s, v)
                if fn is not None:
                    ins = fn(engine)
                    ins.then_inc(sem, inc)

        with nc.Block() as block:
            @block.tensor
            def _(e):
                run(e, ops['pe'])

            @block.scalar
            def _(e):
                run(e, ops['act'])

            @block.vector
            def _(e):
                run(e, ops['dve'])

            @block.gpsimd
            def _(e):
                run(e, ops['pool'])

            @block.sync
            def _(e):
                run(e, ops['sp'])


_CONSTS = None


def _bf(a):
    return np.ascontiguousarray(a.astype(np.float32)).astype(ml_dtypes.bfloat16)


def _consts():
    global _CONSTS
    if _CONSTS is not None:
        return _CONSTS
    c = {}
    c['ident'] = np.eye(128, dtype=np.float32)
    c['ones'] = np.ones((128, 128), np.float32)
    par = np.zeros((128, 2), np.float32)
    for p in range(128):
        par[p, (p // 16) % 2] = 1.0
    c['par'] = par
    rm = np.zeros((128, 4), np.float32)
    cm = np.zeros((128, 4, 128), np.float32)
    for m in range(4):
        rm[32 * m:32 * m + 32, m] = 1.0
        cm[:, m, 32 * m:32 * m + 32] = 1.0
    c['rowmask'] = rm
    c['colmask'] = cm
    for nm, L in (('l', 2048), ('c', 256)):
        N = 2 * L
        nt = L // 128
        t = np.arange(L, dtype=np.float64)
        f = np.arange(L, dtype=np.float64) + 0.5
        ang = 2.0 * np.pi * np.outer(t, f) / N
        C = np.cos(ang)
        Sn = np.sin(ang)
        c['dfc_' + nm] = _bf(C.reshape(nt, 128, nt, 128).transpose(2, 1, 0, 3))
        c['dfs_' + nm] = _bf(Sn.reshape(nt, 128, nt, 128).transpose(2, 1, 0, 3))
        sc = 2.0 / N
        c['dic_' + nm] = _bf((sc * C).reshape(nt, 128, nt, 128).transpose(0, 3, 2, 1))
        c['dis_' + nm] = _bf((sc * Sn).reshape(nt, 128, nt, 128).transpose(0, 3, 2, 1))
        tl = np.linspace(0.0, 1.0, L, dtype=np.float32)[:, None]
        bands = np.linspace(1e-4, 15, 16, dtype=np.float32)
        phase = (np.float32(2.0 * math.pi / L) * np.arange(L, dtype=np.float32)[:, None]) * bands
        z = np.concatenate([tl, np.cos(phase), -np.sin(phase)], axis=-1).astype(np.float32)
        c['zT_' + nm] = np.ascontiguousarray(z.T)
        slow = -math.log(1e-2) / 1.5
        fast = -math.log(1e-2) / 0.3
        deltas = np.linspace(slow, fast, 512, dtype=np.float32)
        dec = np.exp(-tl * deltas).astype(np.float32)
        c['dec_' + nm] = np.ascontiguousarray(dec.reshape(nt, 128, 512).transpose(1, 0, 2))
    rows = 2048 // 64
    row = np.repeat(np.arange(rows, dtype=np.float32), 64)
    col = np.tile(np.arange(64, dtype=np.float32), rows)
    inv = (10000.0 ** (-np.arange(16, dtype=np.float32) / 16)).astype(np.float32)
    ang = np.concatenate([row[:, None] * inv, col[:, None] * inv], axis=-1)
    c['ropec'] = np.ascontiguousarray(np.cos(ang).astype(np.float32).reshape(16, 128, 32).transpose(1, 0, 2))
    c['ropes'] = np.ascontiguousarray(np.sin(ang).astype(np.float32).reshape(16, 128, 32).transpose(1, 0, 2))
    posf = np.arange(TOK, dtype=np.float32)
    posb = np.concatenate([255.0 - np.arange(256), 256.0 + 2047.0 - np.arange(2048)]).astype(np.float32)
    c['pos'] = np.ascontiguousarray(np.stack([np.tile(posf, (128, 1)), np.tile(posb, (128, 1))], axis=1))
    _CONSTS = c
    return c


_CONST_DT = {'dfc_l': BF16, 'dfs_l': BF16, 'dic_l': BF16, 'dis_l': BF16,
             'dfc_c': BF16, 'dfs_c': BF16, 'dic_c': BF16, 'dis_c': BF16}

_IN_SHAPES = {
    'x': [2048, 1024], 'c': [1024], 'ctx': [256, 1024], 'c_ctx': [1024],
    'ada_w': [2, 1024, 6144], 'ada_b': [2, 6144],
    'norm_mix_pre': [2, 1024], 'norm_mix_post': [2, 1024], 'norm_ffn_pre': [2, 1024], 'norm_ffn_post': [2, 1024],
    'ev_w_in': [1024, 2304], 'ev_hy_conv_w': [3, 1536], 'ev_hy_conv_b': [1536],
    'ev_hy_f_w1': [33, 64], 'ev_hy_f_b1': [64], 'ev_hy_f_w2': [64, 64], 'ev_hy_f_b2': [64],
    'ev_hy_f_wout': [64, 2048], 'ev_hy_freq': [64], 'ev_hy_skip': [1024],
    'ev_q_norm': [64], 'ev_k_norm': [64], 'ev_w_out': [1024, 1024],
    'ev_ffn_w_gate': [1, 1024, 2816], 'ev_ffn_w_up': [1, 1024, 2816], 'ev_ffn_w_down': [1, 2816, 1024],
    'od_w_in': [1024, 1024], 'od_s5_lambda_re': [2, 32, 128], 'od_s5_lambda_im': [2, 32, 128],
    'od_s5_log_step': [2, 32, 2], 'od_s5_b_re': [2, 64, 64, 16], 'od_s5_b_im': [2, 64, 64, 16],
    'od_s5_c_re': [2, 1024, 64], 'od_s5_c_im': [2, 1024, 64], 'od_s5_d': [1024],
    'od_glu_w_a': [1024, 1024], 'od_glu_w_b': [1024, 1024], 'od_router': [1024, 8],
    'od_moe_w_gate': [8, 1024, 3584], 'od_moe_w_up': [8, 1024, 3584], 'od_moe_w_down': [8, 3584, 1024],
}


def build(upto=99):
    nc = bass.Bass("TRN2", target_bir_lowering=False)
    S = Sched(nc)
    D = {}
    for k, shp in _IN_SHAPES.items():
        D[k] = nc.dram_tensor(k, list(shp), F32, kind="ExternalInput").ap()
    for k, v in _consts().items():
        D[k] = nc.dram_tensor(k, list(v.shape), _CONST_DT.get(k, F32), kind="ExternalInput").ap()
    xs_out = nc.dram_tensor("xs", [TOK, 1024], F32, kind="ExternalOutput").ap()
    xs = nc.dram_tensor("xs_scr", [TOK, 1024], F32, kind="Internal").ap()
    kscr = {nm: nc.dram_tensor("kscr_" + nm, [2, 2, L, 512], BF16, kind="Internal").ap()
            for nm, L in (('l', 2048), ('c', 256))}

    uid = [0]

    class Pool:
        def __init__(self):
            self.st = ExitStack()

        def sb(self, name, shape, dt=F32):
            uid[0] += 1
            t = self.st.enter_context(nc.sbuf_tensor("%s_%d" % (name, uid[0]), list(shape), dt))
            return t.ap()

        def ps(self, name, shape, dt=F32):
            uid[0] += 1
            epb = 2048 // (2 if dt == BF16 else 4)
            shape = [shape[0], ((shape[1] + epb - 1) // epb) * epb]
            t = self.st.enter_context(nc.psum_tensor("%s_%d" % (name, uid[0]), list(shape), dt))
            return t.ap()

        def close(self):
            S.barrier()
            self.st.close()

    def mm(out, lhsT, rhs, start=True, stop=True):
        S.op('pe', lambda e: e.matmul(out, lhsT=lhsT, rhs=rhs, start=start, stop=stop), reads=[lhsT, rhs], writes=[out])

    def tr(out, in_, ident):
        S.op('pe', lambda e: e.transpose(out, in_, ident), reads=[in_, ident], writes=[out])

    def _isap(x):
        return not isinstance(x, (int, float)) and x is not None

    def act(out, in_, func, bias=None, scale=None, accum=None):
        kw = {}
        rd = [in_]
        if bias is not None:
            kw['bias'] = bias
            if _isap(bias):
                rd.append(bias)
        if scale is not None:
            kw['scale'] = scale
            if _isap(scale):
                rd.append(scale)
        wr = [out]
        if accum is not None:
            kw['accum_out'] = accum
            wr.append(accum)
        S.op('act', lambda e: e.activation(out=out, in_=in_, func=func, **kw), reads=rd, writes=wr)

    def ts(out, in0, s1, s2=None, op0=ALU.mult, op1=None, eng='dve'):
        rd = [in0] + [s for s in (s1, s2) if _isap(s)]
        if op1 is None:
            S.op(eng, lambda e: e.tensor_scalar(out=out, in0=in0, scalar1=s1, scalar2=None, op0=op0), reads=rd, writes=[out])
        else:
            S.op(eng, lambda e: e.tensor_scalar(out=out, in0=in0, scalar1=s1, scalar2=s2, op0=op0, op1=op1), reads=rd, writes=[out])

    def tt(out, in0, in1, op, eng='dve'):
        S.op(eng, lambda e: e.tensor_tensor(out=out, in0=in0, in1=in1, op=op), reads=[in0, in1], writes=[out])

    def stt(out, in0, scalar, in1, op0, op1):
        rd = [in0, in1] + ([scalar] if _isap(scalar) else [])
        S.op('dve', lambda e: e.scalar_tensor_tensor(out=out, in0=in0, scalar=scalar, in1=in1, op0=op0, op1=op1), reads=rd, writes=[out])

    def cp(out, in_, eng='dve'):
        if eng == 'act':
            S.op('act', lambda e: e.copy(out=out, in_=in_), reads=[in_], writes=[out])
        else:
            S.op(eng, lambda e: e.tensor_copy(out=out, in_=in_), reads=[in_], writes=[out])

    def recip(out, in_):
        S.op('dve', lambda e: e.reciprocal(out=out, in_=in_), reads=[in_], writes=[out])

    def memset(ap, val, eng='pool'):
        S.op(eng, lambda e: e.memset(ap, val), writes=[ap])

    def dma(out, in_, eng='sp', **kw):
        S.dma(eng, out, in_, **kw)

    def v3(ap, b):
        return ap.rearrange("p (a b) -> p a b", b=b)

    def bc_last(ap, n):
        return ap.unsqueeze(2).to_broadcast([ap.shape[0], ap.shape[1], n])

    def bc_mid(ap, n):
        return ap.unsqueeze(1).to_broadcast([ap.shape[0], n, ap.shape[1]])

    G = Pool()
    ident = G.sb("ident", [128, 128])
    identb = G.sb("identb", [128, 128], BF16)
    ones = G.sb("ones", [128, 128])
    par = G.sb("par", [128, 2])
    colsA = G.sb("colsA", [128, 112])
    colsB = G.sb("colsB", [128, 120])
    coef = [G.sb("coef%d" % i, [128, 6, 2, 8]) for i in range(2)]
    dma(ident, D['ident'])
    dma(ones, D['ones'])
    dma(par, D['par'])
    cp(identb, ident)
    dma(xs[0:256, :], D['ctx'])
    dma(xs[256:TOK, :], D['x'])

    def phase_filters():
        P = Pool()
        w1 = P.sb("fw1", [33, 64]); w2 = P.sb("fw2", [64, 64]); wout = P.sb("fwout", [64, 2048])
        cols = P.sb("fcols", [64, 3]); frb = P.sb("ffrb", [64, 2])
        dma(w1, D['ev_hy_f_w1']); dma(w2, D['ev_hy_f_w2']); dma(wout, D['ev_hy_f_wout'])
        for i, k in enumerate(('ev_hy_f_b1', 'ev_hy_f_b2', 'ev_hy_freq')):
            dma(cols[:, i:i + 1], D[k].rearrange("(p o) -> p o", o=1))
        tt(frb[:, 0:1], cols[:, 0:1], cols[:, 2:3], ALU.mult)
        tt(frb[:, 1:2], cols[:, 1:2], cols[:, 2:3], ALU.mult)
        psm = [P.ps("fps%d" % i, [128, 512]) for i in range(4)]
        for nm, L in (('l', 2048), ('c', 256)):
            nt = L // 128
            zT = P.sb("fzT", [33, L]); h1 = P.sb("fh1", [64, L]); h2 = P.sb("fh2", [64, L])
            tmp = [P.sb("ftmp%d" % i, [64, 512]) for i in range(2)]
            tki = P.sb("ftki", [64, 512], I32); tkf = P.sb("ftkf", [64, 512])
            dec = P.sb("fdec", [128, nt, 512])
            dma(zT, D['zT_' + nm]); dma(dec, D['dec_' + nm])
            for (wm, src, dst, bcol) in ((w1, zT, h1, 0), (w2, h1, h2, 1)):
                for bi, b0 in enumerate(range(0, L, 512)):
                    n = min(512, L - b0)
                    ps = psm[bi % 2]
                    mm(ps[0:64, 0:n], wm, src[:, b0:b0 + n])
                    t_ = tmp[bi % 2]
                    ts(t_[:, 0:n], ps[0:64, 0:n], cols[:, 2:3], frb[:, bcol:bcol + 1], ALU.mult, ALU.add)
                    ts(tki[:, 0:n], t_[:, 0:n], 1.0 / (2 * PI), None, ALU.mult)
                    cp(tkf[:, 0:n], tki[:, 0:n])
                    stt(t_[:, 0:n], tkf[:, 0:n], -2 * PI, t_[:, 0:n], ALU.mult, ALU.add)
                    ts(t_[:, 0:n], t_[:, 0:n], PI, -PI, ALU.min, ALU.max)
                    act(dst[:, b0:b0 + n], t_[:, 0:n], AF.Sin)
            tf = [P.sb("ftf%d" % i, [128, 512]) for i in range(2)]
            tb = [P.sb("ftb%d" % i, [128, 512]) for i in range(2)]
            dft = [[P.sb("fdft%d%d" % (i, j), [128, nt, 128], BF16) for j in range(2)] for i in range(2)]
            kst = [[P.sb("fkst%d%d" % (i, j), [128, 512], BF16) for j in range(2)] for i in range(2)]
            for o in range(2):
                hs = P.sb("fhs", [128, nt, 512], BF16); hd = P.sb("fhd", [128, nt, 512], BF16)
                for t_i in range(nt):
                    pa = psm[0 + (t_i % 2) * 2]; pb = psm[1 + (t_i % 2) * 2]
                    mm(pa, h2[:, t_i * 128:(t_i + 1) * 128], wout[:, o * 512:(o + 1) * 512])
                    mm(pb, h2[:, t_i * 128:(t_i + 1) * 128], wout[:, 1024 + o * 512:1024 + (o + 1) * 512])
                    a = tf[t_i % 2]; b = tb[t_i % 2]
                    tt(a, pa, dec[:, t_i, :], ALU.mult)
                    tt(b, pb, dec[:, t_i, :], ALU.mult)
                    if t_i == 0:
                        memset(b[0:1, :], 0.0, eng='dve')
                    tt(hs[:, t_i, :], a, b, ALU.add, eng='pool')
                    tt(hd[:, t_i, :], a, b, ALU.subtract, eng='pool')
                for fc in range(nt):
                    cf = dft[fc % 2][0]; sf = dft[fc % 2][1]
                    dma(cf, D['dfc_' + nm][fc]); dma(sf, D['dfs_' + nm][fc])
                    pr = psm[(fc % 2) * 2]; pi_ = psm[(fc % 2) * 2 + 1]
                    for tc in range(nt):
                        mm(pr, cf[:, tc, :], hs[:, tc, :], start=(tc == 0), stop=(tc == nt - 1))
                    for tc in range(nt):
                        mm(pi_, sf[:, tc, :], hd[:, tc, :], start=(tc == 0), stop=(tc == nt - 1))
                    kr = kst[fc % 2][0]; ki = kst[fc % 2][1]
                    cp(kr, pr, eng='dve'); cp(ki, pi_, eng='act')
                    dma(kscr[nm][o, 0, fc * 128:(fc + 1) * 128, :], kr, key='kst')
                    dma(kscr[nm][o, 1, fc * 128:(fc + 1) * 128, :], ki, key='kst')
        P.close()

    def phase_ada():
        P = Pool()
        vecA = P.sb("vecA", [112, 128]); vecB = P.sb("vecB", [120, 128])
        dma(vecA[0:8, :], D['c'].rearrange("(r p) -> r p", p=128))
        dma(vecA[8:16, :], D['c_ctx'].rearrange("(r p) -> r p", p=128))
        for i in range(2):
            dma(vecA[16 + 48 * i:64 + 48 * i, :], D['ada_b'][i].rearrange("(r p) -> r p", p=128))
        r0 = 0
        for k in ('norm_mix_pre', 'norm_mix_post', 'norm_ffn_pre', 'norm_ffn_post'):
            dma(vecB[r0:r0 + 16, :], D[k].rearrange("i (r p) -> (i r) p", p=128))
            r0 += 16
        dma(vecB[64:100, :], D['ev_hy_conv_w'].rearrange("k (r p) -> (k r) p", p=128))
        dma(vecB[100:112, :], D['ev_hy_conv_b'].rearrange("(r p) -> r p", p=128))
        dma(vecB[112:120, :], D['od_s5_d'].rearrange("(r p) -> r p", p=128))
        pt = P.ps("apt", [128, 512])
        tr(pt[:, 0:112], vecA, ident[0:112, 0:112])
        cp(colsA, pt[:, 0:112])
        pt2 = P.ps("apt2", [128, 512])
        tr(pt2[:, 0:120], vecB, ident[0:120, 0:120])
        cp(colsB, pt2[:, 0:120])
        sc2 = P.sb("sc2", [128, 8, 2])
        act(sc2[:, :, 0], colsA[:, 0:8], AF.Silu)
        act(sc2[:, :, 1], colsA[:, 8:16], AF.Silu)
        aw = [P.sb("aw%d" % i, [128, 8, 512]) for i in range(3)]
        pm = [P.ps("apm%d" % i, [128, 96]) for i in range(2)]
        prow = [P.ps("aprow%d" % i, [128, 512]) for i in range(2)]
        rows = P.sb("arows", [2, 6144])
        mod = P.sb("mod", [128, 48, 2])
        for i in range(2):
            for nb in range(12):
                a = aw[nb % 3]
                dma(a, D['ada_w'][i][:, nb * 512:(nb + 1) * 512].rearrange("(kc p) n -> p kc n", p=128))
                pr = prow[nb % 2]
                for kc in range(8):
                    mm(pr[0:2, :], sc2[:, kc, :], a[:, kc, :], start=(kc == 0), stop=(kc == 7))
                cp(rows[:, nb * 512:(nb + 1) * 512], pr[0:2, :], eng=('act' if nb % 2 else 'dve'))
            for m in range(48):
                tr(pm[i][:, m * 2:m * 2 + 2], rows[:, m * 128:(m + 1) * 128], ident[0:2, 0:2])
            tt(mod, v3(pm[i][:, 0:96], 2), bc_last(colsA[:, 16 + 48 * i:64 + 48 * i], 2), ALU.add)
            nmp = colsB[:, 0 + 8 * i:8 + 8 * i]; nmpost = colsB[:, 16 + 8 * i:24 + 8 * i]
            nfp = colsB[:, 32 + 8 * i:40 + 8 * i]; nfpost = colsB[:, 48 + 8 * i:56 + 8 * i]
            for s in range(2):
                stt(coef[i][:, 0, s, :], mod[:, 8:16, s], 1.0, nmp, ALU.add, ALU.mult)
                cp(coef[i][:, 1, s, :], mod[:, 0:8, s])
                tt(coef[i][:, 2, s, :], mod[:, 16:24, s], nmpost, ALU.mult)
                stt(coef[i][:, 3, s, :], mod[:, 32:40, s], 1.0, nfp, ALU.add, ALU.mult)
                cp(coef[i][:, 4, s, :], mod[:, 24:32, s])
                tt(coef[i][:, 5, s, :], mod[:, 40:48, s], nfpost, ALU.mult)
        P.close()

    def make_bc(P, dst, col, pp):
        dg = [P.sb("dg%d" % i, [128, 128]) for i in range(2)]
        for j in range(8):
            d = dg[j % 2]
            ts(d, ident, col[:, j:j + 1], None, ALU.mult)
            mm(pp[:, j * 128:(j + 1) * 128], ones, d)
        cp(dst, pp)

    def norm_to_hT(P, i, kind, hT, tiles, pp, extra=None):
        xin = [P.sb("nx%d" % k, [128, 1024]) for k in range(3)]
        xn = [P.sb("nxn%d" % k, [128, 1024]) for k in range(3)]
        junk = P.sb("njunk", [128, 1024])
        st = P.sb("nst", [128, 3 * NT])
        ka = 0 if kind == 'mix' else 3
        def stageA(idx, n):
            xt = xin[idx % 3]; xo = xn[idx % 3]
            dma(xt, xs[n * 128:(n + 1) * 128, :])
            act(junk, xt, AF.Square, accum=st[:, n:n + 1])
            act(st[:, NT + n:NT + n + 1], st[:, n:n + 1], AF.Sqrt, bias=EPS, scale=1.0 / 1024)
            recip(st[:, 2 * NT + n:2 * NT + n + 1], st[:, NT + n:NT + n + 1])
            act(xo, xt, AF.Identity, scale=st[:, 2 * NT + n:2 * NT + n + 1])

        def stageB(idx, n):
            s = 1 if n < 2 else 0
            xo = xn[idx % 3]; p2 = pp[idx % 2]
            for j in range(8):
                tr(p2[:, j * 128:(j + 1) * 128], xo[:, j * 128:(j + 1) * 128], ident)
            for j in range(8):
                A = coef[i][:, ka, s, j:j + 1]; B = coef[i][:, ka + 1, s, j:j + 1]
                o = hT[:, j, idx * 128:(idx + 1) * 128]
                if j < 4:
                    ts(o, p2[:, j * 128:(j + 1) * 128], A, B, ALU.mult, ALU.add)
                else:
                    act(o, p2[:, j * 128:(j + 1) * 128], AF.Identity, bias=B, scale=A)
            if extra is not None:
                extra(idx, n, p2, coef[i][:, ka, s, :], coef[i][:, ka + 1, s, :])

        tl = list(tiles)
        stageA(0, tl[0])
        for idx in range(1, len(tl)):
            stageA(idx, tl[idx])
            stageB(idx - 1, tl[idx - 1])
        stageB(len(tl) - 1, tl[-1])

    def residual_update(P, bufs, idx, n, src, Gbc):
        xt, tmp, st, junk = bufs
        xt = xt[idx % 3]; tmp = tmp[idx % 3]
        c0 = (idx % 3) * 3
        dma(xt, xs[n * 128:(n + 1) * 128, :])
        act(junk, src, AF.Square, accum=st[:, c0:c0 + 1])
        if upto < 2.82:
            return
        act(st[:, c0 + 1:c0 + 2], st[:, c0:c0 + 1], AF.Sqrt, bias=EPS, scale=1.0 / 1024)
        recip(st[:, c0 + 2:c0 + 3], st[:, c0 + 1:c0 + 2])
        if upto < 2.83:
            return
        stt(tmp, src, st[:, c0 + 2:c0 + 3], Gbc, ALU.mult, ALU.mult)
        if upto < 2.84:
            return
        tt(tmp, tmp, xt, ALU.add, eng='pool')
        if upto < 2.85:
            return
        dma(xs[n * 128:(n + 1) * 128, :], tmp)

    def res_bufs(P):
        return ([P.sb("rx%d" % k, [128, 1024]) for k in range(3)], [P.sb("rt%d" % k, [128, 1024]) for k in range(3)],
                P.sb("rst", [128, 9]), P.sb("rjunk", [128, 1024]))

    def phase_even_mixer():
        P = Pool()
        z_tok = P.sb("z_tok", [128, NT, 1536], BF16)
        y_at = P.sb("y_at", [128, NT, 512], BF16)
        PH = Pool()
        hT = PH.sb("hT", [128, 8, TOK], BF16)
        P3 = Pool()
        pp = [P3.ps("pp%d" % k, [128, 1024]) for k in range(2)]
        norm_to_hT(P3, 0, 'mix', hT, list(range(NT)), pp)
        P3.close()
        if upto < 2.2:
            return
        PQ = Pool()
        QT = PQ.sb("QT", [64, 8, TOK], BF16)
        KT = PQ.sb("KT", [64, 2, TOK], BF16)
        Va = PQ.sb("Va", [128, NT, 2, 65], BF16)
        memset(Va, 1.0)
        P3 = Pool()
        W = P3.sb("Wqkv", [128, 8, 768], BF16)
        for kc in range(8):
            dma(W[:, kc, :], D['ev_w_in'][kc * 128:(kc + 1) * 128, 1536:2304], eng='pool')
        pp = [P3.ps("pp%d" % k, [128, 1024]) for k in range(2)]
        ptb = [P3.ps("ptb%d" % k, [128, 1024], BF16) for k in range(2)]
        gq = P3.sb("gq", [128, 64]); gk = P3.sb("gk", [128, 64])
        dma(gq, D['ev_q_norm'].partition_broadcast(128)); dma(gk, D['ev_k_norm'].partition_broadcast(128))
        qkg = P3.sb("qkg", [128, 10, 64])
        cp(qkg[:, 0:8, :], bc_mid(gq, 8)); cp(qkg[:, 8:10, :], bc_mid(gk, 2))
        ropec = P3.sb("ropec", [128, 16, 32]); ropes = P3.sb("ropes", [128, 16, 32])
        dma(ropec, D['ropec']); dma(ropes, D['ropes'])
        sq2 = [P3.sb("sq%d" % k, [128, 640]) for k in range(2)]; sst2 = [P3.sb("sst%d" % k, [128, 30]) for k in range(2)]
        qn2 = [P3.sb("qn%d" % k, [128, 10, 64]) for k in range(2)]; qr = [P3.sb("qr%d" % k, [128, 10, 64], BF16) for k in range(2)]
        rt2 = [[P3.sb("rt%d_%d" % (k, j), [128, 10, 32]) for k in range(4)] for j in range(2)]
        def stageA(n):
            sq = sq2[n % 2]; sst = sst2[n % 2]; qn = qn2[n % 2]; rt = rt2[n % 2]
            pq = pp[n % 2]
            for kc in range(8):
                mm(pq[:, 0:512], hT[:, kc, n * 128:(n + 1) * 128], W[:, kc, 0:512], start=(kc == 0), stop=(kc == 7))
            for kc in range(8):
                mm(pq[:, 512:768], hT[:, kc, n * 128:(n + 1) * 128], W[:, kc, 512:768], start=(kc == 0), stop=(kc == 7))
            act(sq, pq[:, 0:640], AF.Square)
            S.op('dve', lambda e, o=sst[:, 0:10], i_=v3(sq, 64): e.tensor_reduce(out=o, in_=i_, axis=AX.X, op=ALU.add),
                 reads=[sq], writes=[sst[:, 0:10]])
            act(sst[:, 10:20], sst[:, 0:10], AF.Sqrt, bias=EPS, scale=1.0 / 64)
            recip(sst[:, 20:30], sst[:, 10:20])
            tt(qn, v3(pq[:, 0:640], 64), bc_last(sst[:, 20:30], 64), ALU.mult)
            cp(Va[:, n, :, 0:64], v3(pq[:, 640:768], 64), eng='act')
            q_ = qr[n % 2]
            if n >= 2:
                tt(qn, qn, qkg, ALU.mult)
                cc = bc_mid(ropec[:, n - 2, :], 10); ss_ = bc_mid(ropes[:, n - 2, :], 10)
                x1 = qn[:, :, 0:32]; x2 = qn[:, :, 32:64]
                tt(rt[0], x1, cc, ALU.mult); tt(rt[1], x2, ss_, ALU.mult)
                tt(q_[:, :, 0:32], rt[0], rt[1], ALU.subtract, eng='pool')
                tt(rt[2], x1, ss_, ALU.mult); tt(rt[3], x2, cc, ALU.mult)
                tt(q_[:, :, 32:64], rt[2], rt[3], ALU.add, eng='pool')
            else:
                tt(q_, qn, qkg, ALU.mult)

        def stageB(n):
            q_ = qr[n % 2]
            pt0 = ptb[0]; pt1 = ptb[1]
            for h in range(8):
                tr(pt0[0:64, h * 128:(h + 1) * 128], q_[:, h, :], identb)
            for h in range(2):
                tr(pt1[0:64, h * 128:(h + 1) * 128], q_[:, 8 + h, :], identb)
            cp(QT[:, :, n * 128:(n + 1) * 128], v3(pt0[0:64, :], 128), eng='act')
            cp(KT[:, :, n * 128:(n + 1) * 128], v3(pt1[0:64, 0:256], 128), eng='dve')

        stageA(0)
        for n in range(1, NT):
            stageA(n)
            stageB(n - 1)
        stageB(NT - 1)
        P3.close()
        if upto < 2.3:
            return
        P3 = Pool()
        psc = [P3.ps("psc%d" % k, [128, 512]) for k in range(4)]
        po = [P3.ps("po%d" % k, [128, 512]) for k in range(2)]
        PT = [P3.sb("PT%d" % k, [128, NT, 512], BF16) for k in range(2)]
        rc = P3.sb("rc", [128, 8])
        it = 0
        for h in range(8):
            g = h // 4
            jobs = [(0, 256, [0, 1], 0)] + [(256 + qb * 512, 512, list(range(NT)), 2 + qb * 4) for qb in range(4)]
            for (q0, nq, kcs, tile0) in jobs:
                pt_ = PT[it % 2]
                for kc in kcs:
                    ps = psc[kc % 4]
                    mm(ps[:, 0:nq], KT[:, g, kc * 128:(kc + 1) * 128], QT[:, h, q0:q0 + nq])
                    act(pt_[:, kc, 0:nq], ps[:, 0:nq], AF.Exp, scale=0.125)
                pov = po[it % 2]
                nqt = nq // 128
                for qt in range(nqt):
                    for ki, kc in enumerate(kcs):
                        mm(pov[:, qt * 65:(qt + 1) * 65], pt_[:, kc, qt * 128:(qt + 1) * 128], Va[:, kc, g, :],
                           start=(ki == 0), stop=(ki == len(kcs) - 1))
                pv = v3(pov[:, 0:nqt * 65], 65)
                r_ = rc[:, (it % 2) * 4:(it % 2) * 4 + nqt]
                recip(r_, pv[:, :, 64])
                tt(y_at[:, tile0:tile0 + nqt, h * 64:(h + 1) * 64], pv[:, :, 0:64], bc_last(r_, 64), ALU.mult)
                it += 1
        P3.close()
        PQ.close()
        if upto < 2.4:
            return
        P3 = Pool()
        W = P3.sb("Why", [128, 8, 1536], BF16)
        for kc in range(8):
            dma(W[:, kc, :], D['ev_w_in'][kc * 128:(kc + 1) * 128, 0:1536], eng='pool')
        pa = [P3.ps("pa%d" % k, [128, 512]) for k in range(2)]
        ptb = [P3.ps("ptb%d" % k, [128, 1024], BF16) for k in range(2)]
        pc_c = [P3.sb("pc_c%d" % k, [128, 258]) for k in range(2)]
        pc_l = [P3.sb("pc_l%d" % k, [128, 2050]) for k in range(2)]
        for k in range(2):
            memset(pc_c[k], 0.0); memset(pc_l[k], 0.0)
        zf = P3.sb("zf", [128, TOK])
        zc = [P3.sb("zc%d" % k, [128, TOK], BF16) for k in range(2)]
        blocks = [(0, 256), (256, 512), (768, 512), (1280, 512), (1792, 512)]
        for c in range(12):
            pcc = pc_c[c % 2]; pcl = pc_l[c % 2]
            for bi, (t0, n) in enumerate(blocks):
                ps = pa[bi % 2]
                for kc in range(8):
                    mm(ps[:, 0:n], W[:, kc, c * 128:(c + 1) * 128], hT[:, kc, t0:t0 + n], start=(kc == 0), stop=(kc == 7))
                dst = pcc[:, 1:257] if t0 == 0 else pcl[:, 1 + t0 - 256:1 + t0 - 256 + n]
                cp(dst, ps[:, 0:n], eng='act')
            w0 = colsB[:, 64 + c:65 + c]; w1c = colsB[:, 76 + c:77 + c]; w2c = colsB[:, 88 + c:89 + c]
            bcl = colsB[:, 100 + c:101 + c]
            zcc = zc[c % 2]
            for (pc, L, off) in ((pcc, 256, 0), (pcl, 2048, 256)):
                ts(zf[:, off:off + L], pc[:, 1:L + 1], w1c, bcl, ALU.mult, ALU.add)
                stt(zf[:, off:off + L], pc[:, 0:L], w0, zf[:, off:off + L], ALU.mult, ALU.add)
                stt(zcc[:, off:off + L], pc[:, 2:L + 2], w2c, zf[:, off:off + L], ALU.mult, ALU.add)
            for gi, n0 in enumerate((0, 8, 16)):
                cnt = min(8, NT - n0)
                pt = ptb[gi % 2]
                for k in range(cnt):
                    tr(pt[:, k * 128:(k + 1) * 128], zcc[:, (n0 + k) * 128:(n0 + k + 1) * 128], identb)
                cp(z_tok[:, n0:n0 + cnt, c * 128:(c + 1) * 128], v3(pt[:, 0:cnt * 128], 128), eng=('act' if gi % 2 else 'dve'))
        P3.close()
        PH.close()
        if upto < 2.5:
            return
        P3 = Pool()
        skip = P3.sb("skip", [128, 1024])
        dma(skip, D['ev_hy_skip'].partition_broadcast(128))
        psm = [P3.ps("hps%d" % k, [128, 512]) for k in range(6)]
        for nm, L, tile0 in (('l', 2048, 2), ('c', 256, 0)):
            nt = L // 128
            Yr = P3.sb("Yr_" + nm, [128, nt, 512], BF16); Yi = P3.sb("Yi_" + nm, [128, nt, 512], BF16)
            v1 = P3.sb("v1_" + nm, [128, nt, 512], BF16)
            dft = [[P3.sb("hdft%s%d%d" % (nm, i, j), [128, nt, 128], BF16) for j in range(2)] for i in range(2)]
            tm = [P3.sb("htm%s%d" % (nm, i), [128, 512]) for i in range(6)]
            for o in range(2):
                vsrc = (lambda t_: z_tok[:, tile0 + t_, 0:512]) if o == 0 else (lambda t_: v1[:, t_, :])
                vdst = (lambda t_: v1[:, t_, :]) if o == 0 else (lambda t_: z_tok[:, tile0 + t_, 0:512])
                dma(Yr, kscr[nm][o, 0].rearrange("(f p) c -> p f c", p=128))
                dma(Yi, kscr[nm][o, 1].rearrange("(f p) c -> p f c", p=128))
                for fc in range(nt):
                    cf = dft[fc % 2][0]; sf = dft[fc % 2][1]
                    dma(cf, D['dfc_' + nm][fc]); dma(sf, D['dfs_' + nm][fc])
                    pr = psm[(fc % 2) * 2]; pi_ = psm[(fc % 2) * 2 + 1]
                    for tc in range(nt):
                        mm(pr, cf[:, tc, :], vsrc(tc), start=(tc == 0), stop=(tc == nt - 1))
                    for tc in range(nt):
                        mm(pi_, sf[:, tc, :], vsrc(tc), start=(tc == 0), stop=(tc == nt - 1))
                    tt(tm[0], pr, Yr[:, fc, :], ALU.mult); tt(tm[1], pi_, Yi[:, fc, :], ALU.mult)
                    tt(tm[2], pr, Yi[:, fc, :], ALU.mult); tt(tm[3], pi_, Yr[:, fc, :], ALU.mult)
                    tt(Yr[:, fc, :], tm[0], tm[1], ALU.subtract, eng='pool')
                    tt(Yi[:, fc, :], tm[2], tm[3], ALU.add, eng='pool')
                for t_i in range(nt):
                    ci = dft[t_i % 2][0]; si = dft[t_i % 2][1]
                    dma(ci, D['dic_' + nm][t_i]); dma(si, D['dis_' + nm][t_i])
                    py = psm[4 + t_i % 2]
                    for fc in range(nt):
                        mm(py, ci[:, fc, :], Yr[:, fc, :], start=(fc == 0), stop=False)
                    for fc in range(nt):
                        mm(py, si[:, fc, :], Yi[:, fc, :], start=False, stop=(fc == nt - 1))
                    a = tm[4 + t_i % 2]
                    tt(a, vsrc(t_i), skip[:, o * 512:(o + 1) * 512], ALU.mult)
                    tt(a, a, py, ALU.add)
                    tt(vdst(t_i), a, z_tok[:, tile0 + t_i, (o + 1) * 512:(o + 2) * 512], ALU.mult)
        P3.close()
        if upto < 2.6:
            return
        P3 = Pool()
        Wo = P3.sb("Wo", [128, 8, 1024], BF16)
        for kc in range(8):
            dma(Wo[:, kc, :], D['ev_w_out'][kc * 128:(kc + 1) * 128, :], eng='pool')
        pp = [P3.ps("opp%d" % k, [128, 1024]) for k in range(2)]
        ptb = [P3.ps("optb%d" % k, [128, 1024], BF16) for k in range(2)]
        Gbc = [P3.sb("Gbc%d" % s, [128, 1024]) for s in range(2)]
        for s in range(2):
            make_bc(P3, Gbc[s], coef[0][:, 2, s, :], pp[s])
        mixT = [P3.sb("mixT%d" % k, [128, 8, 128], BF16) for k in range(2)]
        rb = res_bufs(P3)
        if upto < 2.7:
            return
        for n in range(NT):
            pt = ptb[n % 2]; mt = mixT[n % 2]
            for j in range(4):
                tr(pt[:, j * 128:(j + 1) * 128], z_tok[:, n, j * 128:(j + 1) * 128], identb)
            for j in range(4):
                tr(pt[:, (4 + j) * 128:(5 + j) * 128], y_at[:, n, j * 128:(j + 1) * 128], identb)
            cp(mt, v3(pt, 128), eng='act')
            po_ = pp[n % 2]
            for hf in range(2):
                for j in range(8):
                    mm(po_[:, hf * 512:(hf + 1) * 512], mt[:, j, :], Wo[:, j, hf * 512:(hf + 1) * 512], start=(j == 0), stop=(j == 7))
            if upto >= 2.8:
                residual_update(P3, rb, n, n, po_, Gbc[1 if n < 2 else 0])
        P3.close()
        P.close()

    def phase_ffn(i, tiles, experts, dff, G_, router=None):
        P = Pool()
        ntl = len(tiles)
        ntok = ntl * 128
        hT = P.sb("fhT", [128, 8, ntok], BF16)
        acc = P.sb("facc", [128, ntl, 1024])
        gates = None
        P2 = Pool()
        pp = [P2.ps("fpp%d" % k, [128, 1024]) for k in range(2)]
        extra = None
        if router is not None:
            gates = P.sb("gates", [128, ntl, 8])
            rw = P2.sb("rw", [128, 8, 8])
            dma(rw, router.rearrange("(kc p) e -> p kc e", p=128))
            h32 = [P2.sb("h32_%d" % k, [128, 8, 128]) for k in range(2)]
            pl = [P2.ps("fpl%d" % k, [128, 8]) for k in range(2)]
            gs = P2.sb("gs", [128, 48])

            def extra(idx, n, p2, Acol, Bcol):
                h = h32[idx % 2]
                for j in range(8):
                    ts(h[:, j, :], p2[:, j * 128:(j + 1) * 128], Acol[:, j:j + 1], Bcol[:, j:j + 1], ALU.mult, ALU.add)
                plg = pl[idx % 2][:, 0:8]
                for j in range(8):
                    mm(plg, h[:, j, :], rw[:, j, :], start=(j == 0), stop=(j == 7))
                lg = gs[:, 0:8]; m8 = gs[:, 8:16]; ex = gs[:, 16:24]; mk = gs[:, 24:32]
                nm1 = gs[:, 32:33]; e2 = gs[:, 33:34]; rd = gs[:, 34:35]
                cp(lg, plg)
                S.op('dve', lambda e: e.max(out=m8, in_=lg), reads=[lg], writes=[m8])
                ts(nm1, m8[:, 0:1], -1.0, None, ALU.mult)
                act(ex, lg, AF.Exp, bias=nm1)
                act(e2, m8[:, 1:2], AF.Exp, bias=nm1)
                ts(mk, lg, m8[:, 1:2], None, ALU.is_ge)
                ts(e2, e2, 1.0, None, ALU.add)
                recip(rd, e2)
                tt(ex, ex, mk, ALU.mult)
                ts(gates[:, idx, :], ex, rd, None, ALU.mult)
        norm_to_hT(P2, i, 'ffn', hT, tiles, pp, extra=extra)
        P2.close()
        P2 = Pool()
        pg = [P2.ps("fpg%d" % k, [128, 512]) for k in range(2)]
        pu = [P2.ps("fpu%d" % k, [128, 512]) for k in range(2)]
        pd = [P2.ps("fpd%d" % k, [128, 1024]) for k in range(2)]
        groups = G_ if isinstance(G_, (list, tuple)) else [G_] * (dff // (G_ * 128))
        GM = max(groups)
        he = P2.sb("he", [128, GM, ntok], BF16)
        sg = [P2.sb("sg%d" % k, [128, 512]) for k in range(2)]
        Wg = [P2.sb("Wg%d" % k, [128, 8, GM * 128], BF16) for k in range(2)]
        Wu = [P2.sb("Wu%d" % k, [128, 8, GM * 128], BF16) for k in range(2)]
        Wd = [P2.sb("Wd%d" % k, [128, GM, 1024], BF16) for k in range(2)]
        blocks = []
        t0 = 0
        while t0 < ntok:
            n = min(512, ntok - t0)
            if t0 == 0 and ntok % 512 != 0:
                n = ntok % 512
            blocks.append((t0, n)); t0 += n
        it = 0
        first = True
        for e_i, (wg, wu, wd) in enumerate(experts):
            c0 = 0
            for Gc in groups:
                b = it % 2
                dma(Wg[b][:, :, 0:Gc * 128], wg[:, c0:c0 + Gc * 128].rearrange("(kc p) n -> p kc n", p=128), eng='pool')
                dma(Wu[b][:, :, 0:Gc * 128], wu[:, c0:c0 + Gc * 128].rearrange("(kc p) n -> p kc n", p=128), eng='pool')
                dma(Wd[b][:, 0:Gc, :], wd[c0:c0 + Gc * 128, :].rearrange("(c p) n -> p c n", p=128), eng='pool')
                c0 += Gc * 128
                k = 0
                for (t0, n) in blocks:
                    for c in range(Gc):
                        pg_ = pg[k % 2]; pu_ = pu[k % 2]; s_ = sg[k % 2]
                        for kc in range(8):
                            mm(pg_[:, 0:n], Wg[b][:, kc, c * 128:(c + 1) * 128], hT[:, kc, t0:t0 + n], start=(kc == 0), stop=(kc == 7))
                        for kc in range(8):
                            mm(pu_[:, 0:n], Wu[b][:, kc, c * 128:(c + 1) * 128], hT[:, kc, t0:t0 + n], start=(kc == 0), stop=(kc == 7))
                        act(s_[:, 0:n], pg_[:, 0:n], AF.Silu)
                        tt(he[:, c, t0:t0 + n], s_[:, 0:n], pu_[:, 0:n], ALU.mult)
                        k += 1
                for idx in range(ntl):
                    pd_ = pd[idx % 2]
                    for hf in range(2):
                        for c in range(Gc):
                            mm(pd_[:, hf * 512:(hf + 1) * 512], he[:, c, idx * 128:(idx + 1) * 128], Wd[b][:, c, hf * 512:(hf + 1) * 512],
                               start=(c == 0), stop=(c == Gc - 1))
                    a = acc[:, idx, :]
                    if gates is None:
                        if first:
                            cp(a, pd_, eng='dve')
                        else:
                            tt(a, pd_, a, ALU.add)
                    else:
                        gcol = gates[:, idx, e_i:e_i + 1]
                        if first:
                            ts(a, pd_, gcol, None, ALU.mult)
                        else:
                            stt(a, pd_, gcol, a, ALU.mult, ALU.add)
                first = False
                it += 1
        P2.close()
        P2 = Pool()
        pp = [P2.ps("fpp2_%d" % k, [128, 1024]) for k in range(2)]
        Gbc = [P2.sb("fGbc%d" % s, [128, 1024]) for s in range(2)]
        for s in range(2):
            make_bc(P2, Gbc[s], coef[i][:, 5, s, :], pp[s])
        rb = res_bufs(P2)
        for idx, n in enumerate(tiles):
            residual_update(P2, rb, idx, n, acc[:, idx, :], Gbc[1 if n < 2 else 0])
        P2.close()
        P.close()

    def phase_s5():
        P = Pool()
        uT = P.sb("uT", [128, 8, TOK], BF16)
        zT = P.sb("zT", [128, 8, 2048], BF16)
        pc = P.sb("s5pc", [128, 2, 3, 32])
        th = P.sb("s5th", [128, 2, 32]); mag = P.sb("s5mag", [128, 2, 32])
        co = P.sb("s5co", [128, 2, 2, 32])
        Bm = P.sb("s5B", [128, 2, 8, 2, 128], BF16)
        Cm = P.sb("s5C", [128, 2, 8, 2, 128], BF16)
        P2 = Pool()
        hT = P2.sb("shT", [128, 8, TOK], BF16)
        W = P2.sb("sW", [128, 8, 1024], BF16)
        for kc in range(8):
            dma(W[:, kc, :], D['od_w_in'][kc * 128:(kc + 1) * 128, :], eng='pool')
        pp = [P2.ps("spp%d" % k, [128, 1024]) for k in range(2)]
        norm_to_hT(P2, 1, 'mix', hT, list(range(NT)), pp)
        S.barrier()
        pa = [P2.ps("spa%d" % k, [128, 512]) for k in range(2)]
        blocks = [(0, 256), (256, 512), (768, 512), (1280, 512), (1792, 512)]
        k = 0
        for c in range(8):
            for (t0, n) in blocks:
                ps = pa[k % 2]
                for kc in range(8):
                    mm(ps[:, 0:n], W[:, kc, c * 128:(c + 1) * 128], hT[:, kc, t0:t0 + n], start=(kc == 0), stop=(kc == 7))
                cp(uT[:, c, t0:t0 + n], ps[:, 0:n], eng=('act' if k % 2 else 'dve'))
                k += 1
        P2.close()
        if upto < 4.2:
            return
        P2 = Pool()
        prm = P2.sb("prm", [32, 2, 3, 128])
        for d in range(2):
            dma(prm[:, d, 0, :], D['od_s5_lambda_re'][d]); dma(prm[:, d, 1, :], D['od_s5_lambda_im'][d])
            lsr = P2.sb("lsr%d" % d, [32, 2])
            dma(lsr, D['od_s5_log_step'][d])
            cp(v3(prm[:, d, 2, :], 64), bc_last(lsr, 64))
        ptp = P2.ps("sptp", [128, 512])
        for d in range(2):
            for q in range(3):
                tr(ptp[:, (d * 3 + q) * 32:(d * 3 + q + 1) * 32], prm[:, d, q, :], ident[0:32, 0:32])
        cp(pc, ptp[:, 0:192].rearrange("p (d q g) -> p d q g", d=2, q=3))
        w_ = [P2.sb("s5w%d" % k, [128, 2, 32]) for k in range(10)]
        lr, li, dt, ar, ai, den, nr, t1, t2, kk = w_
        ts(lr, pc[:, :, 0, :], -1e-4, None, ALU.min)
        cp(li, pc[:, :, 1, :])
        act(dt, pc[:, :, 2, :], AF.Exp)
        tt(t1, lr, dt, ALU.mult)
        act(mag, t1, AF.Exp)
        tt(th, li, dt, ALU.mult)
        ki = P2.sb("s5ki", [128, 2, 32], I32)
        ts(ki, th, 1.0 / (2 * PI), None, ALU.mult)
        cp(kk, ki)
        stt(t1, kk, -2 * PI, th, ALU.mult, ALU.add)
        ts(t1, t1, PI, -PI, ALU.min, ALU.max)
        act(ai, t1, AF.Sin)
        stt(t2, t1, -1.0, t1, ALU.mult, ALU.max)
        act(ar, t2, AF.Sin, bias=PI / 2, scale=-1.0)
        tt(ar, ar, mag, ALU.mult); tt(ai, ai, mag, ALU.mult)
        tt(den, lr, lr, ALU.mult); tt(t1, li, li, ALU.mult); tt(den, den, t1, ALU.add)
        recip(den, den)
        ts(nr, ar, -1.0, None, ALU.add)
        tt(t1, nr, lr, ALU.mult); tt(t2, ai, li, ALU.mult); tt(t1, t1, t2, ALU.add)
        tt(co[:, 0], t1, den, ALU.mult)
        tt(t1, ai, lr, ALU.mult); tt(t2, nr, li, ALU.mult); tt(t1, t1, t2, ALU.subtract)
        tt(co[:, 1], t1, den, ALU.mult)
        bin_ = [[P2.sb("s5bin%d%d" % (a, b), [128, 128]) for b in range(2)] for a in range(2)]
        bb = [[P2.sb("s5bb%d%d" % (a, b), [128, 128]) for b in range(2)] for a in range(2)]
        cin = [[P2.sb("s5cin%d%d" % (a, b), [128, 128]) for b in range(2)] for a in range(2)]
        craw = [[P2.sb("s5craw%d%d" % (a, b), [128, 64]) for b in range(2)] for a in range(2)]
        ptc = [P2.ps("sptc%d" % k, [128, 512]) for k in range(2)]
        for a in range(2):
            for b in range(2):
                memset(bin_[a][b], 0.0)
        it = 0
        for d in range(2):
            for c in range(8):
                q = it % 2
                for ri, key in enumerate(('od_s5_b_re', 'od_s5_b_im')):
                    for pr_ in range(2):
                        src = D[key][d][c * 8 + pr_:c * 8 + 8:2]
                        dst = bin_[q][ri][pr_ * 64:(pr_ + 1) * 64, :].rearrange("n (g two k) -> n g two k", two=2, k=16)[:, :, pr_, :]
                        dma(dst, src.rearrange("g n k -> n g k"))
                cr = co[:, 0, d, 4 * c:4 * c + 4].unsqueeze(2).to_broadcast([128, 4, 32])
                cim = co[:, 1, d, 4 * c:4 * c + 4].unsqueeze(2).to_broadcast([128, 4, 32])
                br_ = v3(bin_[q][0], 32); bi_ = v3(bin_[q][1], 32)
                o_re = v3(bb[q][0], 32); o_im = v3(bb[q][1], 32)
                tA = v3(cin[q][0], 32); tB = v3(cin[q][1], 32)
                tt(tA, br_, cr, ALU.mult); tt(tB, bi_, cim, ALU.mult); tt(o_re, tA, tB, ALU.subtract, eng='pool')
                tt(tA, bi_, cr, ALU.mult); tt(tB, br_, cim, ALU.mult); tt(o_im, tA, tB, ALU.add, eng='pool')
                pt_ = ptc[q]
                tr(pt_[:, 0:128], bb[q][0], ident); tr(pt_[:, 128:256], bb[q][1], ident)
                cp(Bm[:, d, c, :, :], v3(pt_[:, 0:256], 128), eng='act')
                for ri, key in enumerate(('od_s5_c_re', 'od_s5_c_im')):
                    dma(craw[q][ri], D[key][d][c * 128:(c + 1) * 128, :])
                    sgn = 1.0 if ri == 0 else -1.0
                    ts(cin[q][ri][:, 0:64], craw[q][ri], par[:, 0:1], sgn, ALU.mult, ALU.mult)
                    ts(cin[q][ri][:, 64:128], craw[q][ri], par[:, 1:2], sgn, ALU.mult, ALU.mult)
                tr(pt_[:, 256:384], cin[q][0], ident); tr(pt_[:, 384:512], cin[q][1], ident)
                cp(Cm[:, d, c, :, :], v3(pt_[:, 256:512], 128), eng='dve')
                it += 1
        P2.close()
        if upto < 4.3:
            return
        P2 = Pool()
        pos = P2.sb("s5pos", [128, TOK])
        rowmask = P2.sb("s5rm", [128, 4]); colmask = P2.sb("s5cm", [128, 4, 128])
        dma(rowmask, D['rowmask']); dma(colmask, D['colmask'])
        BmM = P2.sb("s5BmM", [128, 2, 128], BF16); CmM = P2.sb("s5CmM", [128, 2, 128], BF16)
        CmN = P2.sb("s5CmN", [128, 128], BF16)
        tht = P2.sb("s5tht", [128, 2, 32])
        ts(tht, th, 1.0 / (2 * PI), None, ALU.mult)
        dcol = colsB[:, 112:120]
        A_ = P2.sb("s5A", [128, TOK]); Y_ = P2.sb("s5Y", [128, TOK]); F1 = P2.sb("s5F1", [128, TOK])
        SINb = [P2.sb("s5SIN%d" % k, [128, TOK], BF16) for k in range(2)]
        COSb = [P2.sb("s5COS%d" % k, [128, TOK], BF16) for k in range(2)]
        BUr = P2.sb("s5BUr", [128, TOK], BF16); BUi = P2.sb("s5BUi", [128, TOK], BF16)
        Mre = P2.sb("s5Mre", [128, TOK], BF16); Mim = P2.sb("s5Mim", [128, TOK], BF16)
        Wre = P2.sb("s5Wre", [128, TOK], BF16); Wim = P2.sb("s5Wim", [128, TOK], BF16)
        T1 = P2.sb("s5T1", [128, TOK], BF16); T2 = P2.sb("s5T2", [128, TOK], BF16)
        T3 = P2.sb("s5T3", [128, TOK], BF16); T4 = P2.sb("s5T4", [128, TOK], BF16)
        Sre = P2.sb("s5Sre0", [128, 2048], BF16); Sim = P2.sb("s5Sim0", [128, 2048], BF16)
        pdr = [P2.ps("spdr%d" % k, [128, 512]) for k in range(4)]
        py = [P2.ps("spy%d" % k, [128, 512]) for k in range(4)]
        gl = [P2.sb("s5gl%d" % k, [128, 512]) for k in range(3)]
        blocks = [(0, 256), (256, 512), (768, 512), (1280, 512), (1792, 512)]
        MAGIC = 12582912.0

        def rev(ap, n):
            return bass.AP(ap.tensor, int(ap.offset) + n - 1, [list(ap.ap[0]), [-1, n]])

        tiles = [(c, d, m) for c in range(8) for d in range(2) for m in range(4)]

        S5X = ""

        def tablesA(k):
            c, d, m = tiles[k]
            if S5X == "notab" and k > 1:
                return
            if m == 0:
                dma(pos, D['pos'][:, d, :])
            thc = tht[:, d, 4 * c + m:4 * c + m + 1]
            act(A_, pos, AF.Identity, scale=thc)
            act(Y_, A_, AF.Identity, bias=MAGIC)

        def tablesB(k):
            SIN = SINb[k % 2]; COS = COSb[k % 2]
            if S5X == "notab" and k > 1:
                return
            stt(F1, Y_, -MAGIC, A_, ALU.add, ALU.subtract)
            act(SIN, F1, AF.Sin, scale=-6.283185)
            act(A_, F1, AF.Abs)
            act(COS, A_, AF.Sin, scale=-6.283185, bias=PI / 2)

        def drive(k):
            c, d, m = tiles[k]
            act(BmM, Bm[:, d, c, :, :], AF.Identity, scale=rowmask[:, m:m + 1])
            tt(CmM, Cm[:, d, c, :, :], bc_mid(colmask[:, m, :], 2), ALU.mult, eng='pool')
            ts(CmN, CmM[:, 0, :], -1.0, None, ALU.mult, eng='pool')
            for bi, (t0, n) in enumerate(blocks):
                pr_ = pdr[(bi % 2) * 2]; pi_ = pdr[(bi % 2) * 2 + 1]
                mm(pr_[:, 0:n], BmM[:, 0, :], uT[:, c, t0:t0 + n])
                mm(pi_[:, 0:n], BmM[:, 1, :], uT[:, c, t0:t0 + n])
                cp(BUr[:, t0:t0 + n], pr_[:, 0:n], eng='act')
                cp(BUi[:, t0:t0 + n], pi_[:, 0:n], eng='act')

        def stage2(k):
            c, d, m = tiles[k]
            SIN = SINb[k % 2]; COS = COSb[k % 2]
            rcol = mag[:, d, 4 * c + m:4 * c + m + 1]
            if not (S5X == "nomod" and k > 1):
                tt(T1, BUr, COS, ALU.mult); tt(T2, BUi, SIN, ALU.mult); tt(Mre, T1, T2, ALU.add)
                tt(T3, BUi, COS, ALU.mult); tt(T4, BUr, SIN, ALU.mult); tt(Mim, T3, T4, ALU.subtract)
            for (M_, W_) in ((Mre, Wre), (Mim, Wim)):
                for (t0, n) in ((0, 256), (256, 2048)):
                    if S5X == "noscan" and k > 1:
                        continue
                    if d == 0:
                        o_ = W_[:, t0:t0 + n]; i_ = M_[:, t0:t0 + n]
                        init = 0.0 if t0 == 0 else W_[:, 255:256]
                    else:
                        o_ = rev(W_[:, t0:t0 + n], n); i_ = rev(M_[:, t0:t0 + n], n)
                        init = 0.0 if t0 == 0 else W_[:, 0:1]
                    d0 = rcol.to_broadcast([128, n])
                    rd = [M_[:, t0:t0 + n], rcol] + ([init] if t0 else [])
                    S.op('dve', lambda e, o_=o_, i_=i_, d0=d0, init=init: e.tensor_tensor_scan(
                        out=o_, data0=d0, data1=i_, initial=init, op0=ALU.mult, op1=ALU.add),
                        reads=rd, writes=[W_[:, t0:t0 + n]])
            wl = Wre[:, 256:TOK]; wi = Wim[:, 256:TOK]; cl = COS[:, 256:TOK]; sl = SIN[:, 256:TOK]
            a1 = T1[:, 0:2048]; a2 = T2[:, 0:2048]; a3 = T3[:, 0:2048]; a4 = T4[:, 0:2048]
            a1 = Sre; a3 = Sim
            tt(a1, wl, cl, ALU.mult); tt(a2, wi, sl, ALU.mult)
            tt(a3, wl, sl, ALU.mult); tt(a4, wi, cl, ALU.mult)
            for b in range(4):
                bs = slice(b * 512, (b + 1) * 512)
                mm(py[b], CmM[:, 0, :], a1[:, bs], start=(d == 0 and m == 0), stop=False)
                mm(py[b], CmN, a2[:, bs], start=False, stop=False)
                mm(py[b], CmM[:, 1, :], a3[:, bs], start=False, stop=False)
                mm(py[b], CmM[:, 1, :], a4[:, bs], start=False, stop=(d == 1 and m == 3))
            if d == 1 and m == 3:
                for b in range(4):
                    y = gl[0]; a = gl[1]; b_ = gl[2]
                    stt(y, uT[:, c, 256 + b * 512:256 + (b + 1) * 512], dcol[:, c:c + 1], py[b], ALU.mult, ALU.add)
                    act(a, y, AF.Square)
                    ts(a, a, 0.044715, 1.0, ALU.mult, ALU.add)
                    tt(a, a, y, ALU.mult)
                    act(b_, a, AF.Sigmoid, scale=2.0 * math.sqrt(2.0 / PI))
                    tt(zT[:, c, b * 512:(b + 1) * 512], b_, y, ALU.mult)

        tablesA(0); tablesB(0)
        for k in range(len(tiles)):
            c, d, m = tiles[k]
            last_of_chunk = False
            if k + 1 < len(tiles) and not last_of_chunk:
                tablesA(k + 1)
            drive(k)
            if k + 1 < len(tiles) and not last_of_chunk:
                tablesB(k + 1)
            stage2(k)
            if k + 1 < len(tiles) and last_of_chunk:
                tablesA(k + 1); tablesB(k + 1)
        P2.close()
        if upto < 4.4:
            return
        P2 = Pool()
        Wa = P2.sb("Wa", [128, 8, 1024], BF16); Wb = P2.sb("Wb", [128, 8, 1024], BF16)
        for kc in range(8):
            dma(Wa[:, kc, :], D['od_glu_w_a'][kc * 128:(kc + 1) * 128, :], eng='pool')
            dma(Wb[:, kc, :], D['od_glu_w_b'][kc * 128:(kc + 1) * 128, :], eng='pool')
        ppa = [P2.ps("gpa%d" % k, [128, 1024]) for k in range(2)]
        ppb = [P2.ps("gpb%d" % k, [128, 1024]) for k in range(2)]
        Gbc = P2.sb("gGbc", [128, 1024])
        make_bc(P2, Gbc, coef[1][:, 2, 0, :], ppa[0])
        sgm = [P2.sb("gsg%d" % k, [128, 1024]) for k in range(2)]
        go = [P2.sb("ggo%d" % k, [128, 1024]) for k in range(2)]
        rb = res_bufs(P2)
        for idx in range(16):
            pa_ = ppa[idx % 2]; pb_ = ppb[idx % 2]
            for (pp_, Wx) in ((pa_, Wa), (pb_, Wb)):
                for hf in range(2):
                    for j in range(8):
                        mm(pp_[:, hf * 512:(hf + 1) * 512], zT[:, j, idx * 128:(idx + 1) * 128], Wx[:, j, hf * 512:(hf + 1) * 512],
                           start=(j == 0), stop=(j == 7))
            act(sgm[idx % 2], pb_, AF.Sigmoid)
            tt(go[idx % 2], pa_, sgm[idx % 2], ALU.mult)
            residual_update(P2, rb, idx, 2 + idx, go[idx % 2], Gbc)
        P2.close()
        P.close()

    if upto >= 1:
        phase_filters()
    if upto >= 2:
        phase_ada()
    if upto > 2:
        phase_even_mixer()
    if upto >= 4:
        phase_ffn(0, list(range(NT)), [(D['ev_ffn_w_gate'][0], D['ev_ffn_w_up'][0], D['ev_ffn_w_down'][0])], 2816, [5, 5, 4, 4, 4])
    if upto > 4:
        phase_s5()
    if upto >= 6:
        phase_ffn(1, list(range(2, NT)),
                  [(D['od_moe_w_gate'][e], D['od_moe_w_up'][e], D['od_moe_w_down'][e]) for e in range(8)],
                  3584, 4, router=D['od_router'])
    S.barrier()
    dma(xs_out, xs)
    S.barrier()
    S.emit()
    return nc


_NC = {}


def _prep_inputs(inputs, b):
    g = lambda k: np.ascontiguousarray(np.asarray(inputs[k], dtype=np.float32))
    m = {}
    m['x'] = g('x')[b]; m['c'] = g('c')[b]; m['ctx'] = g('ctx')[b]; m['c_ctx'] = g('c_ctx')
    for k in ('ada_w', 'ada_b', 'norm_mix_pre', 'norm_mix_post', 'norm_ffn_pre', 'norm_ffn_post',
              'ev_ffn_w_gate', 'ev_ffn_w_up', 'ev_ffn_w_down'):
        m[k] = g(k)
    for k in ('ev_w_in', 'ev_hy_conv_w', 'ev_hy_conv_b', 'ev_hy_f_w1', 'ev_hy_f_b1', 'ev_hy_f_w2', 'ev_hy_f_b2',
              'ev_hy_f_wout', 'ev_hy_freq', 'ev_q_norm', 'ev_k_norm', 'ev_w_out', 'od_w_in', 'od_s5_d',
              'od_glu_w_a', 'od_glu_w_b', 'od_router', 'od_moe_w_gate', 'od_moe_w_up', 'od_moe_w_down',
              'od_s5_b_re', 'od_s5_b_im'):
        m[k] = g(k)[0]
    m['ev_hy_skip'] = g('ev_hy_skip')[0].reshape(1024)
    m['od_s5_lambda_re'] = g('od_s5_lambda_re')[0].reshape(2, 32, 128)
    m['od_s5_lambda_im'] = g('od_s5_lambda_im')[0].reshape(2, 32, 128)
    m['od_s5_log_step'] = g('od_s5_log_step')[0].reshape(2, 32, 2)
    m['od_s5_c_re'] = g('od_s5_c_re')[0].reshape(2, 1024, 64)
    m['od_s5_c_im'] = g('od_s5_c_im')[0].reshape(2, 1024, 64)
    return m


def kernel(_upto=99, _cores=8, **inputs):
    if _upto not in _NC:
        _NC[_upto] = build(_upto)
    nc = _NC[_upto]
    consts = _consts()
    shared = None
    in_maps = []
    for b in range(_cores):
        m = _prep_inputs(inputs, b)
        if shared is None:
            shared = {k: v for k, v in m.items() if k not in ('x', 'c', 'ctx')}
        else:
            for k in shared:
                m[k] = shared[k]
        m.update(consts)
        in_maps.append(m)
    res = run_bass_kernel_spmd(nc, in_maps, core_ids=list(range(_cores)))
    outs = [np.asarray(r["xs"], dtype=np.float32) for r in res.results]
    if _upto < 99:
        return np.stack(outs, axis=0)
    return np.stack([o[256:] for o in outs], axis=0).astype(np.float32)
```
